# Optimizing a Trainium2 kernel written in Bass

```python
import math
import jax, jax.numpy as jnp
from jax import lax
import numpy as np

D_MODEL = 1024
BATCH = 8
SEQ = 2048
DEPTH = 1

MEM_LEN = 256
CHUNK = 128
A_GROUPS = 8
A_WIDTH = D_MODEL
A_GROUP_DIM = A_WIDTH // A_GROUPS
SB_HEADS = 8
SB_HEAD_DIM = D_MODEL // SB_HEADS
SB_WIDTH = SB_HEADS * SB_HEAD_DIM
Q_BLOCK = 128
MEM_HEADS = 4
MEM_HEAD_DIM = 128
MEM_WIDTH = MEM_HEADS * MEM_HEAD_DIM
N_EXPERTS = 64
TOP_K = 8
N_GROUPS = 8
TOPK_GROUPS = 4
EXPERT_HIDDEN = 256
SHARED_HIDDEN = 256
ROUTED_SCALE = 2.5
MOE_BLOCK = 256
LN_EPS = 1e-5
ALPHA = (2 * DEPTH) ** 0.25
BETA = (8 * DEPTH) ** -0.25
IN_WIDTH = 2 * A_WIDTH + 3 * SB_WIDTH + 2 * D_MODEL

kernel_name = "hybrid_gmlp_stickbreak_mem_moe_deepnorm"


def layer_norm(x, g, b):
    xf = x.astype(jnp.float32)
    mu = jnp.mean(xf, axis=-1, keepdims=True)
    var = jnp.mean(jnp.square(xf - mu), axis=-1, keepdims=True)
    y = (xf - mu) * lax.rsqrt(var + LN_EPS) * g.astype(jnp.float32) + b.astype(jnp.float32)
    return y.astype(x.dtype)


def chunk_gmlp(u, v, ln_g, ln_b, w_s, b_s):
    bsz, seq, _ = u.shape
    n_chunks = seq // CHUNK
    v = layer_norm(v, ln_g, ln_b).reshape(bsz, n_chunks, CHUNK, A_GROUPS, A_GROUP_DIM)
    w = jnp.tril(w_s)
    mixed = jnp.einsum('gts,bcsgd->bctgd', w, v) + b_s.T[None, None, :, :, None]
    return u * mixed.reshape(bsz, seq, A_WIDTH)


def stick_breaking_attention(q, k, v):
    seq = q.shape[1]
    scale = SB_HEAD_DIM ** -0.5
    outs = []
    for i in range(seq // Q_BLOCK):
        q0 = i * Q_BLOCK
        kend = q0 + Q_BLOCK
        qb = q[:, q0:kend]
        kb = k[:, :kend]
        vb = v[:, :kend]
        z = jnp.einsum('bqhd,bkhd->bhqk', qb, kb,
                       preferred_element_type=jnp.float32) * scale
        t_pos = q0 + jnp.arange(Q_BLOCK)
        s_pos = jnp.arange(kend)
        mask = s_pos[None, :] < t_pos[:, None]
        log_stay = jnp.where(mask, jax.nn.log_sigmoid(-z), 0.0)
        log_w = jax.nn.log_sigmoid(z) + lax.cumsum(log_stay, axis=3, reverse=True) - log_stay
        a = jnp.where(mask, jnp.exp(log_w), 0.0)
        outs.append(jnp.einsum('bhqk,bkhd->bqhd', a.astype(v.dtype), vb))
    return jnp.concatenate(outs, axis=1)


def memory_attention(x, mem, w_q, w_kv, w_o):
    bsz, seq, _ = x.shape
    q = (x @ w_q).reshape(bsz, seq, MEM_HEADS, MEM_HEAD_DIM)
    k, v = jnp.split(mem @ w_kv, 2, axis=-1)
    k = k.reshape(bsz, MEM_LEN, MEM_HEADS, MEM_HEAD_DIM)
    v = v.reshape(bsz, MEM_LEN, MEM_HEADS, MEM_HEAD_DIM)
    logits = jnp.einsum('bqhd,bmhd->bhqm', q, k,
                        preferred_element_type=jnp.float32) * (MEM_HEAD_DIM ** -0.5)
    p = jax.nn.softmax(logits, axis=-1).astype(v.dtype)
    o = jnp.einsum('bhqm,bmhd->bqhd', p, v).reshape(bsz, seq, MEM_WIDTH)
    return o @ w_o


def swiglu(x, w_gate, w_up, w_down):
    return (jax.nn.silu(x @ w_gate) * (x @ w_up)) @ w_down


def route(xf, w_router, e_bias):
    n_tok = xf.shape[0]
    scores = jax.nn.sigmoid(xf.astype(jnp.float32) @ w_router.astype(jnp.float32))
    sel = scores + e_bias.astype(jnp.float32)[None, :]
    grouped = sel.reshape(n_tok, N_GROUPS, N_EXPERTS // N_GROUPS)
    group_score = jnp.sum(lax.top_k(grouped, 2)[0], axis=-1)
    _, gidx = lax.top_k(group_score, TOPK_GROUPS)
    gmask = jnp.any(gidx[:, :, None] == jnp.arange(N_GROUPS)[None, None, :], axis=1)
    emask = jnp.repeat(gmask, N_EXPERTS // N_GROUPS, axis=1)
    sel = jnp.where(emask, sel, -jnp.inf)
    _, idx = lax.top_k(sel, TOP_K)
    w = jnp.take_along_axis(scores, idx, axis=1)
    w = w / jnp.sum(w, axis=-1, keepdims=True) * ROUTED_SCALE
    return idx, w


def routed_experts(xf, idx, gate_w, w_gate, w_up, w_down):
    n_tok, d = xf.shape
    n_assign = n_tok * TOP_K
    flat_e = idx.reshape(-1).astype(jnp.int32)
    flat_tok = jnp.repeat(jnp.arange(n_tok, dtype=jnp.int32), TOP_K)
    flat_w = gate_w.reshape(-1)
    order = jnp.argsort(flat_e)
    e_sorted = flat_e[order]
    tok_sorted = flat_tok[order]
    w_sorted = flat_w[order]
    counts = jnp.bincount(flat_e, length=N_EXPERTS).astype(jnp.int32)
    start = jnp.cumsum(counts) - counts
    padded = (counts + MOE_BLOCK - 1) // MOE_BLOCK * MOE_BLOCK
    pad_end = jnp.cumsum(padded)
    pad_start = pad_end - padded
    dest = pad_start[e_sorted] + (jnp.arange(n_assign, dtype=jnp.int32) - start[e_sorted])
    n_blocks = -(-n_assign // MOE_BLOCK) + N_EXPERTS
    n_rows = n_blocks * MOE_BLOCK
    row_tok = jnp.full((n_rows,), n_tok, jnp.int32).at[dest].set(tok_sorted)
    row_w = jnp.zeros((n_rows,), xf.dtype).at[dest].set(w_sorted.astype(xf.dtype))
    block_start = jnp.arange(n_blocks, dtype=jnp.int32) * MOE_BLOCK
    block_e = jnp.minimum(jnp.searchsorted(pad_end, block_start, side='right'),
                          N_EXPERTS - 1).astype(jnp.int32)
    x_pad = jnp.concatenate([xf, jnp.zeros((1, d), xf.dtype)], axis=0)

    def block_fn(args):
        toks, e = args
        xb = x_pad[toks]
        return swiglu(xb, w_gate[e], w_up[e], w_down[e])

    yb = lax.map(block_fn, (row_tok.reshape(n_blocks, MOE_BLOCK), block_e))
    y = yb.reshape(n_rows, d) * row_w[:, None]
    return jax.ops.segment_sum(y, row_tok, num_segments=n_tok + 1)[:n_tok]


def setup_inputs(seed: int = 0) -> dict:
    key = jax.random.key(seed)
    ks = jax.random.split(key, 32)
    f32 = jnp.float32

    def nrm(k, shape, scale):
        return jax.random.normal(k, shape, f32) * scale

    return {
        "x": nrm(ks[0], (BATCH, SEQ, D_MODEL), 1.0),
        "mem": nrm(ks[1], (BATCH, MEM_LEN, D_MODEL), 1.0),
        "ln_in_g": 1.0 + nrm(ks[2], (D_MODEL,), 0.02),
        "ln_in_b": nrm(ks[3], (D_MODEL,), 0.02),
        "w_in": nrm(ks[4], (DEPTH, D_MODEL, IN_WIDTH), D_MODEL ** -0.5),
        "b_in": nrm(ks[5], (DEPTH, IN_WIDTH), 0.02),
        "ln_v_g": 1.0 + nrm(ks[6], (DEPTH, A_WIDTH), 0.02),
        "ln_v_b": nrm(ks[7], (DEPTH, A_WIDTH), 0.02),
        "w_spatial": nrm(ks[8], (DEPTH, A_GROUPS, CHUNK, CHUNK), CHUNK ** -0.5),
        "b_spatial": 1.0 + nrm(ks[9], (DEPTH, A_GROUPS, CHUNK), 0.02),
        "w_out": nrm(ks[10], (DEPTH, D_MODEL, D_MODEL), BETA * D_MODEL ** -0.5),
        "ln1_g": 1.0 + nrm(ks[11], (DEPTH, D_MODEL), 0.02),
        "ln1_b": nrm(ks[12], (DEPTH, D_MODEL), 0.02),
        "w_mem_q": nrm(ks[13], (DEPTH, D_MODEL, MEM_WIDTH), D_MODEL ** -0.5),
        "w_mem_kv": nrm(ks[14], (DEPTH, D_MODEL, 2 * MEM_WIDTH), D_MODEL ** -0.5),
        "w_mem_o": nrm(ks[15], (DEPTH, MEM_WIDTH, D_MODEL), BETA * MEM_WIDTH ** -0.5),
        "ln2_g": 1.0 + nrm(ks[16], (DEPTH, D_MODEL), 0.02),
        "ln2_b": nrm(ks[17], (DEPTH, D_MODEL), 0.02),
        "w_router": nrm(ks[18], (DEPTH, D_MODEL, N_EXPERTS), D_MODEL ** -0.5),
        "router_bias": nrm(ks[19], (DEPTH, N_EXPERTS), 0.01),
        "w_exp_gate": nrm(ks[20], (DEPTH, N_EXPERTS, D_MODEL, EXPERT_HIDDEN), D_MODEL ** -0.5),
        "w_exp_up": nrm(ks[21], (DEPTH, N_EXPERTS, D_MODEL, EXPERT_HIDDEN), D_MODEL ** -0.5),
        "w_exp_down": nrm(ks[22], (DEPTH, N_EXPERTS, EXPERT_HIDDEN, D_MODEL), BETA * EXPERT_HIDDEN ** -0.5),
        "w_sh_gate": nrm(ks[23], (DEPTH, D_MODEL, SHARED_HIDDEN), D_MODEL ** -0.5),
        "w_sh_up": nrm(ks[24], (DEPTH, D_MODEL, SHARED_HIDDEN), D_MODEL ** -0.5),
        "w_sh_down": nrm(ks[25], (DEPTH, SHARED_HIDDEN, D_MODEL), BETA * SHARED_HIDDEN ** -0.5),
        "ln3_g": 1.0 + nrm(ks[26], (DEPTH, D_MODEL), 0.02),
        "ln3_b": nrm(ks[27], (DEPTH, D_MODEL), 0.02),
    }


def reference(x, mem, ln_in_g, ln_in_b, w_in, b_in, ln_v_g, ln_v_b, w_spatial, b_spatial,
              w_out, ln1_g, ln1_b, w_mem_q, w_mem_kv, w_mem_o, ln2_g, ln2_b,
              w_router, router_bias, w_exp_gate, w_exp_up, w_exp_down,
              w_sh_gate, w_sh_up, w_sh_down, ln3_g, ln3_b):
    bsz, seq, d = x.shape
    split_at = [A_WIDTH, 2 * A_WIDTH,
                2 * A_WIDTH + SB_WIDTH,
                2 * A_WIDTH + 2 * SB_WIDTH,
                2 * A_WIDTH + 3 * SB_WIDTH,
                2 * A_WIDTH + 3 * SB_WIDTH + D_MODEL]
    x = layer_norm(x, ln_in_g, ln_in_b)
    for l in range(DEPTH):
        proj = x @ w_in[l] + b_in[l]
        u_a, v_a, q_b, k_b, v_b, g_a, g_b = jnp.split(proj, split_at, axis=-1)
        y_a = chunk_gmlp(jax.nn.gelu(u_a, approximate=False), jax.nn.gelu(v_a, approximate=False),
                         ln_v_g[l], ln_v_b[l], w_spatial[l], b_spatial[l])
        heads = (bsz, seq, SB_HEADS, SB_HEAD_DIM)
        y_b = stick_breaking_attention(q_b.reshape(heads), k_b.reshape(heads),
                                       v_b.reshape(heads)).reshape(bsz, seq, SB_WIDTH)
        merged = jax.nn.sigmoid(g_a) * y_a + jax.nn.sigmoid(g_b) * y_b
        x = layer_norm(ALPHA * x + merged @ w_out[l], ln1_g[l], ln1_b[l])
        m = memory_attention(x, mem, w_mem_q[l], w_mem_kv[l], w_mem_o[l])
        x = layer_norm(ALPHA * x + m, ln2_g[l], ln2_b[l])
        xf = x.reshape(bsz * seq, d)
        idx, gate_w = route(xf, w_router[l], router_bias[l])
        moe = swiglu(xf, w_sh_gate[l], w_sh_up[l], w_sh_down[l]) + \
            routed_experts(xf, idx, gate_w, w_exp_gate[l], w_exp_up[l], w_exp_down[l])
        x = layer_norm(ALPHA * x + moe.reshape(bsz, seq, d), ln3_g[l], ln3_b[l])
    return x
```

```python
import os
import contextlib
import numpy as np
import ml_dtypes
import concourse.bass as bass
import concourse.mybir as mybir
from concourse.bass_utils import run_bass_kernel_spmd

F32 = mybir.dt.float32
BF16 = mybir.dt.bfloat16
AF = mybir.ActivationFunctionType
ALU = mybir.AluOpType
AX = mybir.AxisListType

S = 2048
D = 1024
NT = 16
NKC = 8
ALPHA = 2.0 ** 0.25
LN_EPS = 1e-5
SB_SCALE = 128.0 ** -0.5
MEM_SCALE = 128.0 ** -0.5
NEXP = 64
ROUTED_SCALE = 2.5
CAP = 512
NSLOT = NEXP * CAP
U32 = mybir.dt.uint32


class Tok:
    __slots__ = ("w", "r")

    def __init__(self):
        self.w = None
        self.r = {}


def toks(n):
    return [Tok() for _ in range(n)]


class Eng:
    def __init__(self, name, h, sem):
        self.name = name
        self.h = h
        self.sem = sem
        self.cnt = 0
        self.known = {}
        self.pool = []
        self.dma_i = 0


class Sched:
    def __init__(self, nc, st, ndma=12):
        self.nc = nc
        mk = lambda n: st.enter_context(nc.semaphore(n))
        self.pe = Eng("pe", nc.tensor, mk("s_pe"))
        self.act = Eng("act", nc.scalar, mk("s_act"))
        self.dve = Eng("dve", nc.vector, mk("s_dve"))
        self.pool = Eng("pool", nc.gpsimd, mk("s_pool"))
        self.sp = Eng("sp", nc.sync, mk("s_sp"))
        self.engs = [self.pe, self.act, self.dve, self.pool, self.sp]
        for q in (self.sp, self.pool, self.act):
            q.pool = [[mk("d_%s_%d" % (q.name, i)), 0] for i in range(ndma)]

    def _emit_waits(self, eng, deps):
        for sem, val in deps.items():
            if eng.known.get(sem, 0) < val:
                if sem is eng.sem:
                    assert val <= eng.cnt, "self-wait on future count"
                eng.h.wait_ge(sem, val)
                eng.known[sem] = val

    def _deps(self, eng, reads, writes):
        deps = {}

        def add(d, raw):
            sem, val = d
            if sem is eng.sem and not raw:
                return
            if deps.get(sem, 0) < val:
                deps[sem] = val

        for t in reads:
            if t.w is not None:
                add(t.w, True)
        for t in writes:
            if t.w is not None:
                add(t.w, False)
            for sem, val in t.r.items():
                add((sem, val), False)
        return deps

    def _mark(self, mark, reads, writes):
        for t in reads:
            if t.r.get(mark[0], 0) < mark[1]:
                t.r[mark[0]] = mark[1]
        for t in writes:
            t.w = mark
            t.r = {}

    def op(self, eng, fn, reads=(), writes=(), sig=True):
        self._emit_waits(eng, self._deps(eng, reads, writes))
        ins = fn()
        if sig:
            ins.then_inc(eng.sem, 1)
            eng.cnt += 1
            mark = (eng.sem, eng.cnt)
        else:
            mark = (eng.sem, eng.cnt + 1)
        self._mark(mark, reads, writes)
        return ins

    def dma(self, q, out, in_, reads=(), writes=(), fn=None, **kw):
        slot = q.pool[q.dma_i % len(q.pool)]
        q.dma_i += 1
        deps = self._deps(q, reads, writes)
        if slot[1] > 0:
            deps[slot[0]] = max(deps.get(slot[0], 0), 16 * slot[1])
        self._emit_waits(q, deps)
        ins = fn() if fn is not None else q.h.dma_start(out=out, in_=in_, **kw)
        ins.then_inc(slot[0], 16)
        slot[1] += 1
        self._mark((slot[0], 16 * slot[1]), reads, writes)
        return ins

    def barrier(self, skip_pool_dma=False):
        targets = {}
        for e in (self.pe, self.act, self.dve, self.pool):
            if e.cnt > 0:
                targets[e.sem] = e.cnt
        for q in ((self.sp, self.act) if skip_pool_dma else (self.sp, self.pool, self.act)):
            for sem, used in q.pool:
                if used > 0:
                    targets[sem] = 16 * used
        for e in self.engs:
            d = {s: v for s, v in targets.items() if not (s is e.sem and e.name == "pe")}
            self._emit_waits(e, d)


def build(dbg=False, stop=99):
    nc = bass.Bass("TRN2", target_bir_lowering=False)

    def din(name, shape):
        return nc.dram_tensor(name, list(shape), F32, kind="ExternalInput").ap()

    x_d = din("x", [S, D])
    mem_d = din("mem", [256, D])
    ln_in_g = din("ln_in_g", [1, D])
    ln_in_b = din("ln_in_b", [1, D])
    w_in = din("w_in", [D, 7168])
    b_in = din("b_in", [56, 128])
    ln_v_g = din("ln_v_g", [1, D])
    ln_v_b = din("ln_v_b", [1, D])
    w_sp = din("w_spatial", [8, 128, 128])
    b_sp = din("b_spatial", [8, 128])
    w_out = din("w_out", [D, D])
    ln1_g = din("ln1_g", [1, D])
    ln1_b = din("ln1_b", [1, D])
    w_mq = din("w_mem_q", [D, 512])
    w_mkv = din("w_mem_kv", [D, 1024])
    w_mo = din("w_mem_o", [512, D])
    ln2_g = din("ln2_g", [1, D])
    ln2_b = din("ln2_b", [1, D])
    w_r = din("w_router", [D, NEXP])
    r_b = din("router_bias", [1, NEXP])
    w_eg = din("w_exp_gate", [NEXP, D, 256])
    w_eu = din("w_exp_up", [NEXP, D, 256])
    w_ed = din("w_exp_down", [NEXP, 256, D])
    w_sg = din("w_sh_gate", [D, 256])
    w_su = din("w_sh_up", [D, 256])
    w_sd = din("w_sh_down", [256, D])
    ln3_g = din("ln3_g", [1, D])
    ln3_b = din("ln3_b", [1, D])
    out_d = nc.dram_tensor("out", [S, D], F32, kind="ExternalOutput").ap()
    XG = nc.dram_tensor("XG_scratch", [NSLOT, D], BF16, kind="Internal").ap()
    YG = nc.dram_tensor("YG_scratch", [NSLOT, D], BF16, kind="Internal").ap()
    dbg_d = {}
    if dbg:
        for nm in ("d_x0", "d_x1", "d_x2"):
            dbg_d[nm] = nc.dram_tensor(nm, [S, D], F32, kind="ExternalOutput").ap()

    w_in_v = w_in.rearrange("(c p) n -> p c n", p=128)

    with contextlib.ExitStack() as st:
        K = Sched(nc, st)
        pe, act, dve, pool, sp = K.pe, K.act, K.dve, K.pool, K.sp
        op, dma = K.op, K.dma

        uid = [0]

        def sb(stk, name, shape, dt):
            uid[0] += 1
            return stk.enter_context(nc.sbuf_tensor("%s_%d" % (name, uid[0]), list(shape), dt))

        def ps(stk, name, shape, dt=F32):
            uid[0] += 1
            return stk.enter_context(nc.psum_tensor("%s_%d" % (name, uid[0]), list(shape), dt))

        X = sb(st, "X", [128, NT, D], F32)
        SL8I = sb(st, "SL8I", [128, NT, 8], U32)
        G8v = sb(st, "G8v", [128, NT, 8], F32)
        tSL = toks(NT)
        tZ = toks(NEXP)
        tX = toks(NT)
        tXT = toks(NT)
        identf = sb(st, "identf", [128, 128], F32)
        identb = sb(st, "identb", [128, 128], BF16)
        onesf = sb(st, "onesf", [128, 128], F32)
        BC = sb(st, "BC", [128, 56], F32)
        ONESB = sb(st, "ONESB", [1, 128], BF16)
        NEGH = sb(st, "NEGH", [128, NT], F32)
        tC = Tok()
        stXT = contextlib.ExitStack()
        XT = sb(stXT, "XT", [128, NKC, S], BF16)

        FILL0 = nc.gpsimd.to_reg(0.0)
        FILL1 = nc.gpsimd.to_reg(1.0)
        op(dve, lambda: nc.vector.memset(onesf[:], 1.0), writes=[tC])
        op(dve, lambda: nc.vector.memset(ONESB[:], 1.0), writes=[tC])
        op(dve, lambda: nc.vector.memset(NEGH[:], -0.5), writes=[tC])
        op(pool, lambda: nc.gpsimd.affine_select(out=identf[:], in_=onesf[:], pattern=[[1, 128]],
                                                 compare_op=ALU.is_equal, fill=FILL0, base=0,
                                                 channel_multiplier=-1), reads=[tC], writes=[tC])
        op(pool, lambda: nc.gpsimd.affine_select(out=identb[:], in_=onesf[:], pattern=[[1, 128]],
                                                 compare_op=ALU.is_equal, fill=FILL0, base=0,
                                                 channel_multiplier=-1), reads=[tC], writes=[tC])

        def layer_norm_x(stk, g_d, b_d, PSt, tPSt, want_xt=True, per_tile=None, out_dram=None, dbg_out=None):
            G = sb(stk, "lnG", [128, D], F32)
            Bt = sb(stk, "lnB", [128, D], F32)
            ST = sb(stk, "lnST", [128, NT, 12], F32)
            MV = sb(stk, "lnMV", [128, NT, 2], F32)
            RS = sb(stk, "lnRS", [128, NT], F32)
            tG, tB, tS, tM, tR = Tok(), Tok(), Tok(), Tok(), Tok()
            dma(sp, G[:], g_d.partition_broadcast(128), writes=[tG])
            dma(sp, Bt[:], b_d.partition_broadcast(128), writes=[tB])
            for i in range(NT):
                for hf in range(2):
                    op(dve, lambda i=i, hf=hf: nc.vector.bn_stats(ST[:, i, hf * 6:(hf + 1) * 6],
                                                                  X[:, i, hf * 512:(hf + 1) * 512]),
                       reads=[tX[i]], writes=[tS])
                op(dve, lambda i=i: nc.vector.bn_aggr(MV[:, i, :], ST[:, i, :]), reads=[tS], writes=[tM])
            op(dve, lambda: nc.vector.tensor_scalar(RS[:], MV[:, :, 1], LN_EPS, None, ALU.add),
               reads=[tM], writes=[tR])
            op(pool, lambda: nc.gpsimd.tensor_tensor(RS[:], RS[:], NEGH[:], ALU.pow), reads=[tR, tC], writes=[tR])
            for i in range(NT):
                op(dve, lambda i=i: nc.vector.tensor_scalar(X[:, i, :], X[:, i, :], MV[:, i, 0:1], RS[:, i:i + 1],
                                                            ALU.subtract, ALU.mult),
                   reads=[tX[i], tM, tR], writes=[tX[i]])
                op(dve, lambda i=i: nc.vector.tensor_tensor(X[:, i, :], X[:, i, :], G[:], ALU.mult),
                   reads=[tX[i], tG], writes=[tX[i]])
                op(pool, lambda i=i: nc.gpsimd.tensor_tensor(X[:, i, :], X[:, i, :], Bt[:], ALU.add),
                   reads=[tX[i], tB], writes=[tX[i]])
                if dbg_out is not None:
                    dma(sp, dbg_out[i * 128:(i + 1) * 128, :], X[:, i, :], reads=[tX[i]])
                if out_dram is not None:
                    dma(sp, out_dram[i * 128:(i + 1) * 128, :], X[:, i, :], reads=[tX[i]])
                if want_xt:
                    for hb in range(2):
                        pb = PSt[hb]
                        for c4 in range(4):
                            c = hb * 4 + c4
                            op(pe, lambda i=i, c=c, c4=c4, pb=pb: nc.tensor.transpose(
                                pb[:, c4 * 128:(c4 + 1) * 128], X[:, i, c * 128:(c + 1) * 128], identf[:]),
                               reads=[tX[i], tC], writes=[tPSt[hb]], sig=(c4 == 3))
                        op(act, lambda i=i, hb=hb, pb=pb: nc.scalar.copy(
                            XT[:, hb * 4:(hb + 1) * 4, i * 128:(i + 1) * 128],
                            pb[:].rearrange("p (c t) -> p c t", c=4)),
                           reads=[tPSt[hb]], writes=[tXT[i]])
                        if per_tile is not None:
                            per_tile(i, hb, pb, tPSt[hb])

        def dump_x():
            for i in range(NT):
                dma(sp, out_d[i * 128:(i + 1) * 128, :], X[:, i, :], reads=[tX[i]])
            K.barrier()
            stXT.close()

        with contextlib.ExitStack() as p0:
            PSt = [ps(p0, "p0t%d" % k, [128, 512]) for k in range(2)]
            tPSt = toks(2)
            for i in range(NT):
                dma(sp, X[:, i, :], x_d[i * 128:(i + 1) * 128, :], writes=[tX[i]])
            BI = sb(p0, "BI", [56, 128], F32)
            tBI = Tok()
            dma(sp, BI[:], b_in[:, :], writes=[tBI])
            PSb = ps(p0, "p0b", [128, 512])
            tPSb = Tok()
            op(pe, lambda: nc.tensor.transpose(PSb[:, 0:56], BI[:], identf[0:56, 0:56]),
               reads=[tBI, tC], writes=[tPSb])
            op(act, lambda: nc.scalar.copy(BC[:], PSb[:, 0:56]), reads=[tPSb], writes=[tC])
            layer_norm_x(p0, ln_in_g, ln_in_b, PSt, tPSt, dbg_out=dbg_d.get("d_x0"))
            for i in range(NT):
                op(dve, lambda i=i: nc.vector.tensor_scalar(X[:, i, :], X[:, i, :], ALPHA, None, ALU.mult),
                   reads=[tX[i]], writes=[tX[i]])
            K.barrier()

        def proj_fm(Wblk, tW, PSbanks, tPS, consume):
            for tg in range(4):
                pb = PSbanks[tg % len(PSbanks)]
                tp = tPS[tg % len(PSbanks)]
                for kc in range(NKC):
                    op(pe, lambda kc=kc, tg=tg, pb=pb: nc.tensor.matmul(
                        pb[:], lhsT=Wblk[:, kc, :], rhs=XT[:, kc, tg * 512:(tg + 1) * 512],
                        start=(kc == 0), stop=(kc == NKC - 1)),
                       reads=[tW] + tXT[tg * 4:(tg + 1) * 4], writes=[tp], sig=(kc == NKC - 1))
                consume(tg, pb, tp)

        def out_proj_accum(MT2, tMT2, WO2, tWO2, PSo, tPSo, nu=2):
            for i in range(NT):
                for hf in range(2):
                    k = (i * 2 + hf) % len(PSo)
                    pb, tp = PSo[k], tPSo[k]
                    for u in range(nu):
                        op(pe, lambda i=i, hf=hf, u=u, pb=pb: nc.tensor.matmul(
                            pb[:, 0:512], lhsT=MT2[:, u, i * 128:(i + 1) * 128], rhs=WO2[:, u, hf * 512:(hf + 1) * 512],
                            start=(u == 0), stop=(u == nu - 1)),
                           reads=[tMT2[u], tWO2], writes=[tp], sig=(u == nu - 1))
                    op(dve, lambda i=i, hf=hf, pb=pb: nc.vector.tensor_tensor(
                        X[:, i, hf * 512:(hf + 1) * 512], X[:, i, hf * 512:(hf + 1) * 512], pb[:, 0:512], ALU.add),
                       reads=[tp, tX[i]], writes=[tX[i]])

        if stop == 0:
            dump_x()
            return nc
        with contextlib.ExitStack() as p1:
            VN = sb(p1, "VN", [128, NT, D], BF16)
            tVN = toks(NT)
            PSa = [ps(p1, "p1a%d" % k, [128, 512]) for k in range(4)]
            tPSa = toks(4)
            PSm = [ps(p1, "p1m%d" % k, [128, 512]) for k in range(2)]
            tPSm = toks(2)
            PSo = [ps(p1, "p1o%d" % k, [128, 512]) for k in range(2)]
            tPSo = toks(2)
            with contextlib.ExitStack() as p1a:
                W2 = [sb(p1a, "Wv%d" % k, [128, NKC, 512], BF16) for k in range(2)]
                BR = sb(p1a, "BRv", [1, D], BF16)
                tW2, tBR = toks(2), Tok()
                G = sb(p1a, "vG", [128, D], F32)
                Bt = sb(p1a, "vB", [128, D], F32)
                ST = sb(p1a, "vST", [128, NT, 12], F32)
                MV = sb(p1a, "vMV", [128, NT, 2], F32)
                RS = sb(p1a, "vRS", [128, NT], F32)
                tG, tB = Tok(), Tok()
                tLv = toks(NT)
                dma(sp, G[:], ln_v_g.partition_broadcast(128), writes=[tG])
                dma(sp, Bt[:], ln_v_b.partition_broadcast(128), writes=[tB])
                dma(pool, BR[:], b_in[8:16, :].rearrange("a b -> (a b)").rearrange("(o n) -> o n", o=1), writes=[tBR])
                for hf in range(2):
                    dma(pool, W2[hf][:], w_in_v[:, :, 1024 + hf * 512:1024 + (hf + 1) * 512], writes=[tW2[hf]])
                for hf in range(2):
                    W, tW = W2[hf], tW2[hf]
                    for i in range(NT):
                        k = i % 4
                        pb, tp = PSa[k], tPSa[k]
                        for kc in range(NKC):
                            op(pe, lambda kc=kc, i=i, pb=pb, W=W: nc.tensor.matmul(
                                pb[:], lhsT=XT[:, kc, i * 128:(i + 1) * 128], rhs=W[:, kc, :],
                                start=(kc == 0), stop=False),
                               reads=[tW, tXT[i]], writes=[tp], sig=False)
                        op(pe, lambda hf=hf, pb=pb: nc.tensor.matmul(
                            pb[:], lhsT=ONESB[0:1, :], rhs=BR[0:1, hf * 512:(hf + 1) * 512], start=False, stop=True),
                           reads=[tBR, tC], writes=[tp])
                        op(act, lambda i=i, hf=hf, pb=pb: nc.scalar.activation(
                            VN[:, i, hf * 512:(hf + 1) * 512], pb[:], AF.Gelu),
                           reads=[tp], writes=[tVN[i]])
                        if hf == 1:
                            tl = tLv[i]
                            for h2 in range(2):
                                op(dve, lambda i=i, h2=h2: nc.vector.bn_stats(ST[:, i, h2 * 6:(h2 + 1) * 6],
                                                                              VN[:, i, h2 * 512:(h2 + 1) * 512]),
                                   reads=[tVN[i]], writes=[tl])
                            op(dve, lambda i=i: nc.vector.bn_aggr(MV[:, i, :], ST[:, i, :]), reads=[tl], writes=[tl])
                            op(dve, lambda i=i: nc.vector.tensor_scalar(RS[:, i:i + 1], MV[:, i, 1:2], LN_EPS, None,
                                                                        ALU.add),
                               reads=[tl], writes=[tl])
                            op(pool, lambda i=i: nc.gpsimd.tensor_tensor(RS[:, i:i + 1], RS[:, i:i + 1], NEGH[:, 0:1],
                                                                         ALU.pow),
                               reads=[tl, tC], writes=[tl])
                            op(dve, lambda i=i: nc.vector.tensor_scalar(VN[:, i, :], VN[:, i, :], MV[:, i, 0:1],
                                                                        RS[:, i:i + 1], ALU.subtract, ALU.mult),
                               reads=[tVN[i], tl], writes=[tVN[i]])
                            op(dve, lambda i=i: nc.vector.tensor_tensor(VN[:, i, :], VN[:, i, :], G[:], ALU.mult),
                               reads=[tVN[i], tG], writes=[tVN[i]])
                            op(pool, lambda i=i: nc.gpsimd.tensor_tensor(VN[:, i, :], VN[:, i, :], Bt[:], ALU.add),
                               reads=[tVN[i], tB], writes=[tVN[i]])
                K.barrier()

            with contextlib.ExitStack() as p1b:
                WT = sb(p1b, "WT", [128, 8, 128], BF16)
                WS = sb(p1b, "WS", [128, 8, 128], F32)
                BS = sb(p1b, "BS", [128, 8, 128], F32)
                tWT, tWS, tBS = Tok(), Tok(), Tok()
                dma(sp, WS[:], w_sp.rearrange("g t s -> t g s"), writes=[tWS])
                dma(sp, BS[:].rearrange("p g t -> p (g t)"),
                    b_sp.rearrange("g t -> (g t)").rearrange("(o n) -> o n", o=1).partition_broadcast(128),
                    writes=[tBS])
                ZT = sb(p1b, "ZT", [128, CAP // 128, D], BF16)
                tZT = Tok()
                op(pool, lambda: nc.gpsimd.memset(ZT[:], 0.0), writes=[tZT])
                for g in range(8):
                    op(pool, lambda g=g: nc.gpsimd.affine_select(out=WS[:, g, :], in_=WS[:, g, :], pattern=[[-1, 128]],
                                                                 compare_op=ALU.is_ge, fill=FILL0, base=0,
                                                                 channel_multiplier=1),
                       reads=[tWS], writes=[tWS])
                for g4 in range(2):
                    pb, tp = PSm[g4], tPSm[g4]
                    for gg in range(4):
                        g = g4 * 4 + gg
                        op(pe, lambda g=g, gg=gg, pb=pb: nc.tensor.transpose(
                            pb[:, gg * 128:(gg + 1) * 128], WS[:, g, :], identf[:]),
                           reads=[tWS, tC], writes=[tp], sig=(gg == 3))
                    op(act, lambda g4=g4, pb=pb: nc.scalar.copy(
                        WT[:, g4 * 4:(g4 + 1) * 4, :], pb[:].rearrange("p (g t) -> p g t", g=4)),
                       reads=[tp], writes=[tWT])

                WB = [sb(p1b, "WB%d" % k, [128, 2, NKC, 128], BF16) for k in range(2)]
                tWB = toks(2)
                WO2 = sb(p1b, "WO4", [128, 4, D], BF16)
                tWO2 = Tok()
                U2 = [sb(p1b, "U%d" % k, [128, S], BF16) for k in range(2)]
                GA2 = [sb(p1b, "GA%d" % k, [128, S], BF16) for k in range(2)]
                T1 = [sb(p1b, "T1%d" % k, [128, 512], BF16) for k in range(2)]
                tU2, tGA2, tT1 = [toks(4), toks(4)], [toks(4), toks(4)], toks(2)
                MT2 = sb(p1b, "MT4", [128, 4, S], BF16)
                tMT2 = toks(4)
                def load_group_w(g):
                    par = g % 2
                    dma(pool, WB[par][:, 0, :, :], w_in_v[:, :, g * 128:(g + 1) * 128], writes=[tWB[par]])
                    dma(pool, WB[par][:, 1, :, :], w_in_v[:, :, 5120 + g * 128:5120 + (g + 1) * 128],
                        writes=[tWB[par]])

                load_group_w(0)
                for g in range(8):
                    par = g % 2
                    g4_ = g % 4
                    U, GA, tU, tGA = U2[par], GA2[par], tU2[par], tGA2[par]
                    if g + 1 < 8:
                        load_group_w(g + 1)
                    if g4_ == 0:
                        dma(pool, WO2[:], w_out[g * 128:(g + 4) * 128, :].rearrange("(u p) n -> p u n", p=128),
                            writes=[tWO2])

                    def cons_u(tg, pb, tp, g=g, U=U, tU=tU):
                        op(act, lambda: nc.scalar.activation(U[:, tg * 512:(tg + 1) * 512], pb[:], AF.Gelu,
                                                             bias=BC[:, g:g + 1], scale=1.0),
                           reads=[tp, tC], writes=[tU[tg]])
                    proj_fm(WB[par][:, 0, :, :], tWB[par], PSa, tPSa, cons_u)
                    for e in range(g * 8, (g + 1) * 8):
                        dma(sp, XG[e * CAP:(e + 1) * CAP, :].rearrange("(j p) n -> p j n", p=128), ZT[:],
                            reads=[tZT, tU[3]], writes=[tZ[e]])

                    def cons_ga(tg, pb, tp, g=g, GA=GA, tGA=tGA):
                        op(act, lambda: nc.scalar.activation(GA[:, tg * 512:(tg + 1) * 512], pb[:], AF.Sigmoid,
                                                             bias=BC[:, 40 + g:41 + g], scale=1.0),
                           reads=[tp, tC], writes=[tGA[tg]])
                    proj_fm(WB[par][:, 1, :, :], tWB[par], PSa, tPSa, cons_ga)

                    for tg in range(4):
                        pb, tp = PSm[tg % 2], tPSm[tg % 2]
                        for c4 in range(4):
                            c = tg * 4 + c4
                            op(pe, lambda c=c, c4=c4, pb=pb, g=g: nc.tensor.matmul(
                                pb[:, c4 * 128:(c4 + 1) * 128], lhsT=VN[:, c, g * 128:(g + 1) * 128],
                                rhs=WT[:, g, :], start=True, stop=True),
                               reads=[tVN[c], tWT], writes=[tp], sig=(c4 == 3))
                        t1 = T1[tg % 2]
                        for c4 in range(4):
                            op(dve, lambda c4=c4, pb=pb, t1=t1, g=g: nc.vector.tensor_tensor(
                                t1[:, c4 * 128:(c4 + 1) * 128], pb[:, c4 * 128:(c4 + 1) * 128], BS[:, g, :], ALU.add),
                               reads=[tp, tBS], writes=[tT1[tg % 2]])
                        op(dve, lambda tg=tg, t1=t1, U=U: nc.vector.tensor_tensor(
                            t1[:], t1[:], U[:, tg * 512:(tg + 1) * 512], ALU.mult),
                           reads=[tT1[tg % 2], tU[tg]], writes=[tT1[tg % 2]])
                        op(pool, lambda tg=tg, t1=t1, g4_=g4_, GA=GA: nc.gpsimd.tensor_tensor(
                            MT2[:, g4_, tg * 512:(tg + 1) * 512], t1[:], GA[:, tg * 512:(tg + 1) * 512], ALU.mult),
                           reads=[tT1[tg % 2], tGA[tg]], writes=[tMT2[g4_]])
                    if g4_ == 3:
                        out_proj_accum(MT2, tMT2, WO2, tWO2, PSo, tPSo, nu=4)
                K.barrier()

        if stop == 1:
            dump_x()
            return nc
        with contextlib.ExitStack() as p1c:
            PSz = [ps(p1c, "pz%d" % k, [128, 512]) for k in range(4)]
            tPSz = toks(4)
            zbase = {}
            PSTr = [ps(p1c, "ptr%d" % k, [128, 1024], BF16) for k in range(2)]
            tPSTr = toks(2)
            PSy = ps(p1c, "py", [128, 512])
            tPSy = Tok()
            PSp = [ps(p1c, "pp", [128, 512])]
            tPSp = toks(1)
            WH = [sb(p1c, "WH%d" % k, [128, 4, NKC, 128], BF16) for k in range(2)]
            tWH = toks(2)
            WO2 = sb(p1c, "WO2c", [128, 2, D], BF16)
            tWO2 = Tok()
            BRV = [sb(p1c, "BRV%d" % k, [1, 128], BF16) for k in range(2)]
            tBRV = toks(2)
            QT = [sb(p1c, "QT%d" % k, [128, S], BF16) for k in range(2)]
            KT = [sb(p1c, "KT%d" % k, [128, S], BF16) for k in range(2)]
            GB = [sb(p1c, "GB%d" % k, [128, S], BF16) for k in range(2)]
            VH = [sb(p1c, "VH%d" % k, [128, NT, 128], BF16) for k in range(2)]
            tQT, tKT, tGB, tVH = [toks(4), toks(4)], [toks(4), toks(4)], [toks(4), toks(4)], [toks(4), toks(4)]
            Rb = [sb(p1c, "Rb%d" % k, [128, S], F32) for k in range(2)]
            Bb = [sb(p1c, "Bb%d" % k, [128, S], BF16) for k in range(4)]
            Pb = [sb(p1c, "Pb%d" % k, [128, S + 2], BF16) for k in range(2)]
            ATb = [sb(p1c, "ATb%d" % k, [128, S], BF16) for k in range(2)]
            tRb, tBb, tATb, tPb = toks(2), toks(4), toks(2), toks(2)
            MT2 = sb(p1c, "MT2c", [128, 2, S], BF16)
            tMT2 = toks(2)

            def load_head_w(h, part=None):
                par = h % 2
                for j, off in enumerate((2048, 3072, 4096, 6144)):
                    if part is None or part == j:
                        dma(pool, WH[par][:, j, :, :], w_in_v[:, :, off + h * 128:off + (h + 1) * 128],
                            writes=[tWH[par]])
                if part is None or part == 4:
                    dma(pool, BRV[par][:], b_in[32 + h:33 + h, :], writes=[tBRV[par]])

            def emit_proj(h):
                par = h % 2
                specs = ((0, QT, tQT, AF.Identity, 16), (1, KT, tKT, AF.Identity, 24), (3, GB, tGB, AF.Sigmoid, 48))
                for (j, DST, tDST, fn_, bcol) in specs:
                    for tg in range(4):
                        pb, tp = PSp[0], tPSp[0]
                        for kc in range(NKC):
                            op(pe, lambda kc=kc, tg=tg, pb=pb, j=j: nc.tensor.matmul(
                                pb[:], lhsT=WH[par][:, j, kc, :], rhs=XT[:, kc, tg * 512:(tg + 1) * 512],
                                start=(kc == 0), stop=(kc == NKC - 1)),
                               reads=[tWH[par]] + tXT[tg * 4:(tg + 1) * 4], writes=[tp], sig=(kc == NKC - 1))
                        op(act, lambda tg=tg, pb=pb, DST=DST, fn_=fn_, bcol=bcol: nc.scalar.activation(
                            DST[par][:, tg * 512:(tg + 1) * 512], pb[:], fn_,
                            bias=BC[:, bcol + h:bcol + h + 1], scale=1.0),
                           reads=[tp, tC], writes=[tDST[par][tg]])
                        yield
                for tg in range(4):
                    pb, tp = PSp[0], tPSp[0]
                    for c4 in range(4):
                        i = tg * 4 + c4
                        for kc in range(NKC):
                            op(pe, lambda kc=kc, i=i, c4=c4, pb=pb: nc.tensor.matmul(
                                pb[:, c4 * 128:(c4 + 1) * 128], lhsT=XT[:, kc, i * 128:(i + 1) * 128],
                                rhs=WH[par][:, 2, kc, :], start=(kc == 0), stop=False),
                               reads=[tWH[par], tXT[i]], writes=[tp], sig=False)
                        op(pe, lambda c4=c4, pb=pb: nc.tensor.matmul(
                            pb[:, c4 * 128:(c4 + 1) * 128], lhsT=ONESB[0:1, :],
                            rhs=BRV[par][0:1, :], start=False, stop=True),
                           reads=[tBRV[par], tC], writes=[tp], sig=(c4 == 3))
                    op(act, lambda tg=tg, pb=pb: nc.scalar.copy(
                        VH[par][:, tg * 4:(tg + 1) * 4, :], pb[:].rearrange("p (c d) -> p c d", c=4)),
                       reads=[tp], writes=[tVH[par][tg]])
                    yield

            def stage_a1_pe(h, i, s_):
                par = h % 2
                nk = 128 * (i + 1)
                nch = (nk + 511) // 512
                base = zbase.get(s_ - 1, (0, 0))
                base = (base[0] + base[1]) % 4
                zbase[s_] = (base, nch)
                for ch in range(nch):
                    k0 = ch * 512
                    w_ = min(512, nk - k0)
                    zi = (base + ch) % 4
                    op(pe, lambda zi=zi, w_=w_, k0=k0: nc.tensor.matmul(
                        PSz[zi][:, 0:w_], lhsT=QT[par][:, i * 128:(i + 1) * 128],
                        rhs=KT[par][:, k0:k0 + w_], start=True, stop=True),
                       reads=[tQT[par][i // 4], tKT[par][ch]], writes=[tPSz[zi]])

            def stage_a1_act(h, i, s_):
                bp, bq = s_ % 2, s_ % 4
                nk = 128 * (i + 1)
                base, nch = zbase[s_]
                for ch in range(nch):
                    k0 = ch * 512
                    w_ = min(512, nk - k0)
                    zi = (base + ch) % 4
                    op(act, lambda zi=zi, k0=k0, w_=w_: nc.scalar.activation(
                        Rb[bp][:, k0:k0 + w_], PSz[zi][:, 0:w_], AF.Sigmoid, scale=-SB_SCALE),
                       reads=[tPSz[zi]], writes=[tRb[bp]])
                    op(act, lambda zi=zi, k0=k0, w_=w_: nc.scalar.activation(
                        Bb[bq][:, k0:k0 + w_], PSz[zi][:, 0:w_], AF.Sigmoid, scale=SB_SCALE),
                       reads=[tPSz[zi]], writes=[tBb[bq]])

            def stage_a2a(h, i, s_):
                bp, bq = s_ % 2, s_ % 4
                nk = 128 * (i + 1)
                d0 = i * 128
                op(pool, lambda: nc.gpsimd.affine_select(
                    out=Rb[bp][:, d0:d0 + 128], in_=Rb[bp][:, d0:d0 + 128], pattern=[[-1, 128]],
                    compare_op=ALU.is_gt, fill=FILL1, base=0, channel_multiplier=1),
                   reads=[tRb[bp]], writes=[tRb[bp]])
                op(pool, lambda: nc.gpsimd.affine_select(
                    out=Bb[bq][:, d0:d0 + 128], in_=Bb[bq][:, d0:d0 + 128], pattern=[[-1, 128]],
                    compare_op=ALU.is_gt, fill=FILL0, base=0, channel_multiplier=1),
                   reads=[tBb[bq]], writes=[tBb[bq]])
                op(pool, lambda: nc.gpsimd.memset(Pb[bp][:, nk + 1:nk + 2], 1.0), writes=[tPb[bp]])
                op(dve, lambda: nc.vector.tensor_tensor_scan(
                    out=Pb[bp][:, 1:nk + 1][:, ::-1], data0=Rb[bp][:, 0:nk][:, ::-1], data1=Rb[bp][:, 0:nk][:, ::-1],
                    initial=1.0, op0=ALU.mult, op1=ALU.min),
                   reads=[tRb[bp]], writes=[tPb[bp]])

            def stage_a2b(h, i, s_):
                bp, bq = s_ % 2, s_ % 4
                nk = 128 * (i + 1)
                op(dve, lambda: nc.vector.tensor_tensor(
                    Bb[bq][:, 0:nk], Bb[bq][:, 0:nk], Pb[bp][:, 2:nk + 2], ALU.mult),
                   reads=[tBb[bq], tPb[bp]], writes=[tBb[bq]])

            def stage_b(h, i, s_):
                par = h % 2
                bp, bq = s_ % 2, s_ % 4
                nb = i + 1
                for bk in range((nb + 7) // 8):
                    b0 = bk * 8
                    nbb = min(8, nb - b0)
                    pt, tpt = PSTr[bk % 2], tPSTr[bk % 2]
                    for b_ in range(nbb):
                        op(pe, lambda b_=b_, b0=b0, pt=pt: nc.tensor.transpose(
                            pt[:, b_ * 128:(b_ + 1) * 128], Bb[bq][:, (b0 + b_) * 128:(b0 + b_ + 1) * 128],
                            identb[:]),
                           reads=[tBb[bq], tC], writes=[tpt], sig=(b_ == nbb - 1))
                    op(act, lambda b0=b0, nbb=nbb, pt=pt: nc.scalar.copy(
                        ATb[bp][:, b0 * 128:(b0 + nbb) * 128], pt[:, 0:nbb * 128]),
                       reads=[tpt], writes=[tATb[bp]])

            def stage_b2(h, i, s_):
                par = h % 2
                bp, bq = s_ % 2, s_ % 4
                nb = i + 1
                c4 = i % 4
                for b_ in range(nb):
                    op(pe, lambda b_=b_: nc.tensor.matmul(
                        PSy[:, c4 * 128:(c4 + 1) * 128], lhsT=VH[par][:, b_, :],
                        rhs=ATb[bp][:, b_ * 128:(b_ + 1) * 128], start=(b_ == 0), stop=(b_ == nb - 1)),
                       reads=[tVH[par][b_ // 4], tATb[bp]], writes=[tPSy], sig=(b_ == nb - 1))
                if c4 == 3:
                    tg = i // 4
                    op(dve, lambda: nc.vector.tensor_tensor(
                        MT2[:, par, tg * 512:(tg + 1) * 512], PSy[:], GB[par][:, tg * 512:(tg + 1) * 512], ALU.mult),
                       reads=[tPSy, tGB[par][tg]], writes=[tMT2[par]])
                if par == 1 and i == NT - 1:
                    out_proj_accum(MT2, tMT2, WO2, tWO2, [PSp[0], PSy], [tPSp[0], tPSy])
                    if h + 1 < 8:
                        dma(pool, WO2[:], w_out[(h + 1) * 128:(h + 3) * 128, :].rearrange("(u p) n -> p u n", p=128),
                            writes=[tWO2])

            tiles = [(h, i) for h in range(8) for i in range(NT)]
            dma(pool, WO2[:], w_out[0:256, :].rearrange("(u p) n -> p u n", p=128), writes=[tWO2])
            load_head_w(0)
            load_head_w(1)
            for _ in emit_proj(0):
                pass
            NTL = len(tiles)
            stage_a1_pe(*tiles[0], 0)
            gen = None
            for s_ in range(NTL + 4):
                if s_ < NTL:
                    h, i = tiles[s_]
                    if 9 <= i <= 13 and h + 2 < 8:
                        load_head_w(h + 2, part=i - 9)
                    if i == 4 and h + 1 < 8:
                        gen = emit_proj(h + 1)
                        gen_n = 0
                    stage_a1_act(h, i, s_)
                if s_ + 1 < NTL:
                    stage_a1_pe(*tiles[s_ + 1], s_ + 1)
                if 0 <= s_ - 1 < NTL:
                    stage_a2a(*tiles[s_ - 1], s_ - 1)
                if 0 <= s_ - 2 < NTL:
                    stage_a2b(*tiles[s_ - 2], s_ - 2)
                if 0 <= s_ - 3 < NTL:
                    stage_b(*tiles[s_ - 3], s_ - 3)
                if 0 <= s_ - 4 < NTL:
                    stage_b2(*tiles[s_ - 4], s_ - 4)
                if gen is not None:
                    for _ in range(2 if gen_n < 12 else 1):
                        try:
                            next(gen)
                            gen_n += 1
                        except StopIteration:
                            gen = None
                            break
            K.barrier()

        if stop == 2:
            dump_x()
            return nc
        pmw = contextlib.ExitStack()
        WKV = sb(pmw, "WKV", [128, NKC, 1024], BF16)
        WQ = sb(pmw, "WQ", [128, NKC, 512], BF16)
        WO = sb(pmw, "WOm", [128, 4, D], BF16)
        MS = sb(pmw, "MS", [128, 2, D], F32)
        tWKV, tWQ, tWO, tMS = Tok(), Tok(), Tok(), Tok()
        dma(pool, WKV[:], w_mkv.rearrange("(c p) n -> p c n", p=128), writes=[tWKV])
        dma(pool, WQ[:], w_mq.rearrange("(c p) n -> p c n", p=128), writes=[tWQ])
        dma(pool, WO[:], w_mo.rearrange("(c p) n -> p c n", p=128), writes=[tWO])
        dma(sp, MS[:], mem_d.rearrange("(m p) n -> p m n", p=128), writes=[tMS])
        with contextlib.ExitStack() as pl1:
            PSt = [ps(pl1, "l1t%d" % k, [128, 512]) for k in range(2)]
            tPSt = toks(2)
            layer_norm_x(pl1, ln1_g, ln1_b, PSt, tPSt, dbg_out=dbg_d.get("d_x1"))
            for i in range(NT):
                op(dve, lambda i=i: nc.vector.tensor_scalar(X[:, i, :], X[:, i, :], ALPHA, None, ALU.mult),
                   reads=[tX[i]], writes=[tX[i]])
            K.barrier()

        if stop == 3:
            pmw.close()
            dump_x()
            return nc
        with contextlib.ExitStack() as p2:
            PSAf = ps(p2, "p2a", [128, 1024])
            PSA = [PSAf[:, 0:512], PSAf[:, 512:1024]]
            tPSA = toks(2)
            PSL = ps(p2, "p2l", [128, 1024])
            tPSL = Tok()
            PSTr = ps(p2, "p2tr", [128, 1024], BF16)
            tPSTr = Tok()
            PSO = ps(p2, "p2o", [128, 512])
            tPSO = Tok()
            PSM = [ps(p2, "p2m%d" % k, [128, 512]) for k in range(2)]
            tPSM = toks(2)
            MTm = sb(p2, "MTm", [128, NKC, 256], BF16)
            tMTm = Tok()
            for mt in range(2):
                for hb in range(2):
                    pb, tp = PSA[hb], tPSA[hb]
                    for c4 in range(4):
                        c = hb * 4 + c4
                        op(pe, lambda mt=mt, c=c, c4=c4, pb=pb: nc.tensor.transpose(
                            pb[:, c4 * 128:(c4 + 1) * 128], MS[:, mt, c * 128:(c + 1) * 128], identf[:]),
                           reads=[tMS, tC], writes=[tp], sig=(c4 == 3))
                    op(act, lambda mt=mt, hb=hb, pb=pb: nc.scalar.copy(
                        MTm[:, hb * 4:(hb + 1) * 4, mt * 128:(mt + 1) * 128],
                        pb[:].rearrange("p (c t) -> p c t", c=4)),
                       reads=[tp], writes=[tMTm])
            KM = sb(p2, "KM", [128, 4, 256], BF16)
            VM = sb(p2, "VM", [128, 2, 512], BF16)
            QM = sb(p2, "QM", [128, 4, S], BF16)
            tKM, tVM, tQM = Tok(), Tok(), toks(4)
            for h in range(4):
                pb, tp = PSA[h % 2], tPSA[h % 2]
                for kc in range(NKC):
                    op(pe, lambda h=h, kc=kc, pb=pb: nc.tensor.matmul(
                        pb[:, 0:256], lhsT=WKV[:, kc, h * 128:(h + 1) * 128], rhs=MTm[:, kc, :],
                        start=(kc == 0), stop=(kc == NKC - 1)),
                       reads=[tWKV, tMTm], writes=[tp], sig=(kc == NKC - 1))
                op(act, lambda h=h, pb=pb: nc.scalar.copy(KM[:, h, :], pb[:, 0:256]), reads=[tp], writes=[tKM])
            for mt in range(2):
                pb, tp = PSA[mt % 2], tPSA[mt % 2]
                for kc in range(NKC):
                    op(pe, lambda mt=mt, kc=kc, pb=pb: nc.tensor.matmul(
                        pb[:], lhsT=MTm[:, kc, mt * 128:(mt + 1) * 128], rhs=WKV[:, kc, 512:1024],
                        start=(kc == 0), stop=(kc == NKC - 1)),
                       reads=[tWKV, tMTm], writes=[tp], sig=(kc == NKC - 1))
                op(act, lambda mt=mt, pb=pb: nc.scalar.copy(VM[:, mt, :], pb[:]), reads=[tp], writes=[tVM])
            for h in range(4):
                for tg in range(4):
                    pb, tp = PSA[tg % 2], tPSA[tg % 2]
                    for kc in range(NKC):
                        op(pe, lambda h=h, tg=tg, kc=kc, pb=pb: nc.tensor.matmul(
                            pb[:], lhsT=WQ[:, kc, h * 128:(h + 1) * 128], rhs=XT[:, kc, tg * 512:(tg + 1) * 512],
                            start=(kc == 0), stop=(kc == NKC - 1)),
                           reads=[tWQ] + tXT[tg * 4:(tg + 1) * 4], writes=[tp], sig=(kc == NKC - 1))
                    op(act, lambda h=h, tg=tg, pb=pb: nc.scalar.copy(QM[:, h, tg * 512:(tg + 1) * 512], pb[:]),
                       reads=[tp], writes=[tQM[tg]])
            MX = [sb(p2, "MX%d" % k, [128, 4], F32) for k in range(2)]
            NMX = [sb(p2, "NMX%d" % k, [128, 4], F32) for k in range(2)]
            SS = [sb(p2, "SS%d" % k, [128, 4], F32) for k in range(2)]
            RSS = [sb(p2, "RSS%d" % k, [128, 4], F32) for k in range(2)]
            Pf = [sb(p2, "Pf%d" % k, [128, 4, 256], F32) for k in range(2)]
            Pn = [sb(p2, "Pn%d" % k, [128, 4, 256], BF16) for k in range(2)]
            PTm = [sb(p2, "PTm%d" % k, [128, 8, 128], BF16) for k in range(2)]
            OTm = [sb(p2, "OTm%d" % k, [128, 4, 128], BF16) for k in range(2)]
            tMX, tNMX, tSS, tRSS, tPf, tPn, tPTm, tOTm = (toks(2) for _ in range(8))
            PSL2 = [PSL, PSAf]
            tPSL2 = [tPSL, Tok()]

            def m_s1(i):
                q = i % 2
                psl, tpsl = PSL2[q], tPSL2[q]
                for h in range(4):
                    op(pe, lambda h=h: nc.tensor.matmul(
                        psl[:, h * 256:(h + 1) * 256], lhsT=QM[:, h, i * 128:(i + 1) * 128], rhs=KM[:, h, :],
                        start=True, stop=True),
                       reads=[tQM[i // 4], tKM], writes=[tpsl], sig=(h == 3))
                op(dve, lambda: nc.vector.tensor_reduce(MX[q][:], psl[:].rearrange("p (h m) -> p h m", h=4),
                                                        AX.X, ALU.max),
                   reads=[tpsl], writes=[tMX[q]])
                op(dve, lambda: nc.vector.tensor_scalar(NMX[q][:], MX[q][:], -MEM_SCALE, None, ALU.mult),
                   reads=[tMX[q]], writes=[tNMX[q]])
                for h in range(4):
                    op(act, lambda h=h: nc.scalar.activation(
                        Pf[q][:, h, :], psl[:, h * 256:(h + 1) * 256], AF.Exp, bias=NMX[q][:, h:h + 1],
                        scale=MEM_SCALE, accum_out=SS[q][:, h:h + 1]),
                       reads=[tpsl, tNMX[q]], writes=[tPf[q], tSS[q]])
                op(dve, lambda: nc.vector.reciprocal(RSS[q][:], SS[q][:]), reads=[tSS[q]], writes=[tRSS[q]])
                for h in range(4):
                    op(dve, lambda h=h: nc.vector.tensor_scalar(Pn[q][:, h, :], Pf[q][:, h, :], RSS[q][:, h:h + 1],
                                                                None, ALU.mult),
                       reads=[tPf[q], tRSS[q]], writes=[tPn[q]])

            def m_s2a(i):
                q = i % 2
                for h in range(4):
                    for mt in range(2):
                        j = h * 2 + mt
                        op(pe, lambda h=h, mt=mt, j=j: nc.tensor.transpose(
                            PSTr[:, j * 128:(j + 1) * 128], Pn[q][:, h, mt * 128:(mt + 1) * 128], identb[:]),
                           reads=[tPn[q], tC], writes=[tPSTr], sig=(j == 7))
                op(act, lambda: nc.scalar.copy(PTm[q][:], PSTr[:].rearrange("p (j t) -> p j t", j=8)),
                   reads=[tPSTr], writes=[tPTm[q]])

            def m_s2b(i):
                q = i % 2
                for h in range(4):
                    for mt in range(2):
                        op(pe, lambda h=h, mt=mt: nc.tensor.matmul(
                            PSO[:, h * 128:(h + 1) * 128], lhsT=VM[:, mt, h * 128:(h + 1) * 128],
                            rhs=PTm[q][:, h * 2 + mt, :], start=(mt == 0), stop=(mt == 1)),
                           reads=[tVM, tPTm[q]], writes=[tPSO], sig=(h == 3 and mt == 1))
                op(act, lambda: nc.scalar.copy(OTm[q][:], PSO[:].rearrange("p (h t) -> p h t", h=4)),
                   reads=[tPSO], writes=[tOTm[q]])

            def m_s3(i):
                q = i % 2
                for hf in range(2):
                    pb, tp = PSM[hf], tPSM[hf]
                    for h in range(4):
                        op(pe, lambda h=h, hf=hf, pb=pb: nc.tensor.matmul(
                            pb[:], lhsT=OTm[q][:, h, :], rhs=WO[:, h, hf * 512:(hf + 1) * 512],
                            start=(h == 0), stop=(h == 3)),
                           reads=[tOTm[q], tWO], writes=[tp], sig=(h == 3))
                    op(dve, lambda hf=hf, pb=pb: nc.vector.tensor_tensor(
                        X[:, i, hf * 512:(hf + 1) * 512], X[:, i, hf * 512:(hf + 1) * 512], pb[:], ALU.add),
                       reads=[tp, tX[i]], writes=[tX[i]])

            K.barrier()
            for s_ in range(NT + 3):
                if s_ < NT:
                    m_s1(s_)
                if 0 <= s_ - 1 < NT:
                    m_s2a(s_ - 1)
                if 0 <= s_ - 2 < NT:
                    m_s2b(s_ - 2)
                if 0 <= s_ - 3 < NT:
                    m_s3(s_ - 3)
            K.barrier()

        pmw.close()
        if stop == 4:
            dump_x()
            return nc
        with contextlib.ExitStack() as p3:
            pxb = contextlib.ExitStack()
            XB = sb(pxb, "XB", [128, NT, D], BF16)
            with contextlib.ExitStack() as p3a:
                PSt = [ps(p3a, "l2t%d" % k, [128, 512]) for k in range(2)]
                tPSt = toks(2)
                PSr = ps(p3a, "l2r", [128, 512])
                tPSr = Tok()
                PSpos = ps(p3a, "l2p", [128, 512])
                tPSpos = Tok()
                WR = sb(p3a, "WR", [128, NKC, NEXP], F32)
                RB = sb(p3a, "RB", [128, NEXP], F32)
                tWR, tRB = Tok(), Tok()
                dma(sp, WR[:], w_r.rearrange("(c p) n -> p c n", p=128), writes=[tWR])
                dma(sp, RB[:], r_b.partition_broadcast(128), writes=[tRB])
                tXB = toks(NT)
                MKB = sb(p3a, "MKB", [128, NT, NEXP], BF16)
                tMKB = toks(NT)
                LT = sb(p3a, "LT", [128, 128], BF16)
                ONESM = sb(p3a, "ONESM", [128, 128], BF16)
                EOFF = sb(p3a, "EOFF", [128, NEXP], F32)
                EOFFI = sb(p3a, "EOFFI", [128, NEXP], mybir.dt.int32)
                tK = Tok()
                op(pool, lambda: nc.gpsimd.affine_select(out=LT[:], in_=onesf[:], pattern=[[1, 128]],
                                                         compare_op=ALU.is_gt, fill=FILL0, base=0,
                                                         channel_multiplier=-1), reads=[tC], writes=[tK])
                op(dve, lambda: nc.vector.memset(ONESM[:], 1.0), writes=[tK])
                op(pool, lambda: nc.gpsimd.iota(EOFFI[:], pattern=[[CAP, NEXP]], base=0, channel_multiplier=0),
                   writes=[tK])
                op(dve, lambda: nc.vector.tensor_copy(EOFF[:], EOFFI[:]), reads=[tK], writes=[tK])
                SCA = sb(p3a, "SCA", [128, NT, NEXP], F32)
                tSCA = toks(NT)
                NR = 4
                PSposL = [PSpos] + [ps(p3a, "l2p%d" % k, [128, 512]) for k in range(NR - 1)]
                tPSposL = [tPSpos] + toks(NR - 1)

                def mkset(r):
                    d = {}
                    for nm, shp in (("SEL", [128, NEXP]), ("SELM", [128, NEXP]), ("T8", [128, 8, 8]), ("GS", [128, 8]),
                                    ("G8", [128, 8]), ("GM", [128, 8]), ("E8", [128, 8]), ("MK", [128, NEXP]),
                                    ("WGt", [128, NEXP]), ("SM", [128, 1]), ("GT_", [128, NEXP]),
                                    ("NMK", [128, NEXP]), ("SLOT", [128, NEXP]), ("N8", [128, 8]),
                                    ("SL8f", [128, 8]), ("JK", [128, NEXP])):
                        d[nm] = sb(p3a, "%s_%d" % (nm, r), shp, F32)
                    d["t"] = Tok()
                    return d
                RS_ = [mkset(r) for r in range(NR)]
                tXF = Tok()
                WRH = sb(p3a, "WRH", [128, NKC, NEXP], BF16)
                WRL = sb(p3a, "WRL", [128, NKC, NEXP], BF16)
                XL = sb(p3a, "XL", [128, NKC, 128], BF16)
                tWRH = Tok()
                op(dve, lambda: nc.vector.tensor_copy(WRH[:], WR[:]), reads=[tWR], writes=[tWRH])
                op(dve, lambda: nc.vector.tensor_tensor(WRL[:], WR[:], WRH[:], ALU.subtract),
                   reads=[tWR, tWRH], writes=[tWRH])

                def router(i, hb, pb, tp):
                    op(dve, lambda: nc.vector.tensor_tensor(
                        XL[:, hb * 4:(hb + 1) * 4, :], pb[:].rearrange("p (c t) -> p c t", c=4),
                        XT[:, hb * 4:(hb + 1) * 4, i * 128:(i + 1) * 128], ALU.subtract),
                       reads=[tp, tXT[i]], writes=[tXF])
                    if hb == 0:
                        op(act, lambda: nc.scalar.copy(XB[:, i, :], X[:, i, :]), reads=[tX[i]], writes=[tXB[i]])
                        return
                    n = 0
                    for (a_hi, wt) in ((True, WRH), (False, WRH), (True, WRL)):
                        for kc in range(NKC):
                            lhs = XT[:, kc, i * 128:(i + 1) * 128] if a_hi else XL[:, kc, :]
                            op(pe, lambda lhs=lhs, wt=wt, kc=kc, n=n: nc.tensor.matmul(
                                PSr[:, 0:NEXP], lhsT=lhs, rhs=wt[:, kc, :], start=(n == 0), stop=(n == 23)),
                               reads=[tXF, tXT[i], tWRH], writes=[tPSr], sig=(n == 23))
                            n += 1
                    op(act, lambda: nc.scalar.activation(SCA[:, i, :], PSr[:, 0:NEXP], AF.Sigmoid),
                       reads=[tPSr], writes=[tSCA[i]])

                def route_chain(i, r):
                    d = RS_[r]
                    SEL, SELM, T8, GS, G8, GM, E8, MK = (d[k] for k in ("SEL", "SELM", "T8", "GS", "G8", "GM", "E8", "MK"))
                    WGt, SM, GT_, NMK, SLOT, N8, SL8f, JK = (d[k] for k in ("WGt", "SM", "GT_", "NMK", "SLOT", "N8", "SL8f", "JK"))
                    tr = d["t"]
                    SC = SCA[:, i, :]
                    V = nc.vector
                    R = dict(reads=[tr], writes=[tr])
                    op(dve, lambda: V.tensor_tensor(SEL[:], SC, RB[:], ALU.add), reads=[tSCA[i], tRB], writes=[tr])
                    yield
                    for g in range(8):
                        op(dve, lambda g=g: V.max(out=T8[:, g, :], in_=SEL[:, g * 8:(g + 1) * 8]), **R)
                        yield
                    op(dve, lambda: V.tensor_tensor(GS[:], T8[:, :, 0], T8[:, :, 1], ALU.add), **R)
                    yield
                    op(dve, lambda: V.max(out=G8[:], in_=GS[:]), **R)
                    yield
                    op(dve, lambda: V.tensor_scalar(GM[:], GS[:], G8[:, 3:4], None, ALU.is_ge), **R)
                    yield
                    op(dve, lambda: V.tensor_scalar(GM[:], GM[:], 1.0, 1.0e4, ALU.subtract, ALU.mult), **R)
                    yield
                    for g in range(8):
                        op(dve, lambda g=g: V.tensor_scalar(SELM[:, g * 8:(g + 1) * 8], SEL[:, g * 8:(g + 1) * 8],
                                                            GM[:, g:g + 1], None, ALU.add), **R)
                        yield
                    op(dve, lambda: V.max(out=E8[:], in_=SELM[:]), **R)
                    yield
                    op(dve, lambda: V.tensor_scalar(MK[:], SELM[:], E8[:, 7:8], None, ALU.is_ge), **R)
                    yield
                    op(dve, lambda: V.tensor_copy(MKB[:, i, :], MK[:]), reads=[tr], writes=[tMKB[i]])
                    yield
                    op(dve, lambda: V.tensor_tensor(WGt[:], SC, MK[:], ALU.mult), **R)
                    yield
                    op(dve, lambda: V.tensor_reduce(SM[:], WGt[:], AX.X, ALU.add), **R)
                    yield
                    op(dve, lambda: V.reciprocal(SM[:], SM[:]), **R)
                    yield
                    op(dve, lambda: V.tensor_scalar(GT_[:], WGt[:], SM[:, 0:1], ROUTED_SCALE, ALU.mult, ALU.mult), **R)
                    yield
                    pp, tpp = PSposL[r], tPSposL[r]
                    op(pe, lambda: nc.tensor.matmul(pp[:, 0:NEXP], lhsT=LT[:], rhs=MKB[:, i, :],
                                                    start=True, stop=(i == 0)),
                       reads=[tK, tMKB[i]], writes=[tpp], sig=(i == 0))
                    for i2 in range(i):
                        op(pe, lambda i2=i2: nc.tensor.matmul(pp[:, 0:NEXP], lhsT=ONESM[:], rhs=MKB[:, i2, :],
                                                              start=False, stop=(i2 == i - 1)),
                           reads=[tK, tMKB[i2]], writes=[tpp], sig=(i2 == i - 1))
                    op(dve, lambda: V.tensor_scalar(NMK[:], MK[:], 1.0, -1.0e6, ALU.subtract, ALU.mult), **R)
                    yield
                    op(dve, lambda: V.tensor_tensor(SLOT[:], pp[:, 0:NEXP], EOFF[:], ALU.add),
                       reads=[tpp, tK, tr], writes=[tr])
                    yield
                    op(dve, lambda: V.tensor_tensor(SLOT[:], SLOT[:], NMK[:], ALU.add), **R)
                    yield
                    op(dve, lambda: V.tensor_scalar(SLOT[:], SLOT[:], -1.0, None, ALU.mult), **R)
                    yield
                    op(dve, lambda: V.max(out=N8[:], in_=SLOT[:]), **R)
                    yield
                    op(dve, lambda: V.tensor_scalar(SL8f[:], N8[:], -1.0, None, ALU.mult), **R)
                    yield
                    op(dve, lambda: V.tensor_copy(SL8I[:, i, :], SL8f[:]), reads=[tr], writes=[tSL[i]])
                    yield
                    for k in range(8):
                        op(dve, lambda k=k: V.scalar_tensor_tensor(
                            out=JK[:], in0=SLOT[:], scalar=N8[:, k:k + 1], in1=GT_[:], op0=ALU.is_equal,
                            op1=ALU.mult, accum_out=G8v[:, i, k:k + 1]),
                           reads=[tr], writes=[tr, tSL[i]])
                        yield
                    for k in range(8):
                        dma(pool, None, None, reads=[tXB[i], tSL[i]] + tZ, writes=[Tok()],
                            fn=lambda k=k: nc.gpsimd.indirect_dma_start(
                                out=XG[:, :], out_offset=bass.IndirectOffsetOnAxis(ap=SL8I[:, i, k:k + 1], axis=0),
                                in_=XB[:, i, :], in_offset=None))
                    yield

                layer_norm_x(p3a, ln2_g, ln2_b, PSt, tPSt, per_tile=router, dbg_out=dbg_d.get("d_x2"))
                for base_ in range(0, NT, NR):
                    gens = [route_chain(base_ + r, r) for r in range(NR)]
                    while gens:
                        for g_ in list(gens):
                            try:
                                next(g_)
                            except StopIteration:
                                gens.remove(g_)
                for i in range(NT):
                    op(dve, lambda i=i: nc.vector.tensor_scalar(X[:, i, :], X[:, i, :], ALPHA, None, ALU.mult),
                       reads=[tX[i]], writes=[tX[i]])
                K.barrier(skip_pool_dma=True)

            if stop == 5:
                K.barrier()
                pxb.close()
                dump_x()
                return nc
            with contextlib.ExitStack() as p3s:
                PSg = [ps(p3s, "p3g%d" % k, [128, 512]) for k in range(2)]
                PSu = [ps(p3s, "p3u%d" % k, [128, 512]) for k in range(2)]
                PSd = [ps(p3s, "p3d%d" % k, [128, 512]) for k in range(4)]
                tPSg, tPSu, tPSd = toks(2), toks(2), toks(4)
                WG = sb(p3s, "sWG", [128, NKC, 256], BF16)
                WU = sb(p3s, "sWU", [128, NKC, 256], BF16)
                WD = sb(p3s, "sWD", [128, 2, D], BF16)
                tWG, tWU, tWD = Tok(), Tok(), Tok()
                HT = sb(p3s, "sHT", [128, 2, S], BF16)
                tHT = toks(4)
                SG = [sb(p3s, "sSG%d" % k, [128, 512], BF16) for k in range(2)]
                tSG = toks(2)
                sWGs = sb(p3s, "sWGs", [128, NKC, 256], F32)
                sWUs = sb(p3s, "sWUs", [128, NKC, 256], F32)
                sWDs = sb(p3s, "sWDs", [128, 2, D], F32)
                tsW = toks(3)
                dma(sp, sWGs[:], w_sg.rearrange("(c p) n -> p c n", p=128), writes=[tsW[0]])
                dma(sp, sWUs[:], w_su.rearrange("(c p) n -> p c n", p=128), writes=[tsW[1]])
                dma(sp, sWDs[:], w_sd.rearrange("(c p) n -> p c n", p=128), writes=[tsW[2]])
                op(act, lambda: nc.scalar.copy(WG[:], sWGs[:]), reads=[tsW[0]], writes=[tWG])
                op(dve, lambda: nc.vector.tensor_copy(WU[:], sWUs[:]), reads=[tsW[1]], writes=[tWU])
                op(act, lambda: nc.scalar.copy(WD[:], sWDs[:]), reads=[tsW[2]], writes=[tWD])
                cnt = 0
                for tg in range(4):
                    for hc in range(2):
                        k = cnt % 2
                        cnt += 1
                        for kc in range(NKC):
                            op(pe, lambda kc=kc, tg=tg, hc=hc, k=k: nc.tensor.matmul(
                                PSg[k][:], lhsT=WG[:, kc, hc * 128:(hc + 1) * 128],
                                rhs=XT[:, kc, tg * 512:(tg + 1) * 512], start=(kc == 0), stop=(kc == NKC - 1)),
                               reads=[tWG] + tXT[tg * 4:(tg + 1) * 4], writes=[tPSg[k]], sig=(kc == NKC - 1))
                        for kc in range(NKC):
                            op(pe, lambda kc=kc, tg=tg, hc=hc, k=k: nc.tensor.matmul(
                                PSu[k][:], lhsT=WU[:, kc, hc * 128:(hc + 1) * 128],
                                rhs=XT[:, kc, tg * 512:(tg + 1) * 512], start=(kc == 0), stop=(kc == NKC - 1)),
                               reads=[tWU] + tXT[tg * 4:(tg + 1) * 4], writes=[tPSu[k]], sig=(kc == NKC - 1))
                        op(act, lambda k=k: nc.scalar.activation(SG[k][:], PSg[k][:], AF.Silu),
                           reads=[tPSg[k]], writes=[tSG[k]])
                        op(dve, lambda k=k, tg=tg, hc=hc: nc.vector.tensor_tensor(
                            HT[:, hc, tg * 512:(tg + 1) * 512], SG[k][:], PSu[k][:], ALU.mult),
                           reads=[tSG[k], tPSu[k]], writes=[tHT[tg]])
                for i in range(NT):
                    for hf in range(2):
                        k = (i * 2 + hf) % 4
                        for hc in range(2):
                            op(pe, lambda i=i, hf=hf, hc=hc, k=k: nc.tensor.matmul(
                                PSd[k][:], lhsT=HT[:, hc, i * 128:(i + 1) * 128],
                                rhs=WD[:, hc, hf * 512:(hf + 1) * 512], start=(hc == 0), stop=(hc == 1)),
                               reads=[tHT[i // 4], tWD], writes=[tPSd[k]], sig=(hc == 1))
                        op(dve, lambda i=i, hf=hf, k=k: nc.vector.tensor_tensor(
                            X[:, i, hf * 512:(hf + 1) * 512], X[:, i, hf * 512:(hf + 1) * 512], PSd[k][:], ALU.add),
                           reads=[tPSd[k], tX[i]], writes=[tX[i]])
                K.barrier()

            pxb.close()
            stXT.close()
            with contextlib.ExitStack() as p3b:
                PSTr = [ps(p3b, "p3t%d" % k, [128, 1024], BF16) for k in range(2)]
                PSg = [ps(p3b, "p3g%d" % k, [128, 512]) for k in range(2)]
                PSu = [ps(p3b, "p3u%d" % k, [128, 512]) for k in range(2)]
                PSd = [ps(p3b, "p3d%d" % k, [128, 512]) for k in range(2)]
                tPSTr, tPSg, tPSu, tPSd = toks(2), toks(2), toks(2), toks(2)
                NJ = CAP // 128
                XS = [sb(p3b, "XS%d" % k, [128, NJ, D], BF16) for k in range(2)]
                XGT = [sb(p3b, "XGT%d" % k, [128, NKC, CAP], BF16) for k in range(2)]
                WGs = [sb(p3b, "WGs%d" % k, [128, NKC, 256], F32) for k in range(3)]
                WUs = [sb(p3b, "WUs%d" % k, [128, NKC, 256], F32) for k in range(3)]
                WDs = [sb(p3b, "WDs%d" % k, [128, 2, D], F32) for k in range(3)]
                WG = [sb(p3b, "WG%d" % k, [128, NKC, 256], BF16) for k in range(2)]
                WU = [sb(p3b, "WU%d" % k, [128, NKC, 256], BF16) for k in range(2)]
                WD = [sb(p3b, "WD%d" % k, [128, 2, D], BF16) for k in range(2)]
                HT = [sb(p3b, "HT%d" % k, [128, 2, CAP], BF16) for k in range(2)]
                SG1 = sb(p3b, "SG", [128, CAP], BF16)
                SG = [SG1, SG1]
                YS1 = sb(p3b, "YS", [128, NJ, D], BF16)
                YS = [YS1, YS1]
                tXS, tXGT, tWG, tWU, tWD, tHT = (toks(2) for _ in range(6))
                tSG1, tYS1 = Tok(), Tok()
                tSG, tYS = [tSG1, tSG1], [tYS1, tYS1]
                tWGs, tWUs, tWDs = toks(3), toks(3), toks(3)

                def prefetch_xs(e):
                    par = e % 2
                    dma(sp, XS[par][:], XG[e * CAP:(e + 1) * CAP, :].rearrange("(j p) n -> p j n", p=128),
                        writes=[tXS[par]])

                def prefetch_w(e):
                    p3_ = e % 3
                    dma(sp, WGs[p3_][:], w_eg[e].rearrange("(c p) n -> p c n", p=128), writes=[tWGs[p3_]])
                    dma(sp, WUs[p3_][:], w_eu[e].rearrange("(c p) n -> p c n", p=128), writes=[tWUs[p3_]])
                    dma(sp, WDs[p3_][:], w_ed[e].rearrange("(c p) n -> p c n", p=128), writes=[tWDs[p3_]])

                def cast_w(e):
                    p2_, p3_ = e % 2, e % 3
                    op(act, lambda: nc.scalar.copy(WG[p2_][:], WGs[p3_][:]), reads=[tWGs[p3_]], writes=[tWG[p2_]])
                    op(dve, lambda: nc.vector.tensor_copy(WU[p2_][:], WUs[p3_][:]), reads=[tWUs[p3_]],
                       writes=[tWU[p2_]])
                    op(pool, lambda: nc.gpsimd.tensor_copy(WD[p2_][:], WDs[p3_][:]), reads=[tWDs[p3_]],
                       writes=[tWD[p2_]])

                ev = [0]

                def transposes(e):
                    par = e % 2
                    for j in range(NJ):
                        pt, tpt = PSTr[j % 2], tPSTr[j % 2]
                        for c in range(NKC):
                            op(pe, lambda j=j, c=c, pt=pt: nc.tensor.transpose(
                                pt[:, c * 128:(c + 1) * 128], XS[par][:, j, c * 128:(c + 1) * 128], identb[:]),
                               reads=[tXS[par], tC], writes=[tpt], sig=(c == NKC - 1))
                        ev[0] += 1
                        if ev[0] % 2 == 0:
                            op(act, lambda j=j, pt=pt: nc.scalar.copy(
                                XGT[par][:, :, j * 128:(j + 1) * 128], pt[:].rearrange("p (c t) -> p c t", c=NKC)),
                               reads=[tpt], writes=[tXGT[par]])
                        else:
                            op(dve, lambda j=j, pt=pt: nc.vector.tensor_copy(
                                XGT[par][:, :, j * 128:(j + 1) * 128], pt[:].rearrange("p (c t) -> p c t", c=NKC)),
                               reads=[tpt], writes=[tXGT[par]])

                prefetch_xs(0)
                prefetch_w(0)
                prefetch_w(1)
                prefetch_xs(1)
                cast_w(0)
                transposes(0)
                for e in range(NEXP):
                    par = e % 2
                    if e + 2 < NEXP:
                        prefetch_xs(e + 2)
                        prefetch_w(e + 2)
                    if e + 1 < NEXP:
                        cast_w(e + 1)
                    for hc in range(2):
                        k = hc
                        for kc in range(NKC):
                            op(pe, lambda kc=kc, hc=hc, k=k: nc.tensor.matmul(
                                PSg[k][:], lhsT=WG[par][:, kc, hc * 128:(hc + 1) * 128], rhs=XGT[par][:, kc, :],
                                start=(kc == 0), stop=(kc == NKC - 1)),
                               reads=[tWG[par], tXGT[par]], writes=[tPSg[k]], sig=(kc == NKC - 1))
                        for kc in range(NKC):
                            op(pe, lambda kc=kc, hc=hc, k=k: nc.tensor.matmul(
                                PSu[k][:], lhsT=WU[par][:, kc, hc * 128:(hc + 1) * 128], rhs=XGT[par][:, kc, :],
                                start=(kc == 0), stop=(kc == NKC - 1)),
                               reads=[tWU[par], tXGT[par]], writes=[tPSu[k]], sig=(kc == NKC - 1))
                        op(act, lambda k=k: nc.scalar.activation(SG[k][:], PSg[k][:], AF.Silu),
                           reads=[tPSg[k]], writes=[tSG[k]])
                        op(dve, lambda k=k, hc=hc: nc.vector.tensor_tensor(
                            HT[par][:, hc, :], SG[k][:], PSu[k][:], ALU.mult),
                           reads=[tSG[k], tPSu[k]], writes=[tHT[par]])
                    if e + 1 < NEXP:
                        transposes(e + 1)
                    for j in range(NJ):
                        for hf in range(2):
                            k = (j * 2 + hf) % 2
                            for hc in range(2):
                                op(pe, lambda j=j, hf=hf, hc=hc, k=k: nc.tensor.matmul(
                                    PSd[k][:], lhsT=HT[par][:, hc, j * 128:(j + 1) * 128],
                                    rhs=WD[par][:, hc, hf * 512:(hf + 1) * 512], start=(hc == 0), stop=(hc == 1)),
                                   reads=[tHT[par], tWD[par]], writes=[tPSd[k]], sig=(hc == 1))
                            ev[0] += 1
                            if ev[0] % 2 == 0:
                                op(act, lambda j=j, hf=hf, k=k: nc.scalar.copy(
                                    YS[par][:, j, hf * 512:(hf + 1) * 512], PSd[k][:]),
                                   reads=[tPSd[k]], writes=[tYS[par]])
                            else:
                                op(dve, lambda j=j, hf=hf, k=k: nc.vector.tensor_copy(
                                    YS[par][:, j, hf * 512:(hf + 1) * 512], PSd[k][:]),
                                   reads=[tPSd[k]], writes=[tYS[par]])
                    dma(pool, YG[e * CAP:(e + 1) * CAP, :].rearrange("(j p) n -> p j n", p=128), YS[par][:],
                        reads=[tYS[par]])
                K.barrier()

            with contextlib.ExitStack() as p3c:
                NB = 6
                YR = [sb(p3c, "YR%d" % k, [128, D], BF16) for k in range(NB)]
                tYR = toks(NB)
                n = 0
                for i in range(NT):
                    for k in range(8):
                        bfi = n % NB
                        n += 1
                        dma(pool, None, None, reads=[tSL[i]], writes=[tYR[bfi]],
                            fn=lambda i=i, k=k, bfi=bfi: nc.gpsimd.indirect_dma_start(
                                out=YR[bfi][:], out_offset=None, in_=YG[:, :],
                                in_offset=bass.IndirectOffsetOnAxis(ap=SL8I[:, i, k:k + 1], axis=0)))
                        op(dve, lambda i=i, k=k, bfi=bfi: nc.vector.scalar_tensor_tensor(
                            out=X[:, i, :], in0=YR[bfi][:], scalar=G8v[:, i, k:k + 1], in1=X[:, i, :],
                            op0=ALU.mult, op1=ALU.add),
                           reads=[tYR[bfi], tSL[i], tX[i]], writes=[tX[i]])
                K.barrier()

        with contextlib.ExitStack() as pl3:
            layer_norm_x(pl3, ln3_g, ln3_b, None, None, want_xt=False, out_dram=out_d)
            K.barrier()
    return nc


_NC_CACHE = {}


def _prep_inputs(inputs, b):
    f = lambda a: np.ascontiguousarray(np.asarray(a, dtype=np.float32))
    m = {
        "x": f(inputs["x"][b]),
        "mem": f(inputs["mem"][b]),
        "ln_in_g": f(inputs["ln_in_g"]).reshape(1, D),
        "ln_in_b": f(inputs["ln_in_b"]).reshape(1, D),
        "w_in": f(inputs["w_in"][0]),
        "b_in": f(inputs["b_in"][0]).reshape(56, 128),
        "ln_v_g": f(inputs["ln_v_g"][0]).reshape(1, D),
        "ln_v_b": f(inputs["ln_v_b"][0]).reshape(1, D),
        "w_spatial": f(inputs["w_spatial"][0]),
        "b_spatial": f(inputs["b_spatial"][0]),
        "w_out": f(inputs["w_out"][0]),
        "ln1_g": f(inputs["ln1_g"][0]).reshape(1, D),
        "ln1_b": f(inputs["ln1_b"][0]).reshape(1, D),
        "w_mem_q": f(inputs["w_mem_q"][0]),
        "w_mem_kv": f(inputs["w_mem_kv"][0]),
        "w_mem_o": f(inputs["w_mem_o"][0]),
        "ln2_g": f(inputs["ln2_g"][0]).reshape(1, D),
        "ln2_b": f(inputs["ln2_b"][0]).reshape(1, D),
        "w_router": f(inputs["w_router"][0]),
        "router_bias": f(inputs["router_bias"][0]).reshape(1, NEXP),
        "w_exp_gate": f(inputs["w_exp_gate"][0]),
        "w_exp_up": f(inputs["w_exp_up"][0]),
        "w_exp_down": f(inputs["w_exp_down"][0]),
        "w_sh_gate": f(inputs["w_sh_gate"][0]),
        "w_sh_up": f(inputs["w_sh_up"][0]),
        "w_sh_down": f(inputs["w_sh_down"][0]),
        "ln3_g": f(inputs["ln3_g"][0]).reshape(1, D),
        "ln3_b": f(inputs["ln3_b"][0]).reshape(1, D),
    }
    return m


def kernel(**inputs):
    dbg = bool(os.environ.get("MK_DEBUG"))
    if dbg not in _NC_CACHE:
        _NC_CACHE[dbg] = build(dbg, int(os.environ.get("MK_STOP", "99")))
    nc = _NC_CACHE[dbg]
    shared = _prep_inputs(inputs, 0)
    in_maps = []
    for b in range(8):
        m = dict(shared)
        m["x"] = np.ascontiguousarray(np.asarray(inputs["x"][b], dtype=np.float32))
        m["mem"] = np.ascontiguousarray(np.asarray(inputs["mem"][b], dtype=np.float32))
        in_maps.append(m)
    res = run_bass_kernel_spmd(nc, in_maps, core_ids=list(range(8)))
    out = np.stack([np.asarray(r["out"], dtype=np.float32) for r in res.results], axis=0)
    if dbg:
        kernel.debug = [{k: np.asarray(v) for k, v in r.items()} for r in res.results]
    return out
```

```python
import os
import contextlib
import numpy as np
import ml_dtypes
import concourse.bass as bass
import concourse.mybir as mybir
from concourse.bass_utils import run_bass_kernel_spmd

F32 = mybir.dt.float32
BF16 = mybir.dt.bfloat16
AF = mybir.ActivationFunctionType
ALU = mybir.AluOpType
AX = mybir.AxisListType

S = 2048
D = 1024
NT = 16
NKC = 8
ALPHA = 2.0 ** 0.25
LN_EPS = 1e-5
SB_SCALE = 128.0 ** -0.5
MEM_SCALE = 128.0 ** -0.5
NEXP = 64
ROUTED_SCALE = 2.5
CAP = 512
NSLOT = NEXP * CAP
U32 = mybir.dt.uint32


class Tok:
    __slots__ = ("w", "r")

    def __init__(self):
        self.w = None
        self.r = {}


def toks(n):
    return [Tok() for _ in range(n)]


class Eng:
    def __init__(self, name, h, sem):
        self.name = name
        self.h = h
        self.sem = sem
        self.cnt = 0
        self.known = {}
        self.pool = []
        self.dma_i = 0


class Sched:
    def __init__(self, nc, st, ndma=12):
        self.nc = nc
        mk = lambda n: st.enter_context(nc.semaphore(n))
        self.pe = Eng("pe", nc.tensor, mk("s_pe"))
        self.act = Eng("act", nc.scalar, mk("s_act"))
        self.dve = Eng("dve", nc.vector, mk("s_dve"))
        self.pool = Eng("pool", nc.gpsimd, mk("s_pool"))
        self.sp = Eng("sp", nc.sync, mk("s_sp"))
        self.engs = [self.pe, self.act, self.dve, self.pool, self.sp]
        for q in (self.sp, self.pool, self.act):
            q.pool = [[mk("d_%s_%d" % (q.name, i)), 0] for i in range(ndma)]

    def _emit_waits(self, eng, deps):
        for sem, val in deps.items():
            if eng.known.get(sem, 0) < val:
                if sem is eng.sem:
                    assert val <= eng.cnt, "self-wait on future count"
                eng.h.wait_ge(sem, val)
                eng.known[sem] = val

    def _deps(self, eng, reads, writes):
        deps = {}

        def add(d, raw):
            sem, val = d
            if sem is eng.sem and not raw:
                return
            if deps.get(sem, 0) < val:
                deps[sem] = val

        for t in reads:
            if t.w is not None:
                add(t.w, True)
        for t in writes:
            if t.w is not None:
                add(t.w, False)
            for sem, val in t.r.items():
                add((sem, val), False)
        return deps

    def _mark(self, mark, reads, writes):
        for t in reads:
            if t.r.get(mark[0], 0) < mark[1]:
                t.r[mark[0]] = mark[1]
        for t in writes:
            t.w = mark
            t.r = {}

    def op(self, eng, fn, reads=(), writes=(), sig=True):
        self._emit_waits(eng, self._deps(eng, reads, writes))
        ins = fn()
        if sig:
            ins.then_inc(eng.sem, 1)
            eng.cnt += 1
            mark = (eng.sem, eng.cnt)
        else:
            mark = (eng.sem, eng.cnt + 1)
        self._mark(mark, reads, writes)
        return ins

    def dma(self, q, out, in_, reads=(), writes=(), fn=None, **kw):
        slot = q.pool[q.dma_i % len(q.pool)]
        q.dma_i += 1
        deps = self._deps(q, reads, writes)
        if slot[1] > 0:
            deps[slot[0]] = max(deps.get(slot[0], 0), 16 * slot[1])
        self._emit_waits(q, deps)
        ins = fn() if fn is not None else q.h.dma_start(out=out, in_=in_, **kw)
        ins.then_inc(slot[0], 16)
        slot[1] += 1
        self._mark((slot[0], 16 * slot[1]), reads, writes)
        return ins

    def barrier(self, skip_pool_dma=False):
        targets = {}
        for e in (self.pe, self.act, self.dve, self.pool):
            if e.cnt > 0:
                targets[e.sem] = e.cnt
        for q in ((self.sp, self.act) if skip_pool_dma else (self.sp, self.pool, self.act)):
            for sem, used in q.pool:
                if used > 0:
                    targets[sem] = 16 * used
        for e in self.engs:
            d = {s: v for s, v in targets.items() if not (s is e.sem and e.name == "pe")}
            self._emit_waits(e, d)


def build(dbg=False, stop=99):
    nc = bass.Bass("TRN2", target_bir_lowering=False)

    def din(name, shape):
        return nc.dram_tensor(name, list(shape), F32, kind="ExternalInput").ap()

    x_d = din("x", [S, D])
    mem_d = din("mem", [256, D])
    ln_in_g = din("ln_in_g", [1, D])
    ln_in_b = din("ln_in_b", [1, D])
    w_in = din("w_in", [D, 7168])
    b_in = din("b_in", [56, 128])
    ln_v_g = din("ln_v_g", [1, D])
    ln_v_b = din("ln_v_b", [1, D])
    w_sp = din("w_spatial", [8, 128, 128])
    b_sp = din("b_spatial", [8, 128])
    w_out = din("w_out", [D, D])
    ln1_g = din("ln1_g", [1, D])
    ln1_b = din("ln1_b", [1, D])
    w_mq = din("w_mem_q", [D, 512])
    w_mkv = din("w_mem_kv", [D, 1024])
    w_mo = din("w_mem_o", [512, D])
    ln2_g = din("ln2_g", [1, D])
    ln2_b = din("ln2_b", [1, D])
    w_r = din("w_router", [D, NEXP])
    r_b = din("router_bias", [1, NEXP])
    w_eg = din("w_exp_gate", [NEXP, D, 256])
    w_eu = din("w_exp_up", [NEXP, D, 256])
    w_ed = din("w_exp_down", [NEXP, 256, D])
    w_sg = din("w_sh_gate", [D, 256])
    w_su = din("w_sh_up", [D, 256])
    w_sd = din("w_sh_down", [256, D])
    ln3_g = din("ln3_g", [1, D])
    ln3_b = din("ln3_b", [1, D])
    out_d = nc.dram_tensor("out", [S, D], F32, kind="ExternalOutput").ap()
    XG = nc.dram_tensor("XG_scratch", [NSLOT, D], BF16, kind="Internal").ap()
    YG = nc.dram_tensor("YG_scratch", [NSLOT, D], BF16, kind="Internal").ap()
    dbg_d = {}
    if dbg:
        for nm in ("d_x0", "d_x1", "d_x2"):
            dbg_d[nm] = nc.dram_tensor(nm, [S, D], F32, kind="ExternalOutput").ap()

    w_in_v = w_in.rearrange("(c p) n -> p c n", p=128)

    with contextlib.ExitStack() as st:
        K = Sched(nc, st)
        pe, act, dve, pool, sp = K.pe, K.act, K.dve, K.pool, K.sp
        op, dma = K.op, K.dma

        uid = [0]

        def sb(stk, name, shape, dt):
            uid[0] += 1
            return stk.enter_context(nc.sbuf_tensor("%s_%d" % (name, uid[0]), list(shape), dt))

        def ps(stk, name, shape, dt=F32):
            uid[0] += 1
            return stk.enter_context(nc.psum_tensor("%s_%d" % (name, uid[0]), list(shape), dt))

        X = sb(st, "X", [128, NT, D], F32)
        SL8I = sb(st, "SL8I", [128, NT, 8], U32)
        G8v = sb(st, "G8v", [128, NT, 8], F32)
        tSL = toks(NT)
        tZ = toks(NEXP)
        tX = toks(NT)
        tXT = toks(NT)
        identf = sb(st, "identf", [128, 128], F32)
        identb = sb(st, "identb", [128, 128], BF16)
        onesf = sb(st, "onesf", [128, 128], F32)
        BC = sb(st, "BC", [128, 56], F32)
        ONESB = sb(st, "ONESB", [1, 128], BF16)
        NEGH = sb(st, "NEGH", [128, NT], F32)
        tC = Tok()
        stXT = contextlib.ExitStack()
        XT = sb(stXT, "XT", [128, NKC, S], BF16)

        FILL0 = nc.gpsimd.to_reg(0.0)
        FILL1 = nc.gpsimd.to_reg(1.0)
        op(dve, lambda: nc.vector.memset(onesf[:], 1.0), writes=[tC])
        op(dve, lambda: nc.vector.memset(ONESB[:], 1.0), writes=[tC])
        op(dve, lambda: nc.vector.memset(NEGH[:], -0.5), writes=[tC])
        op(pool, lambda: nc.gpsimd.affine_select(out=identf[:], in_=onesf[:], pattern=[[1, 128]],
                                                 compare_op=ALU.is_equal, fill=FILL0, base=0,
                                                 channel_multiplier=-1), reads=[tC], writes=[tC])
        op(pool, lambda: nc.gpsimd.affine_select(out=identb[:], in_=onesf[:], pattern=[[1, 128]],
                                                 compare_op=ALU.is_equal, fill=FILL0, base=0,
                                                 channel_multiplier=-1), reads=[tC], writes=[tC])

        def layer_norm_x(stk, g_d, b_d, PSt, tPSt, want_xt=True, per_tile=None, out_dram=None, dbg_out=None):
            G = sb(stk, "lnG", [128, D], F32)
            Bt = sb(stk, "lnB", [128, D], F32)
            ST = sb(stk, "lnST", [128, NT, 12], F32)
            MV = sb(stk, "lnMV", [128, NT, 2], F32)
            RS = sb(stk, "lnRS", [128, NT], F32)
            tG, tB, tS, tM, tR = Tok(), Tok(), Tok(), Tok(), Tok()
            dma(sp, G[:], g_d.partition_broadcast(128), writes=[tG])
            dma(sp, Bt[:], b_d.partition_broadcast(128), writes=[tB])
            for i in range(NT):
                for hf in range(2):
                    op(dve, lambda i=i, hf=hf: nc.vector.bn_stats(ST[:, i, hf * 6:(hf + 1) * 6],
                                                                  X[:, i, hf * 512:(hf + 1) * 512]),
                       reads=[tX[i]], writes=[tS])
                op(dve, lambda i=i: nc.vector.bn_aggr(MV[:, i, :], ST[:, i, :]), reads=[tS], writes=[tM])
            op(dve, lambda: nc.vector.tensor_scalar(RS[:], MV[:, :, 1], LN_EPS, None, ALU.add),
               reads=[tM], writes=[tR])
            op(pool, lambda: nc.gpsimd.tensor_tensor(RS[:], RS[:], NEGH[:], ALU.pow), reads=[tR, tC], writes=[tR])
            for i in range(NT):
                op(dve, lambda i=i: nc.vector.tensor_scalar(X[:, i, :], X[:, i, :], MV[:, i, 0:1], RS[:, i:i + 1],
                                                            ALU.subtract, ALU.mult),
                   reads=[tX[i], tM, tR], writes=[tX[i]])
                op(dve, lambda i=i: nc.vector.tensor_tensor(X[:, i, :], X[:, i, :], G[:], ALU.mult),
                   reads=[tX[i], tG], writes=[tX[i]])
                op(pool, lambda i=i: nc.gpsimd.tensor_tensor(X[:, i, :], X[:, i, :], Bt[:], ALU.add),
                   reads=[tX[i], tB], writes=[tX[i]])
                if dbg_out is not None:
                    dma(sp, dbg_out[i * 128:(i + 1) * 128, :], X[:, i, :], reads=[tX[i]])
                if out_dram is not None:
                    dma(sp, out_dram[i * 128:(i + 1) * 128, :], X[:, i, :], reads=[tX[i]])
                if want_xt:
                    for hb in range(2):
                        pb = PSt[hb]
                        for c4 in range(4):
                            c = hb * 4 + c4
                            op(pe, lambda i=i, c=c, c4=c4, pb=pb: nc.tensor.transpose(
                                pb[:, c4 * 128:(c4 + 1) * 128], X[:, i, c * 128:(c + 1) * 128], identf[:]),
                               reads=[tX[i], tC], writes=[tPSt[hb]], sig=(c4 == 3))
                        op(act, lambda i=i, hb=hb, pb=pb: nc.scalar.copy(
                            XT[:, hb * 4:(hb + 1) * 4, i * 128:(i + 1) * 128],
                            pb[:].rearrange("p (c t) -> p c t", c=4)),
                           reads=[tPSt[hb]], writes=[tXT[i]])
                        if per_tile is not None:
                            per_tile(i, hb, pb, tPSt[hb])

        def dump_x():
            for i in range(NT):
                dma(sp, out_d[i * 128:(i + 1) * 128, :], X[:, i, :], reads=[tX[i]])
            K.barrier()
            stXT.close()

        with contextlib.ExitStack() as p0:
            PSt = [ps(p0, "p0t%d" % k, [128, 512]) for k in range(2)]
            tPSt = toks(2)
            for i in range(NT):
                dma(sp, X[:, i, :], x_d[i * 128:(i + 1) * 128, :], writes=[tX[i]])
            BI = sb(p0, "BI", [56, 128], F32)
            tBI = Tok()
            dma(sp, BI[:], b_in[:, :], writes=[tBI])
            PSb = ps(p0, "p0b", [128, 512])
            tPSb = Tok()
            op(pe, lambda: nc.tensor.transpose(PSb[:, 0:56], BI[:], identf[0:56, 0:56]),
               reads=[tBI, tC], writes=[tPSb])
            op(act, lambda: nc.scalar.copy(BC[:], PSb[:, 0:56]), reads=[tPSb], writes=[tC])
            layer_norm_x(p0, ln_in_g, ln_in_b, PSt, tPSt, dbg_out=dbg_d.get("d_x0"))
            for i in range(NT):
                op(dve, lambda i=i: nc.vector.tensor_scalar(X[:, i, :], X[:, i, :], ALPHA, None, ALU.mult),
                   reads=[tX[i]], writes=[tX[i]])
            K.barrier()

        def proj_fm(Wblk, tW, PSbanks, tPS, consume):
            for tg in range(4):
                pb = PSbanks[tg % len(PSbanks)]
                tp = tPS[tg % len(PSbanks)]
                for kc in range(NKC):
                    op(pe, lambda kc=kc, tg=tg, pb=pb: nc.tensor.matmul(
                        pb[:], lhsT=Wblk[:, kc, :], rhs=XT[:, kc, tg * 512:(tg + 1) * 512],
                        start=(kc == 0), stop=(kc == NKC - 1)),
                       reads=[tW] + tXT[tg * 4:(tg + 1) * 4], writes=[tp], sig=(kc == NKC - 1))
                consume(tg, pb, tp)

        def out_proj_accum(MT2, tMT2, WO2, tWO2, PSo, tPSo, nu=2):
            for i in range(NT):
                for hf in range(2):
                    k = (i * 2 + hf) % len(PSo)
                    pb, tp = PSo[k], tPSo[k]
                    for u in range(nu):
                        op(pe, lambda i=i, hf=hf, u=u, pb=pb: nc.tensor.matmul(
                            pb[:, 0:512], lhsT=MT2[:, u, i * 128:(i + 1) * 128], rhs=WO2[:, u, hf * 512:(hf + 1) * 512],
                            start=(u == 0), stop=(u == nu - 1)),
                           reads=[tMT2[u], tWO2], writes=[tp], sig=(u == nu - 1))
                    op(dve, lambda i=i, hf=hf, pb=pb: nc.vector.tensor_tensor(
                        X[:, i, hf * 512:(hf + 1) * 512], X[:, i, hf * 512:(hf + 1) * 512], pb[:, 0:512], ALU.add),
                       reads=[tp, tX[i]], writes=[tX[i]])

        if stop == 0:
            dump_x()
            return nc
        with contextlib.ExitStack() as p1:
            VN = sb(p1, "VN", [128, NT, D], BF16)
            tVN = toks(NT)
            PSa = [ps(p1, "p1a%d" % k, [128, 512]) for k in range(4)]
            tPSa = toks(4)
            PSm = [ps(p1, "p1m%d" % k, [128, 512]) for k in range(2)]
            tPSm = toks(2)
            PSo = [ps(p1, "p1o%d" % k, [128, 512]) for k in range(2)]
            tPSo = toks(2)
            with contextlib.ExitStack() as p1a:
                W2 = [sb(p1a, "Wv%d" % k, [128, NKC, 512], BF16) for k in range(2)]
                BR = sb(p1a, "BRv", [1, D], BF16)
                tW2, tBR = toks(2), Tok()
                G = sb(p1a, "vG", [128, D], F32)
                Bt = sb(p1a, "vB", [128, D], F32)
                ST = sb(p1a, "vST", [128, NT, 12], F32)
                MV = sb(p1a, "vMV", [128, NT, 2], F32)
                RS = sb(p1a, "vRS", [128, NT], F32)
                tG, tB = Tok(), Tok()
                tLv = toks(NT)
                dma(sp, G[:], ln_v_g.partition_broadcast(128), writes=[tG])
                dma(sp, Bt[:], ln_v_b.partition_broadcast(128), writes=[tB])
                dma(pool, BR[:], b_in[8:16, :].rearrange("a b -> (a b)").rearrange("(o n) -> o n", o=1), writes=[tBR])
                for hf in range(2):
                    dma(pool, W2[hf][:], w_in_v[:, :, 1024 + hf * 512:1024 + (hf + 1) * 512], writes=[tW2[hf]])
                for hf in range(2):
                    W, tW = W2[hf], tW2[hf]
                    for i in range(NT):
                        k = i % 4
                        pb, tp = PSa[k], tPSa[k]
                        for kc in range(NKC):
                            op(pe, lambda kc=kc, i=i, pb=pb, W=W: nc.tensor.matmul(
                                pb[:], lhsT=XT[:, kc, i * 128:(i + 1) * 128], rhs=W[:, kc, :],
                                start=(kc == 0), stop=False),
                               reads=[tW, tXT[i]], writes=[tp], sig=False)
                        op(pe, lambda hf=hf, pb=pb: nc.tensor.matmul(
                            pb[:], lhsT=ONESB[0:1, :], rhs=BR[0:1, hf * 512:(hf + 1) * 512], start=False, stop=True),
                           reads=[tBR, tC], writes=[tp])
                        op(act, lambda i=i, hf=hf, pb=pb: nc.scalar.activation(
                            VN[:, i, hf * 512:(hf + 1) * 512], pb[:], AF.Gelu),
                           reads=[tp], writes=[tVN[i]])
                        if hf == 1:
                            tl = tLv[i]
                            for h2 in range(2):
                                op(dve, lambda i=i, h2=h2: nc.vector.bn_stats(ST[:, i, h2 * 6:(h2 + 1) * 6],
                                                                              VN[:, i, h2 * 512:(h2 + 1) * 512]),
                                   reads=[tVN[i]], writes=[tl])
                            op(dve, lambda i=i: nc.vector.bn_aggr(MV[:, i, :], ST[:, i, :]), reads=[tl], writes=[tl])
                            op(dve, lambda i=i: nc.vector.tensor_scalar(RS[:, i:i + 1], MV[:, i, 1:2], LN_EPS, None,
                                                                        ALU.add),
                               reads=[tl], writes=[tl])
                            op(pool, lambda i=i: nc.gpsimd.tensor_tensor(RS[:, i:i + 1], RS[:, i:i + 1], NEGH[:, 0:1],
                                                                         ALU.pow),
                               reads=[tl, tC], writes=[tl])
                            op(dve, lambda i=i: nc.vector.tensor_scalar(VN[:, i, :], VN[:, i, :], MV[:, i, 0:1],
                                                                        RS[:, i:i + 1], ALU.subtract, ALU.mult),
                               reads=[tVN[i], tl], writes=[tVN[i]])
                            op(dve, lambda i=i: nc.vector.tensor_tensor(VN[:, i, :], VN[:, i, :], G[:], ALU.mult),
                               reads=[tVN[i], tG], writes=[tVN[i]])
                            op(pool, lambda i=i: nc.gpsimd.tensor_tensor(VN[:, i, :], VN[:, i, :], Bt[:], ALU.add),
                               reads=[tVN[i], tB], writes=[tVN[i]])
                K.barrier()

            with contextlib.ExitStack() as p1b:
                WT = sb(p1b, "WT", [128, 8, 128], BF16)
                WS = sb(p1b, "WS", [128, 8, 128], F32)
                BS = sb(p1b, "BS", [128, 8, 128], F32)
                tWT, tWS, tBS = Tok(), Tok(), Tok()
                dma(sp, WS[:], w_sp.rearrange("g t s -> t g s"), writes=[tWS])
                dma(sp, BS[:].rearrange("p g t -> p (g t)"),
                    b_sp.rearrange("g t -> (g t)").rearrange("(o n) -> o n", o=1).partition_broadcast(128),
                    writes=[tBS])
                ZT = sb(p1b, "ZT", [128, CAP // 128, D], BF16)
                tZT = Tok()
                op(pool, lambda: nc.gpsimd.memset(ZT[:], 0.0), writes=[tZT])
                for g in range(8):
                    op(pool, lambda g=g: nc.gpsimd.affine_select(out=WS[:, g, :], in_=WS[:, g, :], pattern=[[-1, 128]],
                                                                 compare_op=ALU.is_ge, fill=FILL0, base=0,
                                                                 channel_multiplier=1),
                       reads=[tWS], writes=[tWS])
                for g4 in range(2):
                    pb, tp = PSm[g4], tPSm[g4]
                    for gg in range(4):
                        g = g4 * 4 + gg
                        op(pe, lambda g=g, gg=gg, pb=pb: nc.tensor.transpose(
                            pb[:, gg * 128:(gg + 1) * 128], WS[:, g, :], identf[:]),
                           reads=[tWS, tC], writes=[tp], sig=(gg == 3))
                    op(act, lambda g4=g4, pb=pb: nc.scalar.copy(
                        WT[:, g4 * 4:(g4 + 1) * 4, :], pb[:].rearrange("p (g t) -> p g t", g=4)),
                       reads=[tp], writes=[tWT])

                WB = [sb(p1b, "WB%d" % k, [128, 2, NKC, 128], BF16) for k in range(2)]
                tWB = toks(2)
                WO2 = sb(p1b, "WO4", [128, 4, D], BF16)
                tWO2 = Tok()
                U2 = [sb(p1b, "U%d" % k, [128, S], BF16) for k in range(2)]
                GA2 = [sb(p1b, "GA%d" % k, [128, S], BF16) for k in range(2)]
                T1 = [sb(p1b, "T1%d" % k, [128, 512], BF16) for k in range(2)]
                tU2, tGA2, tT1 = [toks(4), toks(4)], [toks(4), toks(4)], toks(2)
                MT2 = sb(p1b, "MT4", [128, 4, S], BF16)
                tMT2 = toks(4)
                def load_group_w(g):
                    par = g % 2
                    dma(pool, WB[par][:, 0, :, :], w_in_v[:, :, g * 128:(g + 1) * 128], writes=[tWB[par]])
                    dma(pool, WB[par][:, 1, :, :], w_in_v[:, :, 5120 + g * 128:5120 + (g + 1) * 128],
                        writes=[tWB[par]])

                load_group_w(0)
                for g in range(8):
                    par = g % 2
                    g4_ = g % 4
                    U, GA, tU, tGA = U2[par], GA2[par], tU2[par], tGA2[par]
                    if g + 1 < 8:
                        load_group_w(g + 1)
                    if g4_ == 0:
                        dma(pool, WO2[:], w_out[g * 128:(g + 4) * 128, :].rearrange("(u p) n -> p u n", p=128),
                            writes=[tWO2])

                    def cons_u(tg, pb, tp, g=g, U=U, tU=tU):
                        op(act, lambda: nc.scalar.activation(U[:, tg * 512:(tg + 1) * 512], pb[:], AF.Gelu,
                                                             bias=BC[:, g:g + 1], scale=1.0),
                           reads=[tp, tC], writes=[tU[tg]])
                    proj_fm(WB[par][:, 0, :, :], tWB[par], PSa, tPSa, cons_u)
                    for e in range(g * 8, (g + 1) * 8):
                        dma(sp, XG[e * CAP:(e + 1) * CAP, :].rearrange("(j p) n -> p j n", p=128), ZT[:],
                            reads=[tZT, tU[3]], writes=[tZ[e]])

                    def cons_ga(tg, pb, tp, g=g, GA=GA, tGA=tGA):
                        op(act, lambda: nc.scalar.activation(GA[:, tg * 512:(tg + 1) * 512], pb[:], AF.Sigmoid,
                                                             bias=BC[:, 40 + g:41 + g], scale=1.0),
                           reads=[tp, tC], writes=[tGA[tg]])
                    proj_fm(WB[par][:, 1, :, :], tWB[par], PSa, tPSa, cons_ga)

                    for tg in range(4):
                        pb, tp = PSm[tg % 2], tPSm[tg % 2]
                        for c4 in range(4):
                            c = tg * 4 + c4
                            op(pe, lambda c=c, c4=c4, pb=pb, g=g: nc.tensor.matmul(
                                pb[:, c4 * 128:(c4 + 1) * 128], lhsT=VN[:, c, g * 128:(g + 1) * 128],
                                rhs=WT[:, g, :], start=True, stop=True),
                               reads=[tVN[c], tWT], writes=[tp], sig=(c4 == 3))
                        t1 = T1[tg % 2]
                        for c4 in range(4):
                            op(dve, lambda c4=c4, pb=pb, t1=t1, g=g: nc.vector.tensor_tensor(
                                t1[:, c4 * 128:(c4 + 1) * 128], pb[:, c4 * 128:(c4 + 1) * 128], BS[:, g, :], ALU.add),
                               reads=[tp, tBS], writes=[tT1[tg % 2]])
                        op(dve, lambda tg=tg, t1=t1, U=U: nc.vector.tensor_tensor(
                            t1[:], t1[:], U[:, tg * 512:(tg + 1) * 512], ALU.mult),
                           reads=[tT1[tg % 2], tU[tg]], writes=[tT1[tg % 2]])
                        op(pool, lambda tg=tg, t1=t1, g4_=g4_, GA=GA: nc.gpsimd.tensor_tensor(
                            MT2[:, g4_, tg * 512:(tg + 1) * 512], t1[:], GA[:, tg * 512:(tg + 1) * 512], ALU.mult),
                           reads=[tT1[tg % 2], tGA[tg]], writes=[tMT2[g4_]])
                    if g4_ == 3:
                        out_proj_accum(MT2, tMT2, WO2, tWO2, PSo, tPSo, nu=4)
                K.barrier()

        if stop == 1:
            dump_x()
            return nc
        with contextlib.ExitStack() as p1c:
            PSz = [ps(p1c, "pz%d" % k, [128, 512]) for k in range(4)]
            tPSz = toks(4)
            zbase = {}
            PSTr = [ps(p1c, "ptr%d" % k, [128, 1024], BF16) for k in range(2)]
            tPSTr = toks(2)
            PSy = ps(p1c, "py", [128, 512])
            tPSy = Tok()
            PSp = [ps(p1c, "pp", [128, 512])]
            tPSp = toks(1)
            WH = [sb(p1c, "WH%d" % k, [128, 4, NKC, 128], BF16) for k in range(2)]
            tWH = toks(2)
            WO2 = sb(p1c, "WO2c", [128, 2, D], BF16)
            tWO2 = Tok()
            BRV = [sb(p1c, "BRV%d" % k, [1, 128], BF16) for k in range(2)]
            tBRV = toks(2)
            QT = [sb(p1c, "QT%d" % k, [128, S], BF16) for k in range(2)]
            KT = [sb(p1c, "KT%d" % k, [128, S], BF16) for k in range(2)]
            GB = [sb(p1c, "GB%d" % k, [128, S], BF16) for k in range(2)]
            VH = [sb(p1c, "VH%d" % k, [128, NT, 128], BF16) for k in range(2)]
            tQT, tKT, tGB, tVH = [toks(4), toks(4)], [toks(4), toks(4)], [toks(4), toks(4)], [toks(4), toks(4)]
            Rb = [sb(p1c, "Rb%d" % k, [128, S], F32) for k in range(2)]
            Bb = [sb(p1c, "Bb%d" % k, [128, S], BF16) for k in range(4)]
            Pb = [sb(p1c, "Pb%d" % k, [128, S + 2], BF16) for k in range(2)]
            ATb = [sb(p1c, "ATb%d" % k, [128, S], BF16) for k in range(2)]
            tRb, tBb, tATb, tPb = toks(2), toks(4), toks(2), toks(2)
            MT2 = sb(p1c, "MT2c", [128, 2, S], BF16)
            tMT2 = toks(2)

            def load_head_w(h, part=None):
                par = h % 2
                for j, off in enumerate((2048, 3072, 4096, 6144)):
                    if part is None or part == j:
                        dma(pool, WH[par][:, j, :, :], w_in_v[:, :, off + h * 128:off + (h + 1) * 128],
                            writes=[tWH[par]])
                if part is None or part == 4:
                    dma(pool, BRV[par][:], b_in[32 + h:33 + h, :], writes=[tBRV[par]])

            def emit_proj(h):
                par = h % 2
                specs = ((0, QT, tQT, AF.Identity, 16), (1, KT, tKT, AF.Identity, 24), (3, GB, tGB, AF.Sigmoid, 48))
                for (j, DST, tDST, fn_, bcol) in specs:
                    for tg in range(4):
                        pb, tp = PSp[0], tPSp[0]
                        for kc in range(NKC):
                            op(pe, lambda kc=kc, tg=tg, pb=pb, j=j: nc.tensor.matmul(
                                pb[:], lhsT=WH[par][:, j, kc, :], rhs=XT[:, kc, tg * 512:(tg + 1) * 512],
                                start=(kc == 0), stop=(kc == NKC - 1)),
                               reads=[tWH[par]] + tXT[tg * 4:(tg + 1) * 4], writes=[tp], sig=(kc == NKC - 1))
                        op(act, lambda tg=tg, pb=pb, DST=DST, fn_=fn_, bcol=bcol: nc.scalar.activation(
                            DST[par][:, tg * 512:(tg + 1) * 512], pb[:], fn_,
                            bias=BC[:, bcol + h:bcol + h + 1], scale=1.0),
                           reads=[tp, tC], writes=[tDST[par][tg]])
                        yield
                for tg in range(4):
                    pb, tp = PSp[0], tPSp[0]
                    for c4 in range(4):
                        i = tg * 4 + c4
                        for kc in range(NKC):
                            op(pe, lambda kc=kc, i=i, c4=c4, pb=pb: nc.tensor.matmul(
                                pb[:, c4 * 128:(c4 + 1) * 128], lhsT=XT[:, kc, i * 128:(i + 1) * 128],
                                rhs=WH[par][:, 2, kc, :], start=(kc == 0), stop=False),
                               reads=[tWH[par], tXT[i]], writes=[tp], sig=False)
                        op(pe, lambda c4=c4, pb=pb: nc.tensor.matmul(
                            pb[:, c4 * 128:(c4 + 1) * 128], lhsT=ONESB[0:1, :],
                            rhs=BRV[par][0:1, :], start=False, stop=True),
                           reads=[tBRV[par], tC], writes=[tp], sig=(c4 == 3))
                    op(act, lambda tg=tg, pb=pb: nc.scalar.copy(
                        VH[par][:, tg * 4:(tg + 1) * 4, :], pb[:].rearrange("p (c d) -> p c d", c=4)),
                       reads=[tp], writes=[tVH[par][tg]])
                    yield

            def stage_a1_pe(h, i, s_):
                par = h % 2
                nk = 128 * (i + 1)
                nch = (nk + 511) // 512
                base = zbase.get(s_ - 1, (0, 0))
                base = (base[0] + base[1]) % 4
                zbase[s_] = (base, nch)
                for ch in range(nch):
                    k0 = ch * 512
                    w_ = min(512, nk - k0)
                    zi = (base + ch) % 4
                    op(pe, lambda zi=zi, w_=w_, k0=k0: nc.tensor.matmul(
                        PSz[zi][:, 0:w_], lhsT=QT[par][:, i * 128:(i + 1) * 128],
                        rhs=KT[par][:, k0:k0 + w_], start=True, stop=True),
                       reads=[tQT[par][i // 4], tKT[par][ch]], writes=[tPSz[zi]])

            def stage_a1_act(h, i, s_):
                bp, bq = s_ % 2, s_ % 4
                nk = 128 * (i + 1)
                base, nch = zbase[s_]
                for ch in range(nch):
                    k0 = ch * 512
                    w_ = min(512, nk - k0)
                    zi = (base + ch) % 4
                    op(act, lambda zi=zi, k0=k0, w_=w_: nc.scalar.activation(
                        Rb[bp][:, k0:k0 + w_], PSz[zi][:, 0:w_], AF.Sigmoid, scale=-SB_SCALE),
                       reads=[tPSz[zi]], writes=[tRb[bp]])
                    op(act, lambda zi=zi, k0=k0, w_=w_: nc.scalar.activation(
                        Bb[bq][:, k0:k0 + w_], PSz[zi][:, 0:w_], AF.Sigmoid, scale=SB_SCALE),
                       reads=[tPSz[zi]], writes=[tBb[bq]])

            def stage_a2a(h, i, s_):
                bp, bq = s_ % 2, s_ % 4
                nk = 128 * (i + 1)
                d0 = i * 128
                op(pool, lambda: nc.gpsimd.affine_select(
                    out=Rb[bp][:, d0:d0 + 128], in_=Rb[bp][:, d0:d0 + 128], pattern=[[-1, 128]],
                    compare_op=ALU.is_gt, fill=FILL1, base=0, channel_multiplier=1),
                   reads=[tRb[bp]], writes=[tRb[bp]])
                op(pool, lambda: nc.gpsimd.affine_select(
                    out=Bb[bq][:, d0:d0 + 128], in_=Bb[bq][:, d0:d0 + 128], pattern=[[-1, 128]],
                    compare_op=ALU.is_gt, fill=FILL0, base=0, channel_multiplier=1),
                   reads=[tBb[bq]], writes=[tBb[bq]])
                op(pool, lambda: nc.gpsimd.memset(Pb[bp][:, nk + 1:nk + 2], 1.0), writes=[tPb[bp]])
                op(dve, lambda: nc.vector.tensor_tensor_scan(
                    out=Pb[bp][:, 1:nk + 1][:, ::-1], data0=Rb[bp][:, 0:nk][:, ::-1], data1=Rb[bp][:, 0:nk][:, ::-1],
                    initial=1.0, op0=ALU.mult, op1=ALU.min),
                   reads=[tRb[bp]], writes=[tPb[bp]])

            def stage_a2b(h, i, s_):
                bp, bq = s_ % 2, s_ % 4
                nk = 128 * (i + 1)
                op(dve, lambda: nc.vector.tensor_tensor(
                    Bb[bq][:, 0:nk], Bb[bq][:, 0:nk], Pb[bp][:, 2:nk + 2], ALU.mult),
                   reads=[tBb[bq], tPb[bp]], writes=[tBb[bq]])

            def stage_b(h, i, s_):
                par = h % 2
                bp, bq = s_ % 2, s_ % 4
                nb = i + 1
                for bk in range((nb + 7) // 8):
                    b0 = bk * 8
                    nbb = min(8, nb - b0)
                    pt, tpt = PSTr[bk % 2], tPSTr[bk % 2]
                    for b_ in range(nbb):
                        op(pe, lambda b_=b_, b0=b0, pt=pt: nc.tensor.transpose(
                            pt[:, b_ * 128:(b_ + 1) * 128], Bb[bq][:, (b0 + b_) * 128:(b0 + b_ + 1) * 128],
                            identb[:]),
                           reads=[tBb[bq], tC], writes=[tpt], sig=(b_ == nbb - 1))
                    op(act, lambda b0=b0, nbb=nbb, pt=pt: nc.scalar.copy(
                        ATb[bp][:, b0 * 128:(b0 + nbb) * 128], pt[:, 0:nbb * 128]),
                       reads=[tpt], writes=[tATb[bp]])

            def stage_b2(h, i, s_):
                par = h % 2
                bp, bq = s_ % 2, s_ % 4
                nb = i + 1
                c4 = i % 4
                for b_ in range(nb):
                    op(pe, lambda b_=b_: nc.tensor.matmul(
                        PSy[:, c4 * 128:(c4 + 1) * 128], lhsT=VH[par][:, b_, :],
                        rhs=ATb[bp][:, b_ * 128:(b_ + 1) * 128], start=(b_ == 0), stop=(b_ == nb - 1)),
                       reads=[tVH[par][b_ // 4], tATb[bp]], writes=[tPSy], sig=(b_ == nb - 1))
                if c4 == 3:
                    tg = i // 4
                    op(dve, lambda: nc.vector.tensor_tensor(
                        MT2[:, par, tg * 512:(tg + 1) * 512], PSy[:], GB[par][:, tg * 512:(tg + 1) * 512], ALU.mult),
                       reads=[tPSy, tGB[par][tg]], writes=[tMT2[par]])
                if par == 1 and i == NT - 1:
                    out_proj_accum(MT2, tMT2, WO2, tWO2, [PSp[0], PSy], [tPSp[0], tPSy])
                    if h + 1 < 8:
                        dma(pool, WO2[:], w_out[(h + 1) * 128:(h + 3) * 128, :].rearrange("(u p) n -> p u n", p=128),
                            writes=[tWO2])

            tiles = [(h, i) for h in range(8) for i in range(NT)]
            dma(pool, WO2[:], w_out[0:256, :].rearrange("(u p) n -> p u n", p=128), writes=[tWO2])
            load_head_w(0)
            load_head_w(1)
            for _ in emit_proj(0):
                pass
            NTL = len(tiles)
            stage_a1_pe(*tiles[0], 0)
            gen = None
            for s_ in range(NTL + 4):
                if s_ < NTL:
                    h, i = tiles[s_]
                    if 9 <= i <= 13 and h + 2 < 8:
                        load_head_w(h + 2, part=i - 9)
                    if i == 4 and h + 1 < 8:
                        gen = emit_proj(h + 1)
                        gen_n = 0
                    stage_a1_act(h, i, s_)
                if s_ + 1 < NTL:
                    stage_a1_pe(*tiles[s_ + 1], s_ + 1)
                if 0 <= s_ - 1 < NTL:
                    stage_a2a(*tiles[s_ - 1], s_ - 1)
                if 0 <= s_ - 2 < NTL:
                    stage_a2b(*tiles[s_ - 2], s_ - 2)
                if 0 <= s_ - 3 < NTL:
                    stage_b(*tiles[s_ - 3], s_ - 3)
                if 0 <= s_ - 4 < NTL:
                    stage_b2(*tiles[s_ - 4], s_ - 4)
                if gen is not None:
                    for _ in range(2 if gen_n < 12 else 1):
                        try:
                            next(gen)
                            gen_n += 1
                        except StopIteration:
                            gen = None
                            break
            K.barrier()

        if stop == 2:
            dump_x()
            return nc
        pmw = contextlib.ExitStack()
        WKV = sb(pmw, "WKV", [128, NKC, 1024], BF16)
        WQ = sb(pmw, "WQ", [128, NKC, 512], BF16)
        WO = sb(pmw, "WOm", [128, 4, D], BF16)
        MS = sb(pmw, "MS", [128, 2, D], F32)
        tWKV, tWQ, tWO, tMS = Tok(), Tok(), Tok(), Tok()
        dma(pool, WKV[:], w_mkv.rearrange("(c p) n -> p c n", p=128), writes=[tWKV])
        dma(pool, WQ[:], w_mq.rearrange("(c p) n -> p c n", p=128), writes=[tWQ])
        dma(pool, WO[:], w_mo.rearrange("(c p) n -> p c n", p=128), writes=[tWO])
        dma(sp, MS[:], mem_d.rearrange("(m p) n -> p m n", p=128), writes=[tMS])
        with contextlib.ExitStack() as pl1:
            PSt = [ps(pl1, "l1t%d" % k, [128, 512]) for k in range(2)]
            tPSt = toks(2)
            layer_norm_x(pl1, ln1_g, ln1_b, PSt, tPSt, dbg_out=dbg_d.get("d_x1"))
            for i in range(NT):
                op(dve, lambda i=i: nc.vector.tensor_scalar(X[:, i, :], X[:, i, :], ALPHA, None, ALU.mult),
                   reads=[tX[i]], writes=[tX[i]])
            K.barrier()

        if stop == 3:
            pmw.close()
            dump_x()
            return nc
        with contextlib.ExitStack() as p2:
            PSAf = ps(p2, "p2a", [128, 1024])
            PSA = [PSAf[:, 0:512], PSAf[:, 512:1024]]
            tPSA = toks(2)
            PSL = ps(p2, "p2l", [128, 1024])
            tPSL = Tok()
            PSTr = ps(p2, "p2tr", [128, 1024], BF16)
            tPSTr = Tok()
            PSO = ps(p2, "p2o", [128, 512])
            tPSO = Tok()
            PSM = [ps(p2, "p2m%d" % k, [128, 512]) for k in range(2)]
            tPSM = toks(2)
            MTm = sb(p2, "MTm", [128, NKC, 256], BF16)
            tMTm = Tok()
            for mt in range(2):
                for hb in range(2):
                    pb, tp = PSA[hb], tPSA[hb]
                    for c4 in range(4):
                        c = hb * 4 + c4
                        op(pe, lambda mt=mt, c=c, c4=c4, pb=pb: nc.tensor.transpose(
                            pb[:, c4 * 128:(c4 + 1) * 128], MS[:, mt, c * 128:(c + 1) * 128], identf[:]),
                           reads=[tMS, tC], writes=[tp], sig=(c4 == 3))
                    op(act, lambda mt=mt, hb=hb, pb=pb: nc.scalar.copy(
                        MTm[:, hb * 4:(hb + 1) * 4, mt * 128:(mt + 1) * 128],
                        pb[:].rearrange("p (c t) -> p c t", c=4)),
                       reads=[tp], writes=[tMTm])
            KM = sb(p2, "KM", [128, 4, 256], BF16)
            VM = sb(p2, "VM", [128, 2, 512], BF16)
            QM = sb(p2, "QM", [128, 4, S], BF16)
            tKM, tVM, tQM = Tok(), Tok(), toks(4)
            for h in range(4):
                pb, tp = PSA[h % 2], tPSA[h % 2]
                for kc in range(NKC):
                    op(pe, lambda h=h, kc=kc, pb=pb: nc.tensor.matmul(
                        pb[:, 0:256], lhsT=WKV[:, kc, h * 128:(h + 1) * 128], rhs=MTm[:, kc, :],
                        start=(kc == 0), stop=(kc == NKC - 1)),
                       reads=[tWKV, tMTm], writes=[tp], sig=(kc == NKC - 1))
                op(act, lambda h=h, pb=pb: nc.scalar.copy(KM[:, h, :], pb[:, 0:256]), reads=[tp], writes=[tKM])
            for mt in range(2):
                pb, tp = PSA[mt % 2], tPSA[mt % 2]
                for kc in range(NKC):
                    op(pe, lambda mt=mt, kc=kc, pb=pb: nc.tensor.matmul(
                        pb[:], lhsT=MTm[:, kc, mt * 128:(mt + 1) * 128], rhs=WKV[:, kc, 512:1024],
                        start=(kc == 0), stop=(kc == NKC - 1)),
                       reads=[tWKV, tMTm], writes=[tp], sig=(kc == NKC - 1))
                op(act, lambda mt=mt, pb=pb: nc.scalar.copy(VM[:, mt, :], pb[:]), reads=[tp], writes=[tVM])
            for h in range(4):
                for tg in range(4):
                    pb, tp = PSA[tg % 2], tPSA[tg % 2]
                    for kc in range(NKC):
                        op(pe, lambda h=h, tg=tg, kc=kc, pb=pb: nc.tensor.matmul(
                            pb[:], lhsT=WQ[:, kc, h * 128:(h + 1) * 128], rhs=XT[:, kc, tg * 512:(tg + 1) * 512],
                            start=(kc == 0), stop=(kc == NKC - 1)),
                           reads=[tWQ] + tXT[tg * 4:(tg + 1) * 4], writes=[tp], sig=(kc == NKC - 1))
                    op(act, lambda h=h, tg=tg, pb=pb: nc.scalar.copy(QM[:, h, tg * 512:(tg + 1) * 512], pb[:]),
                       reads=[tp], writes=[tQM[tg]])
            MX = [sb(p2, "MX%d" % k, [128, 4], F32) for k in range(2)]
            NMX = [sb(p2, "NMX%d" % k, [128, 4], F32) for k in range(2)]
            SS = [sb(p2, "SS%d" % k, [128, 4], F32) for k in range(2)]
            RSS = [sb(p2, "RSS%d" % k, [128, 4], F32) for k in range(2)]
            Pf = [sb(p2, "Pf%d" % k, [128, 4, 256], F32) for k in range(2)]
            Pn = [sb(p2, "Pn%d" % k, [128, 4, 256], BF16) for k in range(2)]
            PTm = [sb(p2, "PTm%d" % k, [128, 8, 128], BF16) for k in range(2)]
            OTm = [sb(p2, "OTm%d" % k, [128, 4, 128], BF16) for k in range(2)]
            tMX, tNMX, tSS, tRSS, tPf, tPn, tPTm, tOTm = (toks(2) for _ in range(8))
            PSL2 = [PSL, PSAf]
            tPSL2 = [tPSL, Tok()]

            def m_s1(i):
                q = i % 2
                psl, tpsl = PSL2[q], tPSL2[q]
                for h in range(4):
                    op(pe, lambda h=h: nc.tensor.matmul(
                        psl[:, h * 256:(h + 1) * 256], lhsT=QM[:, h, i * 128:(i + 1) * 128], rhs=KM[:, h, :],
                        start=True, stop=True),
                       reads=[tQM[i // 4], tKM], writes=[tpsl], sig=(h == 3))
                op(dve, lambda: nc.vector.tensor_reduce(MX[q][:], psl[:].rearrange("p (h m) -> p h m", h=4),
                                                        AX.X, ALU.max),
                   reads=[tpsl], writes=[tMX[q]])
                op(dve, lambda: nc.vector.tensor_scalar(NMX[q][:], MX[q][:], -MEM_SCALE, None, ALU.mult),
                   reads=[tMX[q]], writes=[tNMX[q]])
                for h in range(4):
                    op(act, lambda h=h: nc.scalar.activation(
                        Pf[q][:, h, :], psl[:, h * 256:(h + 1) * 256], AF.Exp, bias=NMX[q][:, h:h + 1],
                        scale=MEM_SCALE, accum_out=SS[q][:, h:h + 1]),
                       reads=[tpsl, tNMX[q]], writes=[tPf[q], tSS[q]])
                op(dve, lambda: nc.vector.reciprocal(RSS[q][:], SS[q][:]), reads=[tSS[q]], writes=[tRSS[q]])
                for h in range(4):
                    op(dve, lambda h=h: nc.vector.tensor_scalar(Pn[q][:, h, :], Pf[q][:, h, :], RSS[q][:, h:h + 1],
                                                                None, ALU.mult),
                       reads=[tPf[q], tRSS[q]], writes=[tPn[q]])

            def m_s2a(i):
                q = i % 2
                for h in range(4):
                    for mt in range(2):
                        j = h * 2 + mt
                        op(pe, lambda h=h, mt=mt, j=j: nc.tensor.transpose(
                            PSTr[:, j * 128:(j + 1) * 128], Pn[q][:, h, mt * 128:(mt + 1) * 128], identb[:]),
                           reads=[tPn[q], tC], writes=[tPSTr], sig=(j == 7))
                op(act, lambda: nc.scalar.copy(PTm[q][:], PSTr[:].rearrange("p (j t) -> p j t", j=8)),
                   reads=[tPSTr], writes=[tPTm[q]])

            def m_s2b(i):
                q = i % 2
                for h in range(4):
                    for mt in range(2):
                        op(pe, lambda h=h, mt=mt: nc.tensor.matmul(
                            PSO[:, h * 128:(h + 1) * 128], lhsT=VM[:, mt, h * 128:(h + 1) * 128],
                            rhs=PTm[q][:, h * 2 + mt, :], start=(mt == 0), stop=(mt == 1)),
                           reads=[tVM, tPTm[q]], writes=[tPSO], sig=(h == 3 and mt == 1))
                op(act, lambda: nc.scalar.copy(OTm[q][:], PSO[:].rearrange("p (h t) -> p h t", h=4)),
                   reads=[tPSO], writes=[tOTm[q]])

            def m_s3(i):
                q = i % 2
                for hf in range(2):
                    pb, tp = PSM[hf], tPSM[hf]
                    for h in range(4):
                        op(pe, lambda h=h, hf=hf, pb=pb: nc.tensor.matmul(
                            pb[:], lhsT=OTm[q][:, h, :], rhs=WO[:, h, hf * 512:(hf + 1) * 512],
                            start=(h == 0), stop=(h == 3)),
                           reads=[tOTm[q], tWO], writes=[tp], sig=(h == 3))
                    op(dve, lambda hf=hf, pb=pb: nc.vector.tensor_tensor(
                        X[:, i, hf * 512:(hf + 1) * 512], X[:, i, hf * 512:(hf + 1) * 512], pb[:], ALU.add),
                       reads=[tp, tX[i]], writes=[tX[i]])

            K.barrier()
            for s_ in range(NT + 3):
                if s_ < NT:
                    m_s1(s_)
                if 0 <= s_ - 1 < NT:
                    m_s2a(s_ - 1)
                if 0 <= s_ - 2 < NT:
                    m_s2b(s_ - 2)
                if 0 <= s_ - 3 < NT:
                    m_s3(s_ - 3)
            K.barrier()

        pmw.close()
        if stop == 4:
            dump_x()
            return nc
        with contextlib.ExitStack() as p3:
            pxb = contextlib.ExitStack()
            XB = sb(pxb, "XB", [128, NT, D], BF16)
            sWGs = sb(pxb, "sWGs", [128, NKC, 256], F32)
            sWUs = sb(pxb, "sWUs", [128, NKC, 256], F32)
            sWDs = sb(pxb, "sWDs", [128, 2, D], F32)
            tsW = toks(3)
            dma(sp, sWGs[:], w_sg.rearrange("(c p) n -> p c n", p=128), writes=[tsW[0]])
            dma(sp, sWUs[:], w_su.rearrange("(c p) n -> p c n", p=128), writes=[tsW[1]])
            dma(sp, sWDs[:], w_sd.rearrange("(c p) n -> p c n", p=128), writes=[tsW[2]])
            with contextlib.ExitStack() as p3a:
                PSt = [ps(p3a, "l2t%d" % k, [128, 512]) for k in range(2)]
                tPSt = toks(2)
                PSr = ps(p3a, "l2r", [128, 512])
                tPSr = Tok()
                PSpos = ps(p3a, "l2p", [128, 512])
                tPSpos = Tok()
                WR = sb(p3a, "WR", [128, NKC, NEXP], F32)
                RB = sb(p3a, "RB", [128, NEXP], F32)
                tWR, tRB = Tok(), Tok()
                dma(sp, WR[:], w_r.rearrange("(c p) n -> p c n", p=128), writes=[tWR])
                dma(sp, RB[:], r_b.partition_broadcast(128), writes=[tRB])
                tXB = toks(NT)
                MKB = sb(p3a, "MKB", [128, NT, NEXP], BF16)
                tMKB = toks(NT)
                LT = sb(p3a, "LT", [128, 128], BF16)
                ONESM = sb(p3a, "ONESM", [128, 128], BF16)
                EOFF = sb(p3a, "EOFF", [128, NEXP], F32)
                EOFFI = sb(p3a, "EOFFI", [128, NEXP], mybir.dt.int32)
                tK = Tok()
                op(pool, lambda: nc.gpsimd.affine_select(out=LT[:], in_=onesf[:], pattern=[[1, 128]],
                                                         compare_op=ALU.is_gt, fill=FILL0, base=0,
                                                         channel_multiplier=-1), reads=[tC], writes=[tK])
                op(dve, lambda: nc.vector.memset(ONESM[:], 1.0), writes=[tK])
                op(pool, lambda: nc.gpsimd.iota(EOFFI[:], pattern=[[CAP, NEXP]], base=0, channel_multiplier=0),
                   writes=[tK])
                op(dve, lambda: nc.vector.tensor_copy(EOFF[:], EOFFI[:]), reads=[tK], writes=[tK])
                SCA = sb(p3a, "SCA", [128, NT, NEXP], F32)
                tSCA = toks(NT)
                NR = 4
                PSposL = [PSpos] + [ps(p3a, "l2p%d" % k, [128, 512]) for k in range(NR - 1)]
                tPSposL = [tPSpos] + toks(NR - 1)

                def mkset(r):
                    d = {}
                    for nm, shp in (("SEL", [128, NEXP]), ("SELM", [128, NEXP]), ("T8", [128, 8, 8]), ("GS", [128, 8]),
                                    ("G8", [128, 8]), ("GM", [128, 8]), ("E8", [128, 8]), ("MK", [128, NEXP]),
                                    ("WGt", [128, NEXP]), ("SM", [128, 1]), ("GT_", [128, NEXP]),
                                    ("NMK", [128, NEXP]), ("SLOT", [128, NEXP]), ("N8", [128, 8]),
                                    ("SL8f", [128, 8]), ("JK", [128, NEXP])):
                        d[nm] = sb(p3a, "%s_%d" % (nm, r), shp, F32)
                    d["t"] = Tok()
                    return d
                RS_ = [mkset(r) for r in range(NR)]
                tXF = Tok()
                WRH = sb(p3a, "WRH", [128, NKC, NEXP], BF16)
                WRL = sb(p3a, "WRL", [128, NKC, NEXP], BF16)
                XL = sb(p3a, "XL", [128, NKC, 128], BF16)
                tWRH = Tok()
                op(dve, lambda: nc.vector.tensor_copy(WRH[:], WR[:]), reads=[tWR], writes=[tWRH])
                op(dve, lambda: nc.vector.tensor_tensor(WRL[:], WR[:], WRH[:], ALU.subtract),
                   reads=[tWR, tWRH], writes=[tWRH])

                def router(i, hb, pb, tp):
                    op(dve, lambda: nc.vector.tensor_tensor(
                        XL[:, hb * 4:(hb + 1) * 4, :], pb[:].rearrange("p (c t) -> p c t", c=4),
                        XT[:, hb * 4:(hb + 1) * 4, i * 128:(i + 1) * 128], ALU.subtract),
                       reads=[tp, tXT[i]], writes=[tXF])
                    if hb == 0:
                        op(act, lambda: nc.scalar.copy(XB[:, i, :], X[:, i, :]), reads=[tX[i]], writes=[tXB[i]])
                        return
                    n = 0
                    for (a_hi, wt) in ((True, WRH), (False, WRH), (True, WRL)):
                        for kc in range(NKC):
                            lhs = XT[:, kc, i * 128:(i + 1) * 128] if a_hi else XL[:, kc, :]
                            op(pe, lambda lhs=lhs, wt=wt, kc=kc, n=n: nc.tensor.matmul(
                                PSr[:, 0:NEXP], lhsT=lhs, rhs=wt[:, kc, :], start=(n == 0), stop=(n == 23)),
                               reads=[tXF, tXT[i], tWRH], writes=[tPSr], sig=(n == 23))
                            n += 1
                    op(act, lambda: nc.scalar.activation(SCA[:, i, :], PSr[:, 0:NEXP], AF.Sigmoid),
                       reads=[tPSr], writes=[tSCA[i]])

                def route_chain(i, r):
                    d = RS_[r]
                    SEL, SELM, T8, GS, G8, GM, E8, MK = (d[k] for k in ("SEL", "SELM", "T8", "GS", "G8", "GM", "E8", "MK"))
                    WGt, SM, GT_, NMK, SLOT, N8, SL8f, JK = (d[k] for k in ("WGt", "SM", "GT_", "NMK", "SLOT", "N8", "SL8f", "JK"))
                    tr = d["t"]
                    SC = SCA[:, i, :]
                    V = nc.vector
                    R = dict(reads=[tr], writes=[tr])
                    op(dve, lambda: V.tensor_tensor(SEL[:], SC, RB[:], ALU.add), reads=[tSCA[i], tRB], writes=[tr])
                    yield
                    for g in range(8):
                        op(dve, lambda g=g: V.max(out=T8[:, g, :], in_=SEL[:, g * 8:(g + 1) * 8]), **R)
                        yield
                    op(dve, lambda: V.tensor_tensor(GS[:], T8[:, :, 0], T8[:, :, 1], ALU.add), **R)
                    yield
                    op(dve, lambda: V.max(out=G8[:], in_=GS[:]), **R)
                    yield
                    op(dve, lambda: V.tensor_scalar(GM[:], GS[:], G8[:, 3:4], None, ALU.is_ge), **R)
                    yield
                    op(dve, lambda: V.tensor_scalar(GM[:], GM[:], 1.0, 1.0e4, ALU.subtract, ALU.mult), **R)
                    yield
                    for g in range(8):
                        op(dve, lambda g=g: V.tensor_scalar(SELM[:, g * 8:(g + 1) * 8], SEL[:, g * 8:(g + 1) * 8],
                                                            GM[:, g:g + 1], None, ALU.add), **R)
                        yield
                    op(dve, lambda: V.max(out=E8[:], in_=SELM[:]), **R)
                    yield
                    op(dve, lambda: V.tensor_scalar(MK[:], SELM[:], E8[:, 7:8], None, ALU.is_ge), **R)
                    yield
                    op(dve, lambda: V.tensor_copy(MKB[:, i, :], MK[:]), reads=[tr], writes=[tMKB[i]])
                    yield
                    op(dve, lambda: V.tensor_tensor(WGt[:], SC, MK[:], ALU.mult), **R)
                    yield
                    op(dve, lambda: V.tensor_reduce(SM[:], WGt[:], AX.X, ALU.add), **R)
                    yield
                    op(dve, lambda: V.reciprocal(SM[:], SM[:]), **R)
                    yield
                    op(dve, lambda: V.tensor_scalar(GT_[:], WGt[:], SM[:, 0:1], ROUTED_SCALE, ALU.mult, ALU.mult), **R)
                    yield
                    pp, tpp = PSposL[r], tPSposL[r]
                    op(pe, lambda: nc.tensor.matmul(pp[:, 0:NEXP], lhsT=LT[:], rhs=MKB[:, i, :],
                                                    start=True, stop=(i == 0)),
                       reads=[tK, tMKB[i]], writes=[tpp], sig=(i == 0))
                    for i2 in range(i):
                        op(pe, lambda i2=i2: nc.tensor.matmul(pp[:, 0:NEXP], lhsT=ONESM[:], rhs=MKB[:, i2, :],
                                                              start=False, stop=(i2 == i - 1)),
                           reads=[tK, tMKB[i2]], writes=[tpp], sig=(i2 == i - 1))
                    op(dve, lambda: V.tensor_scalar(NMK[:], MK[:], 1.0, -1.0e6, ALU.subtract, ALU.mult), **R)
                    yield
                    op(dve, lambda: V.tensor_tensor(SLOT[:], pp[:, 0:NEXP], EOFF[:], ALU.add),
                       reads=[tpp, tK, tr], writes=[tr])
                    yield
                    op(dve, lambda: V.tensor_tensor(SLOT[:], SLOT[:], NMK[:], ALU.add), **R)
                    yield
                    op(dve, lambda: V.tensor_scalar(SLOT[:], SLOT[:], -1.0, None, ALU.mult), **R)
                    yield
                    op(dve, lambda: V.max(out=N8[:], in_=SLOT[:]), **R)
                    yield
                    op(dve, lambda: V.tensor_scalar(SL8f[:], N8[:], -1.0, None, ALU.mult), **R)
                    yield
                    op(dve, lambda: V.tensor_copy(SL8I[:, i, :], SL8f[:]), reads=[tr], writes=[tSL[i]])
                    yield
                    for k in range(8):
                        op(dve, lambda k=k: V.scalar_tensor_tensor(
                            out=JK[:], in0=SLOT[:], scalar=N8[:, k:k + 1], in1=GT_[:], op0=ALU.is_equal,
                            op1=ALU.mult, accum_out=G8v[:, i, k:k + 1]),
                           reads=[tr], writes=[tr, tSL[i]])
                        yield
                    for k in range(8):
                        dma(pool, None, None, reads=[tXB[i], tSL[i]] + tZ, writes=[Tok()],
                            fn=lambda k=k: nc.gpsimd.indirect_dma_start(
                                out=XG[:, :], out_offset=bass.IndirectOffsetOnAxis(ap=SL8I[:, i, k:k + 1], axis=0),
                                in_=XB[:, i, :], in_offset=None))
                    yield

                layer_norm_x(p3a, ln2_g, ln2_b, PSt, tPSt, per_tile=router, dbg_out=dbg_d.get("d_x2"))
                for base_ in range(0, NT, NR):
                    gens = [route_chain(base_ + r, r) for r in range(NR)]
                    while gens:
                        for g_ in list(gens):
                            try:
                                next(g_)
                            except StopIteration:
                                gens.remove(g_)
                for i in range(NT):
                    op(dve, lambda i=i: nc.vector.tensor_scalar(X[:, i, :], X[:, i, :], ALPHA, None, ALU.mult),
                       reads=[tX[i]], writes=[tX[i]])
                K.barrier(skip_pool_dma=True)

            if stop == 5:
                K.barrier()
                pxb.close()
                dump_x()
                return nc
            with contextlib.ExitStack() as p3s:
                PSg = [ps(p3s, "p3g%d" % k, [128, 512]) for k in range(2)]
                PSu = [ps(p3s, "p3u%d" % k, [128, 512]) for k in range(2)]
                PSd = [ps(p3s, "p3d%d" % k, [128, 512]) for k in range(4)]
                tPSg, tPSu, tPSd = toks(2), toks(2), toks(4)
                WG = sb(p3s, "sWG", [128, NKC, 256], BF16)
                WU = sb(p3s, "sWU", [128, NKC, 256], BF16)
                WD = sb(p3s, "sWD", [128, 2, D], BF16)
                tWG, tWU, tWD = Tok(), Tok(), Tok()
                HT = sb(p3s, "sHT", [128, 2, S], BF16)
                tHT = toks(4)
                SG = [sb(p3s, "sSG%d" % k, [128, 512], BF16) for k in range(2)]
                tSG = toks(2)
                op(act, lambda: nc.scalar.copy(WG[:], sWGs[:]), reads=[tsW[0]], writes=[tWG])
                op(dve, lambda: nc.vector.tensor_copy(WU[:], sWUs[:]), reads=[tsW[1]], writes=[tWU])
                op(act, lambda: nc.scalar.copy(WD[:], sWDs[:]), reads=[tsW[2]], writes=[tWD])
                cnt = 0
                for tg in range(4):
                    for hc in range(2):
                        k = cnt % 2
                        cnt += 1
                        for kc in range(NKC):
                            op(pe, lambda kc=kc, tg=tg, hc=hc, k=k: nc.tensor.matmul(
                                PSg[k][:], lhsT=WG[:, kc, hc * 128:(hc + 1) * 128],
                                rhs=XT[:, kc, tg * 512:(tg + 1) * 512], start=(kc == 0), stop=(kc == NKC - 1)),
                               reads=[tWG] + tXT[tg * 4:(tg + 1) * 4], writes=[tPSg[k]], sig=(kc == NKC - 1))
                        for kc in range(NKC):
                            op(pe, lambda kc=kc, tg=tg, hc=hc, k=k: nc.tensor.matmul(
                                PSu[k][:], lhsT=WU[:, kc, hc * 128:(hc + 1) * 128],
                                rhs=XT[:, kc, tg * 512:(tg + 1) * 512], start=(kc == 0), stop=(kc == NKC - 1)),
                               reads=[tWU] + tXT[tg * 4:(tg + 1) * 4], writes=[tPSu[k]], sig=(kc == NKC - 1))
                        op(act, lambda k=k: nc.scalar.activation(SG[k][:], PSg[k][:], AF.Silu),
                           reads=[tPSg[k]], writes=[tSG[k]])
                        op(dve, lambda k=k, tg=tg, hc=hc: nc.vector.tensor_tensor(
                            HT[:, hc, tg * 512:(tg + 1) * 512], SG[k][:], PSu[k][:], ALU.mult),
                           reads=[tSG[k], tPSu[k]], writes=[tHT[tg]])
                for i in range(NT):
                    for hf in range(2):
                        k = (i * 2 + hf) % 4
                        for hc in range(2):
                            op(pe, lambda i=i, hf=hf, hc=hc, k=k: nc.tensor.matmul(
                                PSd[k][:], lhsT=HT[:, hc, i * 128:(i + 1) * 128],
                                rhs=WD[:, hc, hf * 512:(hf + 1) * 512], start=(hc == 0), stop=(hc == 1)),
                               reads=[tHT[i // 4], tWD], writes=[tPSd[k]], sig=(hc == 1))
                        op(dve, lambda i=i, hf=hf, k=k: nc.vector.tensor_tensor(
                            X[:, i, hf * 512:(hf + 1) * 512], X[:, i, hf * 512:(hf + 1) * 512], PSd[k][:], ALU.add),
                           reads=[tPSd[k], tX[i]], writes=[tX[i]])
                K.barrier()

            pxb.close()
            stXT.close()
            with contextlib.ExitStack() as p3b:
                PSTr = [ps(p3b, "p3t%d" % k, [128, 1024], BF16) for k in range(2)]
                PSg = [ps(p3b, "p3g%d" % k, [128, 512]) for k in range(2)]
                PSu = [ps(p3b, "p3u%d" % k, [128, 512]) for k in range(2)]
                PSd = [ps(p3b, "p3d%d" % k, [128, 512]) for k in range(2)]
                tPSTr, tPSg, tPSu, tPSd = toks(2), toks(2), toks(2), toks(2)
                NJ = CAP // 128
                XS = [sb(p3b, "XS%d" % k, [128, NJ, D], BF16) for k in range(2)]
                XGT = [sb(p3b, "XGT%d" % k, [128, NKC, CAP], BF16) for k in range(2)]
                WGs = [sb(p3b, "WGs%d" % k, [128, NKC, 256], F32) for k in range(3)]
                WUs = [sb(p3b, "WUs%d" % k, [128, NKC, 256], F32) for k in range(3)]
                WDs = [sb(p3b, "WDs%d" % k, [128, 2, D], F32) for k in range(3)]
                WG = [sb(p3b, "WG%d" % k, [128, NKC, 256], BF16) for k in range(2)]
                WU = [sb(p3b, "WU%d" % k, [128, NKC, 256], BF16) for k in range(2)]
                WD = [sb(p3b, "WD%d" % k, [128, 2, D], BF16) for k in range(2)]
                HT = [sb(p3b, "HT%d" % k, [128, 2, CAP], BF16) for k in range(2)]
                SG1 = sb(p3b, "SG", [128, CAP], BF16)
                SG = [SG1, SG1]
                YS1 = sb(p3b, "YS", [128, NJ, D], BF16)
                YS = [YS1, YS1]
                tXS, tXGT, tWG, tWU, tWD, tHT = (toks(2) for _ in range(6))
                tSG1, tYS1 = Tok(), Tok()
                tSG, tYS = [tSG1, tSG1], [tYS1, tYS1]
                tWGs, tWUs, tWDs = toks(3), toks(3), toks(3)

                def prefetch_xs(e):
                    par = e % 2
                    dma(sp, XS[par][:], XG[e * CAP:(e + 1) * CAP, :].rearrange("(j p) n -> p j n", p=128),
                        writes=[tXS[par]])

                def prefetch_w(e):
                    p3_ = e % 3
                    dma(sp, WGs[p3_][:], w_eg[e].rearrange("(c p) n -> p c n", p=128), writes=[tWGs[p3_]])
                    dma(sp, WUs[p3_][:], w_eu[e].rearrange("(c p) n -> p c n", p=128), writes=[tWUs[p3_]])
                    dma(sp, WDs[p3_][:], w_ed[e].rearrange("(c p) n -> p c n", p=128), writes=[tWDs[p3_]])

                def cast_w(e):
                    p2_, p3_ = e % 2, e % 3
                    op(act, lambda: nc.scalar.copy(WG[p2_][:], WGs[p3_][:]), reads=[tWGs[p3_]], writes=[tWG[p2_]])
                    op(dve, lambda: nc.vector.tensor_copy(WU[p2_][:], WUs[p3_][:]), reads=[tWUs[p3_]],
                       writes=[tWU[p2_]])
                    op(pool, lambda: nc.gpsimd.tensor_copy(WD[p2_][:], WDs[p3_][:]), reads=[tWDs[p3_]],
                       writes=[tWD[p2_]])

                ev = [0]

                def transposes(e):
                    par = e % 2
                    for j in range(NJ):
                        pt, tpt = PSTr[j % 2], tPSTr[j % 2]
                        for c in range(NKC):
                            op(pe, lambda j=j, c=c, pt=pt: nc.tensor.transpose(
                                pt[:, c * 128:(c + 1) * 128], XS[par][:, j, c * 128:(c + 1) * 128], identb[:]),
                               reads=[tXS[par], tC], writes=[tpt], sig=(c == NKC - 1))
                        ev[0] += 1
                        if ev[0] % 2 == 0:
                            op(act, lambda j=j, pt=pt: nc.scalar.copy(
                                XGT[par][:, :, j * 128:(j + 1) * 128], pt[:].rearrange("p (c t) -> p c t", c=NKC)),
                               reads=[tpt], writes=[tXGT[par]])
                        else:
                            op(dve, lambda j=j, pt=pt: nc.vector.tensor_copy(
                                XGT[par][:, :, j * 128:(j + 1) * 128], pt[:].rearrange("p (c t) -> p c t", c=NKC)),
                               reads=[tpt], writes=[tXGT[par]])

                prefetch_xs(0)
                prefetch_w(0)
                prefetch_w(1)
                prefetch_xs(1)
                cast_w(0)
                transposes(0)
                for e in range(NEXP):
                    par = e % 2
                    if e + 2 < NEXP:
                        prefetch_xs(e + 2)
                        prefetch_w(e + 2)
                    if e + 1 < NEXP:
                        cast_w(e + 1)
                    for hc in range(2):
                        k = hc
                        for kc in range(NKC):
                            op(pe, lambda kc=kc, hc=hc, k=k: nc.tensor.matmul(
                                PSg[k][:], lhsT=WG[par][:, kc, hc * 128:(hc + 1) * 128], rhs=XGT[par][:, kc, :],
                                start=(kc == 0), stop=(kc == NKC - 1)),
                               reads=[tWG[par], tXGT[par]], writes=[tPSg[k]], sig=(kc == NKC - 1))
                        for kc in range(NKC):
                            op(pe, lambda kc=kc, hc=hc, k=k: nc.tensor.matmul(
                                PSu[k][:], lhsT=WU[par][:, kc, hc * 128:(hc + 1) * 128], rhs=XGT[par][:, kc, :],
                                start=(kc == 0), stop=(kc == NKC - 1)),
                               reads=[tWU[par], tXGT[par]], writes=[tPSu[k]], sig=(kc == NKC - 1))
                        op(act, lambda k=k: nc.scalar.activation(SG[k][:], PSg[k][:], AF.Silu),
                           reads=[tPSg[k]], writes=[tSG[k]])
                        op(dve, lambda k=k, hc=hc: nc.vector.tensor_tensor(
                            HT[par][:, hc, :], SG[k][:], PSu[k][:], ALU.mult),
                           reads=[tSG[k], tPSu[k]], writes=[tHT[par]])
                    if e + 1 < NEXP:
                        transposes(e + 1)
                    for j in range(NJ):
                        for hf in range(2):
                            k = (j * 2 + hf) % 2
                            for hc in range(2):
                                op(pe, lambda j=j, hf=hf, hc=hc, k=k: nc.tensor.matmul(
                                    PSd[k][:], lhsT=HT[par][:, hc, j * 128:(j + 1) * 128],
                                    rhs=WD[par][:, hc, hf * 512:(hf + 1) * 512], start=(hc == 0), stop=(hc == 1)),
                                   reads=[tHT[par], tWD[par]], writes=[tPSd[k]], sig=(hc == 1))
                            ev[0] += 1
                            if ev[0] % 2 == 0:
                                op(act, lambda j=j, hf=hf, k=k: nc.scalar.copy(
                                    YS[par][:, j, hf * 512:(hf + 1) * 512], PSd[k][:]),
                                   reads=[tPSd[k]], writes=[tYS[par]])
                            else:
                                op(dve, lambda j=j, hf=hf, k=k: nc.vector.tensor_copy(
                                    YS[par][:, j, hf * 512:(hf + 1) * 512], PSd[k][:]),
                                   reads=[tPSd[k]], writes=[tYS[par]])
                    dma(pool, YG[e * CAP:(e + 1) * CAP, :].rearrange("(j p) n -> p j n", p=128), YS[par][:],
                        reads=[tYS[par]])
                K.barrier()

            with contextlib.ExitStack() as p3c:
                NB = 6
                YR = [sb(p3c, "YR%d" % k, [128, D], BF16) for k in range(NB)]
                tYR = toks(NB)
                n = 0
                for i in range(NT):
                    for k in range(8):
                        bfi = n % NB
                        n += 1
                        dma(pool, None, None, reads=[tSL[i]], writes=[tYR[bfi]],
                            fn=lambda i=i, k=k, bfi=bfi: nc.gpsimd.indirect_dma_start(
                                out=YR[bfi][:], out_offset=None, in_=YG[:, :],
                                in_offset=bass.IndirectOffsetOnAxis(ap=SL8I[:, i, k:k + 1], axis=0)))
                        op(dve, lambda i=i, k=k, bfi=bfi: nc.vector.scalar_tensor_tensor(
                            out=X[:, i, :], in0=YR[bfi][:], scalar=G8v[:, i, k:k + 1], in1=X[:, i, :],
                            op0=ALU.mult, op1=ALU.add),
                           reads=[tYR[bfi], tSL[i], tX[i]], writes=[tX[i]])
                K.barrier()

        with contextlib.ExitStack() as pl3:
            layer_norm_x(pl3, ln3_g, ln3_b, None, None, want_xt=False, out_dram=out_d)
            K.barrier()
    return nc


_NC_CACHE = {}


def _prep_inputs(inputs, b):
    f = lambda a: np.ascontiguousarray(np.asarray(a, dtype=np.float32))
    m = {
        "x": f(inputs["x"][b]),
        "mem": f(inputs["mem"][b]),
        "ln_in_g": f(inputs["ln_in_g"]).reshape(1, D),
        "ln_in_b": f(inputs["ln_in_b"]).reshape(1, D),
        "w_in": f(inputs["w_in"][0]),
        "b_in": f(inputs["b_in"][0]).reshape(56, 128),
        "ln_v_g": f(inputs["ln_v_g"][0]).reshape(1, D),
        "ln_v_b": f(inputs["ln_v_b"][0]).reshape(1, D),
        "w_spatial": f(inputs["w_spatial"][0]),
        "b_spatial": f(inputs["b_spatial"][0]),
        "w_out": f(inputs["w_out"][0]),
        "ln1_g": f(inputs["ln1_g"][0]).reshape(1, D),
        "ln1_b": f(inputs["ln1_b"][0]).reshape(1, D),
        "w_mem_q": f(inputs["w_mem_q"][0]),
        "w_mem_kv": f(inputs["w_mem_kv"][0]),
        "w_mem_o": f(inputs["w_mem_o"][0]),
        "ln2_g": f(inputs["ln2_g"][0]).reshape(1, D),
        "ln2_b": f(inputs["ln2_b"][0]).reshape(1, D),
        "w_router": f(inputs["w_router"][0]),
        "router_bias": f(inputs["router_bias"][0]).reshape(1, NEXP),
        "w_exp_gate": f(inputs["w_exp_gate"][0]),
        "w_exp_up": f(inputs["w_exp_up"][0]),
        "w_exp_down": f(inputs["w_exp_down"][0]),
        "w_sh_gate": f(inputs["w_sh_gate"][0]),
        "w_sh_up": f(inputs["w_sh_up"][0]),
        "w_sh_down": f(inputs["w_sh_down"][0]),
        "ln3_g": f(inputs["ln3_g"][0]).reshape(1, D),
        "ln3_b": f(inputs["ln3_b"][0]).reshape(1, D),
    }
    return m


def kernel(**inputs):
    dbg = bool(os.environ.get("MK_DEBUG"))
    if dbg not in _NC_CACHE:
        _NC_CACHE[dbg] = build(dbg, int(os.environ.get("MK_STOP", "99")))
    nc = _NC_CACHE[dbg]
    shared = _prep_inputs(inputs, 0)
    in_maps = []
    for b in range(8):
        m = dict(shared)
        m["x"] = np.ascontiguousarray(np.asarray(inputs["x"][b], dtype=np.float32))
        m["mem"] = np.ascontiguousarray(np.asarray(inputs["mem"][b], dtype=np.float32))
        in_maps.append(m)
    res = run_bass_kernel_spmd(nc, in_maps, core_ids=list(range(8)))
    out = np.stack([np.asarray(r["out"], dtype=np.float32) for r in res.results], axis=0)
    if dbg:
        kernel.debug = [{k: np.asarray(v) for k, v in r.items()} for r in res.results]
    return out
```

```python
import os
import contextlib
import numpy as np
import ml_dtypes
import concourse.bass as bass
import concourse.mybir as mybir
from concourse.bass_utils import run_bass_kernel_spmd

F32 = mybir.dt.float32
BF16 = mybir.dt.bfloat16
AF = mybir.ActivationFunctionType
ALU = mybir.AluOpType
AX = mybir.AxisListType

S = 2048
D = 1024
NT = 16
NKC = 8
ALPHA = 2.0 ** 0.25
LN_EPS = 1e-5
SB_SCALE = 128.0 ** -0.5
MEM_SCALE = 128.0 ** -0.5
NEXP = 64
ROUTED_SCALE = 2.5
CAP = 512
NSLOT = NEXP * CAP
U32 = mybir.dt.uint32


class Tok:
    __slots__ = ("w", "r")

    def __init__(self):
        self.w = None
        self.r = {}


def toks(n):
    return [Tok() for _ in range(n)]


class Eng:
    def __init__(self, name, h, sem):
        self.name = name
        self.h = h
        self.sem = sem
        self.cnt = 0
        self.known = {}
        self.pool = []
        self.dma_i = 0


class Sched:
    def __init__(self, nc, st, ndma=12):
        self.nc = nc
        mk = lambda n: st.enter_context(nc.semaphore(n))
        self.pe = Eng("pe", nc.tensor, mk("s_pe"))
        self.act = Eng("act", nc.scalar, mk("s_act"))
        self.dve = Eng("dve", nc.vector, mk("s_dve"))
        self.pool = Eng("pool", nc.gpsimd, mk("s_pool"))
        self.sp = Eng("sp", nc.sync, mk("s_sp"))
        self.engs = [self.pe, self.act, self.dve, self.pool, self.sp]
        for q in (self.sp, self.pool, self.act):
            q.pool = [[mk("d_%s_%d" % (q.name, i)), 0] for i in range(ndma)]

    def _emit_waits(self, eng, deps):
        for sem, val in deps.items():
            if eng.known.get(sem, 0) < val:
                if sem is eng.sem:
                    assert val <= eng.cnt, "self-wait on future count"
                eng.h.wait_ge(sem, val)
                eng.known[sem] = val

    def _deps(self, eng, reads, writes):
        deps = {}

        def add(d, raw):
            sem, val = d
            if sem is eng.sem and not raw:
                return
            if deps.get(sem, 0) < val:
                deps[sem] = val

        for t in reads:
            if t.w is not None:
                add(t.w, True)
        for t in writes:
            if t.w is not None:
                add(t.w, False)
            for sem, val in t.r.items():
                add((sem, val), False)
        return deps

    def _mark(self, mark, reads, writes):
        for t in reads:
            if t.r.get(mark[0], 0) < mark[1]:
                t.r[mark[0]] = mark[1]
        for t in writes:
            t.w = mark
            t.r = {}

    def op(self, eng, fn, reads=(), writes=(), sig=True):
        self._emit_waits(eng, self._deps(eng, reads, writes))
        ins = fn()
        if sig:
            ins.then_inc(eng.sem, 1)
            eng.cnt += 1
            mark = (eng.sem, eng.cnt)
        else:
            mark = (eng.sem, eng.cnt + 1)
        self._mark(mark, reads, writes)
        return ins

    def dma(self, q, out, in_, reads=(), writes=(), fn=None, **kw):
        slot = q.pool[q.dma_i % len(q.pool)]
        q.dma_i += 1
        deps = self._deps(q, reads, writes)
        if slot[1] > 0:
            deps[slot[0]] = max(deps.get(slot[0], 0), 16 * slot[1])
        self._emit_waits(q, deps)
        ins = fn() if fn is not None else q.h.dma_start(out=out, in_=in_, **kw)
        ins.then_inc(slot[0], 16)
        slot[1] += 1
        self._mark((slot[0], 16 * slot[1]), reads, writes)
        return ins

    def barrier(self, skip_pool_dma=False):
        targets = {}
        for e in (self.pe, self.act, self.dve, self.pool):
            if e.cnt > 0:
                targets[e.sem] = e.cnt
        for q in ((self.sp, self.act) if skip_pool_dma else (self.sp, self.pool, self.act)):
            for sem, used in q.pool:
                if used > 0:
                    targets[sem] = 16 * used
        for e in self.engs:
            d = {s: v for s, v in targets.items() if not (s is e.sem and e.name == "pe")}
            self._emit_waits(e, d)


def build(dbg=False, stop=99):
    nc = bass.Bass("TRN2", target_bir_lowering=False)

    def din(name, shape):
        return nc.dram_tensor(name, list(shape), F32, kind="ExternalInput").ap()

    x_d = din("x", [S, D])
    mem_d = din("mem", [256, D])
    ln_in_g = din("ln_in_g", [1, D])
    ln_in_b = din("ln_in_b", [1, D])
    w_in = din("w_in", [D, 7168])
    b_in = din("b_in", [56, 128])
    ln_v_g = din("ln_v_g", [1, D])
    ln_v_b = din("ln_v_b", [1, D])
    w_sp = din("w_spatial", [8, 128, 128])
    b_sp = din("b_spatial", [8, 128])
    w_out = din("w_out", [D, D])
    ln1_g = din("ln1_g", [1, D])
    ln1_b = din("ln1_b", [1, D])
    w_mq = din("w_mem_q", [D, 512])
    w_mkv = din("w_mem_kv", [D, 1024])
    w_mo = din("w_mem_o", [512, D])
    ln2_g = din("ln2_g", [1, D])
    ln2_b = din("ln2_b", [1, D])
    w_r = din("w_router", [D, NEXP])
    r_b = din("router_bias", [1, NEXP])
    w_eg = din("w_exp_gate", [NEXP, D, 256])
    w_eu = din("w_exp_up", [NEXP, D, 256])
    w_ed = din("w_exp_down", [NEXP, 256, D])
    w_sg = din("w_sh_gate", [D, 256])
    w_su = din("w_sh_up", [D, 256])
    w_sd = din("w_sh_down", [256, D])
    ln3_g = din("ln3_g", [1, D])
    ln3_b = din("ln3_b", [1, D])
    out_d = nc.dram_tensor("out", [S, D], F32, kind="ExternalOutput").ap()
    XG = nc.dram_tensor("XG_scratch", [NSLOT, D], BF16, kind="Internal").ap()
    YG = nc.dram_tensor("YG_scratch", [NSLOT, D], BF16, kind="Internal").ap()
    dbg_d = {}
    if dbg:
        for nm in ("d_x0", "d_x1", "d_x2"):
            dbg_d[nm] = nc.dram_tensor(nm, [S, D], F32, kind="ExternalOutput").ap()

    w_in_v = w_in.rearrange("(c p) n -> p c n", p=128)

    with contextlib.ExitStack() as st:
        K = Sched(nc, st)
        pe, act, dve, pool, sp = K.pe, K.act, K.dve, K.pool, K.sp
        op, dma = K.op, K.dma

        uid = [0]

        def sb(stk, name, shape, dt):
            uid[0] += 1
            return stk.enter_context(nc.sbuf_tensor("%s_%d" % (name, uid[0]), list(shape), dt))

        def ps(stk, name, shape, dt=F32):
            uid[0] += 1
            return stk.enter_context(nc.psum_tensor("%s_%d" % (name, uid[0]), list(shape), dt))

        X = sb(st, "X", [128, NT, D], F32)
        SL8I = sb(st, "SL8I", [128, NT, 8], U32)
        G8v = sb(st, "G8v", [128, NT, 8], F32)
        tSL = toks(NT)
        tZ = toks(NEXP)
        tX = toks(NT)
        tXT = toks(NT)
        identf = sb(st, "identf", [128, 128], F32)
        identb = sb(st, "identb", [128, 128], BF16)
        onesf = sb(st, "onesf", [128, 128], F32)
        BC = sb(st, "BC", [128, 56], F32)
        ONESB = sb(st, "ONESB", [1, 128], BF16)
        NEGH = sb(st, "NEGH", [128, NT], F32)
        tC = Tok()
        stXT = contextlib.ExitStack()
        XT = sb(stXT, "XT", [128, NKC, S], BF16)

        FILL0 = nc.gpsimd.to_reg(0.0)
        FILL1 = nc.gpsimd.to_reg(1.0)
        op(dve, lambda: nc.vector.memset(onesf[:], 1.0), writes=[tC])
        op(dve, lambda: nc.vector.memset(ONESB[:], 1.0), writes=[tC])
        op(dve, lambda: nc.vector.memset(NEGH[:], -0.5), writes=[tC])
        op(pool, lambda: nc.gpsimd.affine_select(out=identf[:], in_=onesf[:], pattern=[[1, 128]],
                                                 compare_op=ALU.is_equal, fill=FILL0, base=0,
                                                 channel_multiplier=-1), reads=[tC], writes=[tC])
        op(pool, lambda: nc.gpsimd.affine_select(out=identb[:], in_=onesf[:], pattern=[[1, 128]],
                                                 compare_op=ALU.is_equal, fill=FILL0, base=0,
                                                 channel_multiplier=-1), reads=[tC], writes=[tC])

        def layer_norm_x(stk, g_d, b_d, PSt, tPSt, want_xt=True, per_tile=None, out_dram=None, dbg_out=None):
            G = sb(stk, "lnG", [128, D], F32)
            Bt = sb(stk, "lnB", [128, D], F32)
            ST = sb(stk, "lnST", [128, NT, 12], F32)
            MV = sb(stk, "lnMV", [128, NT, 2], F32)
            RS = sb(stk, "lnRS", [128, NT], F32)
            tG, tB, tS, tM, tR = Tok(), Tok(), Tok(), Tok(), Tok()
            dma(sp, G[:], g_d.partition_broadcast(128), writes=[tG])
            dma(sp, Bt[:], b_d.partition_broadcast(128), writes=[tB])
            for i in range(NT):
                for hf in range(2):
                    op(dve, lambda i=i, hf=hf: nc.vector.bn_stats(ST[:, i, hf * 6:(hf + 1) * 6],
                                                                  X[:, i, hf * 512:(hf + 1) * 512]),
                       reads=[tX[i]], writes=[tS])
                op(dve, lambda i=i: nc.vector.bn_aggr(MV[:, i, :], ST[:, i, :]), reads=[tS], writes=[tM])
            op(dve, lambda: nc.vector.tensor_scalar(RS[:], MV[:, :, 1], LN_EPS, None, ALU.add),
               reads=[tM], writes=[tR])
            op(pool, lambda: nc.gpsimd.tensor_tensor(RS[:], RS[:], NEGH[:], ALU.pow), reads=[tR, tC], writes=[tR])
            for i in range(NT):
                op(dve, lambda i=i: nc.vector.tensor_scalar(X[:, i, :], X[:, i, :], MV[:, i, 0:1], RS[:, i:i + 1],
                                                            ALU.subtract, ALU.mult),
                   reads=[tX[i], tM, tR], writes=[tX[i]])
                op(dve, lambda i=i: nc.vector.tensor_tensor(X[:, i, :], X[:, i, :], G[:], ALU.mult),
                   reads=[tX[i], tG], writes=[tX[i]])
                op(pool, lambda i=i: nc.gpsimd.tensor_tensor(X[:, i, :], X[:, i, :], Bt[:], ALU.add),
                   reads=[tX[i], tB], writes=[tX[i]])
                if dbg_out is not None:
                    dma(sp, dbg_out[i * 128:(i + 1) * 128, :], X[:, i, :], reads=[tX[i]])
                if out_dram is not None:
                    dma(sp, out_dram[i * 128:(i + 1) * 128, :], X[:, i, :], reads=[tX[i]])
                if want_xt:
                    for hb in range(2):
                        pb = PSt[hb]
                        for c4 in range(4):
                            c = hb * 4 + c4
                            op(pe, lambda i=i, c=c, c4=c4, pb=pb: nc.tensor.transpose(
                                pb[:, c4 * 128:(c4 + 1) * 128], X[:, i, c * 128:(c + 1) * 128], identf[:]),
                               reads=[tX[i], tC], writes=[tPSt[hb]], sig=(c4 == 3))
                        op(act, lambda i=i, hb=hb, pb=pb: nc.scalar.copy(
                            XT[:, hb * 4:(hb + 1) * 4, i * 128:(i + 1) * 128],
                            pb[:].rearrange("p (c t) -> p c t", c=4)),
                           reads=[tPSt[hb]], writes=[tXT[i]])
                        if per_tile is not None:
                            per_tile(i, hb, pb, tPSt[hb])

        def dump_x():
            for i in range(NT):
                dma(sp, out_d[i * 128:(i + 1) * 128, :], X[:, i, :], reads=[tX[i]])
            K.barrier()
            stXT.close()

        with contextlib.ExitStack() as p0:
            PSt = [ps(p0, "p0t%d" % k, [128, 512]) for k in range(2)]
            tPSt = toks(2)
            for i in range(NT):
                dma(sp, X[:, i, :], x_d[i * 128:(i + 1) * 128, :], writes=[tX[i]])
            BI = sb(p0, "BI", [56, 128], F32)
            tBI = Tok()
            dma(sp, BI[:], b_in[:, :], writes=[tBI])
            PSb = ps(p0, "p0b", [128, 512])
            tPSb = Tok()
            op(pe, lambda: nc.tensor.transpose(PSb[:, 0:56], BI[:], identf[0:56, 0:56]),
               reads=[tBI, tC], writes=[tPSb])
            op(act, lambda: nc.scalar.copy(BC[:], PSb[:, 0:56]), reads=[tPSb], writes=[tC])
            layer_norm_x(p0, ln_in_g, ln_in_b, PSt, tPSt, dbg_out=dbg_d.get("d_x0"))
            for i in range(NT):
                op(dve, lambda i=i: nc.vector.tensor_scalar(X[:, i, :], X[:, i, :], ALPHA, None, ALU.mult),
                   reads=[tX[i]], writes=[tX[i]])
            K.barrier()

        def proj_fm(Wblk, tW, PSbanks, tPS, consume):
            for tg in range(4):
                pb = PSbanks[tg % len(PSbanks)]
                tp = tPS[tg % len(PSbanks)]
                for kc in range(NKC):
                    op(pe, lambda kc=kc, tg=tg, pb=pb: nc.tensor.matmul(
                        pb[:], lhsT=Wblk[:, kc, :], rhs=XT[:, kc, tg * 512:(tg + 1) * 512],
                        start=(kc == 0), stop=(kc == NKC - 1)),
                       reads=[tW] + tXT[tg * 4:(tg + 1) * 4], writes=[tp], sig=(kc == NKC - 1))
                consume(tg, pb, tp)

        def out_proj_accum(MT2, tMT2, WO2, tWO2, PSo, tPSo, nu=2):
            for i in range(NT):
                for hf in range(2):
                    k = (i * 2 + hf) % len(PSo)
                    pb, tp = PSo[k], tPSo[k]
                    for u in range(nu):
                        op(pe, lambda i=i, hf=hf, u=u, pb=pb: nc.tensor.matmul(
                            pb[:, 0:512], lhsT=MT2[:, u, i * 128:(i + 1) * 128], rhs=WO2[:, u, hf * 512:(hf + 1) * 512],
                            start=(u == 0), stop=(u == nu - 1)),
                           reads=[tMT2[u], tWO2], writes=[tp], sig=(u == nu - 1))
                    op(dve, lambda i=i, hf=hf, pb=pb: nc.vector.tensor_tensor(
                        X[:, i, hf * 512:(hf + 1) * 512], X[:, i, hf * 512:(hf + 1) * 512], pb[:, 0:512], ALU.add),
                       reads=[tp, tX[i]], writes=[tX[i]])

        if stop == 0:
            dump_x()
            return nc
        with contextlib.ExitStack() as p1:
            VN = sb(p1, "VN", [128, NT, D], BF16)
            tVN = toks(NT)
            PSa = [ps(p1, "p1a%d" % k, [128, 512]) for k in range(4)]
            tPSa = toks(4)
            PSm = [ps(p1, "p1m%d" % k, [128, 512]) for k in range(2)]
            tPSm = toks(2)
            PSo = [ps(p1, "p1o%d" % k, [128, 512]) for k in range(2)]
            tPSo = toks(2)
            with contextlib.ExitStack() as p1a:
                W2 = [sb(p1a, "Wv%d" % k, [128, NKC, 512], BF16) for k in range(2)]
                BR = sb(p1a, "BRv", [1, D], BF16)
                tW2, tBR = toks(2), Tok()
                G = sb(p1a, "vG", [128, D], F32)
                Bt = sb(p1a, "vB", [128, D], F32)
                ST = sb(p1a, "vST", [128, NT, 12], F32)
                MV = sb(p1a, "vMV", [128, NT, 2], F32)
                RS = sb(p1a, "vRS", [128, NT], F32)
                tG, tB = Tok(), Tok()
                tLv = toks(NT)
                dma(sp, G[:], ln_v_g.partition_broadcast(128), writes=[tG])
                dma(sp, Bt[:], ln_v_b.partition_broadcast(128), writes=[tB])
                dma(pool, BR[:], b_in[8:16, :].rearrange("a b -> (a b)").rearrange("(o n) -> o n", o=1), writes=[tBR])
                for hf in range(2):
                    dma(pool, W2[hf][:], w_in_v[:, :, 1024 + hf * 512:1024 + (hf + 1) * 512], writes=[tW2[hf]])
                for hf in range(2):
                    W, tW = W2[hf], tW2[hf]
                    for i in range(NT):
                        k = i % 4
                        pb, tp = PSa[k], tPSa[k]
                        for kc in range(NKC):
                            op(pe, lambda kc=kc, i=i, pb=pb, W=W: nc.tensor.matmul(
                                pb[:], lhsT=XT[:, kc, i * 128:(i + 1) * 128], rhs=W[:, kc, :],
                                start=(kc == 0), stop=False),
                               reads=[tW, tXT[i]], writes=[tp], sig=False)
                        op(pe, lambda hf=hf, pb=pb: nc.tensor.matmul(
                            pb[:], lhsT=ONESB[0:1, :], rhs=BR[0:1, hf * 512:(hf + 1) * 512], start=False, stop=True),
                           reads=[tBR, tC], writes=[tp])
                        op(act, lambda i=i, hf=hf, pb=pb: nc.scalar.activation(
                            VN[:, i, hf * 512:(hf + 1) * 512], pb[:], AF.Gelu),
                           reads=[tp], writes=[tVN[i]])
                        if hf == 1:
                            tl = tLv[i]
                            for h2 in range(2):
                                op(dve, lambda i=i, h2=h2: nc.vector.bn_stats(ST[:, i, h2 * 6:(h2 + 1) * 6],
                                                                              VN[:, i, h2 * 512:(h2 + 1) * 512]),
                                   reads=[tVN[i]], writes=[tl])
                            op(dve, lambda i=i: nc.vector.bn_aggr(MV[:, i, :], ST[:, i, :]), reads=[tl], writes=[tl])
                            op(dve, lambda i=i: nc.vector.tensor_scalar(RS[:, i:i + 1], MV[:, i, 1:2], LN_EPS, None,
                                                                        ALU.add),
                               reads=[tl], writes=[tl])
                            op(pool, lambda i=i: nc.gpsimd.tensor_tensor(RS[:, i:i + 1], RS[:, i:i + 1], NEGH[:, 0:1],
                                                                         ALU.pow),
                               reads=[tl, tC], writes=[tl])
                            op(dve, lambda i=i: nc.vector.tensor_scalar(VN[:, i, :], VN[:, i, :], MV[:, i, 0:1],
                                                                        RS[:, i:i + 1], ALU.subtract, ALU.mult),
                               reads=[tVN[i], tl], writes=[tVN[i]])
                            op(dve, lambda i=i: nc.vector.tensor_tensor(VN[:, i, :], VN[:, i, :], G[:], ALU.mult),
                               reads=[tVN[i], tG], writes=[tVN[i]])
                            op(pool, lambda i=i: nc.gpsimd.tensor_tensor(VN[:, i, :], VN[:, i, :], Bt[:], ALU.add),
                               reads=[tVN[i], tB], writes=[tVN[i]])
                K.barrier()

            with contextlib.ExitStack() as p1b:
                WT = sb(p1b, "WT", [128, 8, 128], BF16)
                WS = sb(p1b, "WS", [128, 8, 128], F32)
                BS = sb(p1b, "BS", [128, 8, 128], F32)
                tWT, tWS, tBS = Tok(), Tok(), Tok()
                dma(sp, WS[:], w_sp.rearrange("g t s -> t g s"), writes=[tWS])
                dma(sp, BS[:].rearrange("p g t -> p (g t)"),
                    b_sp.rearrange("g t -> (g t)").rearrange("(o n) -> o n", o=1).partition_broadcast(128),
                    writes=[tBS])
                ZT = sb(p1b, "ZT", [128, CAP // 128, D], BF16)
                tZT = Tok()
                op(pool, lambda: nc.gpsimd.memset(ZT[:], 0.0), writes=[tZT])
                for g in range(8):
                    op(pool, lambda g=g: nc.gpsimd.affine_select(out=WS[:, g, :], in_=WS[:, g, :], pattern=[[-1, 128]],
                                                                 compare_op=ALU.is_ge, fill=FILL0, base=0,
                                                                 channel_multiplier=1),
                       reads=[tWS], writes=[tWS])
                for g4 in range(2):
                    pb, tp = PSm[g4], tPSm[g4]
                    for gg in range(4):
                        g = g4 * 4 + gg
                        op(pe, lambda g=g, gg=gg, pb=pb: nc.tensor.transpose(
                            pb[:, gg * 128:(gg + 1) * 128], WS[:, g, :], identf[:]),
                           reads=[tWS, tC], writes=[tp], sig=(gg == 3))
                    op(act, lambda g4=g4, pb=pb: nc.scalar.copy(
                        WT[:, g4 * 4:(g4 + 1) * 4, :], pb[:].rearrange("p (g t) -> p g t", g=4)),
                       reads=[tp], writes=[tWT])

                WB = [sb(p1b, "WB%d" % k, [128, 2, NKC, 128], BF16) for k in range(2)]
                tWB = toks(2)
                WO2 = sb(p1b, "WO4", [128, 4, D], BF16)
                tWO2 = Tok()
                U2 = [sb(p1b, "U%d" % k, [128, S], BF16) for k in range(2)]
                GA2 = [sb(p1b, "GA%d" % k, [128, S], BF16) for k in range(2)]
                T1 = [sb(p1b, "T1%d" % k, [128, 512], BF16) for k in range(2)]
                tU2, tGA2, tT1 = [toks(4), toks(4)], [toks(4), toks(4)], toks(2)
                MT2 = sb(p1b, "MT4", [128, 4, S], BF16)
                tMT2 = toks(4)
                def load_group_w(g):
                    par = g % 2
                    dma(pool, WB[par][:, 0, :, :], w_in_v[:, :, g * 128:(g + 1) * 128], writes=[tWB[par]])
                    dma(pool, WB[par][:, 1, :, :], w_in_v[:, :, 5120 + g * 128:5120 + (g + 1) * 128],
                        writes=[tWB[par]])

                load_group_w(0)
                for g in range(8):
                    par = g % 2
                    g4_ = g % 4
                    U, GA, tU, tGA = U2[par], GA2[par], tU2[par], tGA2[par]
                    if g + 1 < 8:
                        load_group_w(g + 1)
                    if g4_ == 0:
                        dma(pool, WO2[:], w_out[g * 128:(g + 4) * 128, :].rearrange("(u p) n -> p u n", p=128),
                            writes=[tWO2])

                    def cons_u(tg, pb, tp, g=g, U=U, tU=tU):
                        op(act, lambda: nc.scalar.activation(U[:, tg * 512:(tg + 1) * 512], pb[:], AF.Gelu,
                                                             bias=BC[:, g:g + 1], scale=1.0),
                           reads=[tp, tC], writes=[tU[tg]])
                    proj_fm(WB[par][:, 0, :, :], tWB[par], PSa, tPSa, cons_u)
                    for e in range(g * 8, (g + 1) * 8):
                        dma(sp, XG[e * CAP:(e + 1) * CAP, :].rearrange("(j p) n -> p j n", p=128), ZT[:],
                            reads=[tZT, tU[3]], writes=[tZ[e]])

                    def cons_ga(tg, pb, tp, g=g, GA=GA, tGA=tGA):
                        op(act, lambda: nc.scalar.activation(GA[:, tg * 512:(tg + 1) * 512], pb[:], AF.Sigmoid,
                                                             bias=BC[:, 40 + g:41 + g], scale=1.0),
                           reads=[tp, tC], writes=[tGA[tg]])
                    proj_fm(WB[par][:, 1, :, :], tWB[par], PSa, tPSa, cons_ga)

                    for tg in range(4):
                        pb, tp = PSm[tg % 2], tPSm[tg % 2]
                        for c4 in range(4):
                            c = tg * 4 + c4
                            op(pe, lambda c=c, c4=c4, pb=pb, g=g: nc.tensor.matmul(
                                pb[:, c4 * 128:(c4 + 1) * 128], lhsT=VN[:, c, g * 128:(g + 1) * 128],
                                rhs=WT[:, g, :], start=True, stop=True),
                               reads=[tVN[c], tWT], writes=[tp], sig=(c4 == 3))
                        t1 = T1[tg % 2]
                        for c4 in range(4):
                            op(dve, lambda c4=c4, pb=pb, t1=t1, g=g: nc.vector.tensor_tensor(
                                t1[:, c4 * 128:(c4 + 1) * 128], pb[:, c4 * 128:(c4 + 1) * 128], BS[:, g, :], ALU.add),
                               reads=[tp, tBS], writes=[tT1[tg % 2]])
                        op(dve, lambda tg=tg, t1=t1, U=U: nc.vector.tensor_tensor(
                            t1[:], t1[:], U[:, tg * 512:(tg + 1) * 512], ALU.mult),
                           reads=[tT1[tg % 2], tU[tg]], writes=[tT1[tg % 2]])
                        op(pool, lambda tg=tg, t1=t1, g4_=g4_, GA=GA: nc.gpsimd.tensor_tensor(
                            MT2[:, g4_, tg * 512:(tg + 1) * 512], t1[:], GA[:, tg * 512:(tg + 1) * 512], ALU.mult),
                           reads=[tT1[tg % 2], tGA[tg]], writes=[tMT2[g4_]])
                    if g4_ == 3:
                        out_proj_accum(MT2, tMT2, WO2, tWO2, PSo, tPSo, nu=4)
                K.barrier()

        if stop == 1:
            dump_x()
            return nc
        with contextlib.ExitStack() as p1c:
            PSz = [ps(p1c, "pz%d" % k, [128, 512]) for k in range(4)]
            tPSz = toks(4)
            zbase = {}
            PSTr = [ps(p1c, "ptr%d" % k, [128, 1024], BF16) for k in range(2)]
            tPSTr = toks(2)
            PSy = ps(p1c, "py", [128, 512])
            tPSy = Tok()
            PSp = [ps(p1c, "pp", [128, 512])]
            tPSp = toks(1)
            WH = [sb(p1c, "WH%d" % k, [128, 4, NKC, 128], BF16) for k in range(2)]
            tWH = toks(2)
            WO2 = sb(p1c, "WO2c", [128, 2, D], BF16)
            tWO2 = Tok()
            BRV = [sb(p1c, "BRV%d" % k, [1, 128], BF16) for k in range(2)]
            tBRV = toks(2)
            QT = [sb(p1c, "QT%d" % k, [128, S], BF16) for k in range(2)]
            KT = [sb(p1c, "KT%d" % k, [128, S], BF16) for k in range(2)]
            GB = [sb(p1c, "GB%d" % k, [128, S], BF16) for k in range(2)]
            VH = [sb(p1c, "VH%d" % k, [128, NT, 128], BF16) for k in range(2)]
            tQT, tKT, tGB, tVH = [toks(4), toks(4)], [toks(4), toks(4)], [toks(4), toks(4)], [toks(4), toks(4)]
            Rb = [sb(p1c, "Rb%d" % k, [128, S], F32) for k in range(2)]
            Bb = [sb(p1c, "Bb%d" % k, [128, S], BF16) for k in range(4)]
            Pb = [sb(p1c, "Pb%d" % k, [128, S + 2], BF16) for k in range(2)]
            ATb = [sb(p1c, "ATb%d" % k, [128, S], BF16) for k in range(2)]
            tRb, tBb, tATb, tPb = toks(2), toks(4), toks(2), toks(2)
            MT2 = sb(p1c, "MT2c", [128, 2, S], BF16)
            tMT2 = toks(2)

            def load_head_w(h, part=None):
                par = h % 2
                for j, off in enumerate((2048, 3072, 4096, 6144)):
                    if part is None or part == j:
                        dma(pool, WH[par][:, j, :, :], w_in_v[:, :, off + h * 128:off + (h + 1) * 128],
                            writes=[tWH[par]])
                if part is None or part == 4:
                    dma(pool, BRV[par][:], b_in[32 + h:33 + h, :], writes=[tBRV[par]])

            def emit_proj(h):
                par = h % 2
                specs = ((0, QT, tQT, AF.Identity, 16), (1, KT, tKT, AF.Identity, 24), (3, GB, tGB, AF.Sigmoid, 48))
                for (j, DST, tDST, fn_, bcol) in specs:
                    for tg in range(4):
                        pb, tp = PSp[0], tPSp[0]
                        for kc in range(NKC):
                            op(pe, lambda kc=kc, tg=tg, pb=pb, j=j: nc.tensor.matmul(
                                pb[:], lhsT=WH[par][:, j, kc, :], rhs=XT[:, kc, tg * 512:(tg + 1) * 512],
                                start=(kc == 0), stop=(kc == NKC - 1)),
                               reads=[tWH[par]] + tXT[tg * 4:(tg + 1) * 4], writes=[tp], sig=(kc == NKC - 1))
                        op(act, lambda tg=tg, pb=pb, DST=DST, fn_=fn_, bcol=bcol: nc.scalar.activation(
                            DST[par][:, tg * 512:(tg + 1) * 512], pb[:], fn_,
                            bias=BC[:, bcol + h:bcol + h + 1], scale=1.0),
                           reads=[tp, tC], writes=[tDST[par][tg]])
                        yield
                for tg in range(4):
                    pb, tp = PSp[0], tPSp[0]
                    for c4 in range(4):
                        i = tg * 4 + c4
                        for kc in range(NKC):
                            op(pe, lambda kc=kc, i=i, c4=c4, pb=pb: nc.tensor.matmul(
                                pb[:, c4 * 128:(c4 + 1) * 128], lhsT=XT[:, kc, i * 128:(i + 1) * 128],
                                rhs=WH[par][:, 2, kc, :], start=(kc == 0), stop=False),
                               reads=[tWH[par], tXT[i]], writes=[tp], sig=False)
                        op(pe, lambda c4=c4, pb=pb: nc.tensor.matmul(
                            pb[:, c4 * 128:(c4 + 1) * 128], lhsT=ONESB[0:1, :],
                            rhs=BRV[par][0:1, :], start=False, stop=True),
                           reads=[tBRV[par], tC], writes=[tp], sig=(c4 == 3))
                    op(act, lambda tg=tg, pb=pb: nc.scalar.copy(
                        VH[par][:, tg * 4:(tg + 1) * 4, :], pb[:].rearrange("p (c d) -> p c d", c=4)),
                       reads=[tp], writes=[tVH[par][tg]])
                    yield

            def stage_a1_pe(h, i, s_):
                par = h % 2
                nk = 128 * (i + 1)
                nch = (nk + 511) // 512
                base = zbase.get(s_ - 1, (0, 0))
                base = (base[0] + base[1]) % 4
                zbase[s_] = (base, nch)
                for ch in range(nch):
                    k0 = ch * 512
                    w_ = min(512, nk - k0)
                    zi = (base + ch) % 4
                    op(pe, lambda zi=zi, w_=w_, k0=k0: nc.tensor.matmul(
                        PSz[zi][:, 0:w_], lhsT=QT[par][:, i * 128:(i + 1) * 128],
                        rhs=KT[par][:, k0:k0 + w_], start=True, stop=True),
                       reads=[tQT[par][i // 4], tKT[par][ch]], writes=[tPSz[zi]])

            def stage_a1_act(h, i, s_):
                bp, bq = s_ % 2, s_ % 4
                nk = 128 * (i + 1)
                base, nch = zbase[s_]
                for ch in range(nch):
                    k0 = ch * 512
                    w_ = min(512, nk - k0)
                    zi = (base + ch) % 4
                    op(act, lambda zi=zi, k0=k0, w_=w_: nc.scalar.activation(
                        Rb[bp][:, k0:k0 + w_], PSz[zi][:, 0:w_], AF.Sigmoid, scale=-SB_SCALE),
                       reads=[tPSz[zi]], writes=[tRb[bp]])
                    op(act, lambda zi=zi, k0=k0, w_=w_: nc.scalar.activation(
                        Bb[bq][:, k0:k0 + w_], PSz[zi][:, 0:w_], AF.Sigmoid, scale=SB_SCALE),
                       reads=[tPSz[zi]], writes=[tBb[bq]])

            def stage_a2a(h, i, s_):
                bp, bq = s_ % 2, s_ % 4
                nk = 128 * (i + 1)
                d0 = i * 128
                op(pool, lambda: nc.gpsimd.affine_select(
                    out=Rb[bp][:, d0:d0 + 128], in_=Rb[bp][:, d0:d0 + 128], pattern=[[-1, 128]],
                    compare_op=ALU.is_gt, fill=FILL1, base=0, channel_multiplier=1),
                   reads=[tRb[bp]], writes=[tRb[bp]])
                op(pool, lambda: nc.gpsimd.affine_select(
                    out=Bb[bq][:, d0:d0 + 128], in_=Bb[bq][:, d0:d0 + 128], pattern=[[-1, 128]],
                    compare_op=ALU.is_gt, fill=FILL0, base=0, channel_multiplier=1),
                   reads=[tBb[bq]], writes=[tBb[bq]])
                op(pool, lambda: nc.gpsimd.memset(Pb[bp][:, nk + 1:nk + 2], 1.0), writes=[tPb[bp]])
                op(dve, lambda: nc.vector.tensor_tensor_scan(
                    out=Pb[bp][:, 1:nk + 1][:, ::-1], data0=Rb[bp][:, 0:nk][:, ::-1], data1=Rb[bp][:, 0:nk][:, ::-1],
                    initial=1.0, op0=ALU.mult, op1=ALU.min),
                   reads=[tRb[bp]], writes=[tPb[bp]])

            def stage_a2b(h, i, s_):
                bp, bq = s_ % 2, s_ % 4
                nk = 128 * (i + 1)
                op(dve, lambda: nc.vector.tensor_tensor(
                    Bb[bq][:, 0:nk], Bb[bq][:, 0:nk], Pb[bp][:, 2:nk + 2], ALU.mult),
                   reads=[tBb[bq], tPb[bp]], writes=[tBb[bq]])

            def stage_b(h, i, s_):
                par = h % 2
                bp, bq = s_ % 2, s_ % 4
                nb = i + 1
                for bk in range((nb + 7) // 8):
                    b0 = bk * 8
                    nbb = min(8, nb - b0)
                    pt, tpt = PSTr[bk % 2], tPSTr[bk % 2]
                    for b_ in range(nbb):
                        op(pe, lambda b_=b_, b0=b0, pt=pt: nc.tensor.transpose(
                            pt[:, b_ * 128:(b_ + 1) * 128], Bb[bq][:, (b0 + b_) * 128:(b0 + b_ + 1) * 128],
                            identb[:]),
                           reads=[tBb[bq], tC], writes=[tpt], sig=(b_ == nbb - 1))
                    op(act, lambda b0=b0, nbb=nbb, pt=pt: nc.scalar.copy(
                        ATb[bp][:, b0 * 128:(b0 + nbb) * 128], pt[:, 0:nbb * 128]),
                       reads=[tpt], writes=[tATb[bp]])

            def stage_b2(h, i, s_):
                par = h % 2
                bp, bq = s_ % 2, s_ % 4
                nb = i + 1
                c4 = i % 4
                for b_ in range(nb):
                    op(pe, lambda b_=b_: nc.tensor.matmul(
                        PSy[:, c4 * 128:(c4 + 1) * 128], lhsT=VH[par][:, b_, :],
                        rhs=ATb[bp][:, b_ * 128:(b_ + 1) * 128], start=(b_ == 0), stop=(b_ == nb - 1)),
                       reads=[tVH[par][b_ // 4], tATb[bp]], writes=[tPSy], sig=(b_ == nb - 1))
                if c4 == 3:
                    tg = i // 4
                    op(dve, lambda: nc.vector.tensor_tensor(
                        MT2[:, par, tg * 512:(tg + 1) * 512], PSy[:], GB[par][:, tg * 512:(tg + 1) * 512], ALU.mult),
                       reads=[tPSy, tGB[par][tg]], writes=[tMT2[par]])
                if par == 1 and i == NT - 1:
                    out_proj_accum(MT2, tMT2, WO2, tWO2, [PSp[0], PSy], [tPSp[0], tPSy])
                    if h + 1 < 8:
                        dma(pool, WO2[:], w_out[(h + 1) * 128:(h + 3) * 128, :].rearrange("(u p) n -> p u n", p=128),
                            writes=[tWO2])

            tiles = [(h, i) for h in range(8) for i in range(NT)]
            dma(pool, WO2[:], w_out[0:256, :].rearrange("(u p) n -> p u n", p=128), writes=[tWO2])
            load_head_w(0)
            load_head_w(1)
            for _ in emit_proj(0):
                pass
            NTL = len(tiles)
            stage_a1_pe(*tiles[0], 0)
            gen = None
            for s_ in range(NTL + 4):
                if s_ < NTL:
                    h, i = tiles[s_]
                    if 9 <= i <= 13 and h + 2 < 8:
                        load_head_w(h + 2, part=i - 9)
                    if i == 4 and h + 1 < 8:
                        gen = emit_proj(h + 1)
                        gen_n = 0
                    stage_a1_act(h, i, s_)
                if s_ + 1 < NTL:
                    stage_a1_pe(*tiles[s_ + 1], s_ + 1)
                if 0 <= s_ - 1 < NTL:
                    stage_a2a(*tiles[s_ - 1], s_ - 1)
                if 0 <= s_ - 2 < NTL:
                    stage_a2b(*tiles[s_ - 2], s_ - 2)
                if 0 <= s_ - 3 < NTL:
                    stage_b(*tiles[s_ - 3], s_ - 3)
                if 0 <= s_ - 4 < NTL:
                    stage_b2(*tiles[s_ - 4], s_ - 4)
                if gen is not None:
                    for _ in range(2 if gen_n < 12 else 1):
                        try:
                            next(gen)
                            gen_n += 1
                        except StopIteration:
                            gen = None
                            break
            K.barrier()

        if stop == 2:
            dump_x()
            return nc
        pmw = contextlib.ExitStack()
        WKV = sb(pmw, "WKV", [128, NKC, 1024], BF16)
        WQ = sb(pmw, "WQ", [128, NKC, 512], BF16)
        WO = sb(pmw, "WOm", [128, 4, D], BF16)
        MS = sb(pmw, "MS", [128, 2, D], F32)
        tWKV, tWQ, tWO, tMS = Tok(), Tok(), Tok(), Tok()
        dma(pool, WKV[:], w_mkv.rearrange("(c p) n -> p c n", p=128), writes=[tWKV])
        dma(pool, WQ[:], w_mq.rearrange("(c p) n -> p c n", p=128), writes=[tWQ])
        dma(pool, WO[:], w_mo.rearrange("(c p) n -> p c n", p=128), writes=[tWO])
        dma(sp, MS[:], mem_d.rearrange("(m p) n -> p m n", p=128), writes=[tMS])
        with contextlib.ExitStack() as pl1:
            PSt = [ps(pl1, "l1t%d" % k, [128, 512]) for k in range(2)]
            tPSt = toks(2)
            layer_norm_x(pl1, ln1_g, ln1_b, PSt, tPSt, dbg_out=dbg_d.get("d_x1"))
            for i in range(NT):
                op(dve, lambda i=i: nc.vector.tensor_scalar(X[:, i, :], X[:, i, :], ALPHA, None, ALU.mult),
                   reads=[tX[i]], writes=[tX[i]])
            K.barrier()

        if stop == 3:
            pmw.close()
            dump_x()
            return nc
        with contextlib.ExitStack() as p2:
            PSAf = ps(p2, "p2a", [128, 1024])
            PSA = [PSAf[:, 0:512], PSAf[:, 512:1024]]
            tPSA = toks(2)
            PSL = ps(p2, "p2l", [128, 1024])
            tPSL = Tok()
            PSTr = ps(p2, "p2tr", [128, 1024], BF16)
            tPSTr = Tok()
            PSO = ps(p2, "p2o", [128, 512])
            tPSO = Tok()
            PSM = [ps(p2, "p2m%d" % k, [128, 512]) for k in range(2)]
            tPSM = toks(2)
            MTm = sb(p2, "MTm", [128, NKC, 256], BF16)
            tMTm = Tok()
            for mt in range(2):
                for hb in range(2):
                    pb, tp = PSA[hb], tPSA[hb]
                    for c4 in range(4):
                        c = hb * 4 + c4
                        op(pe, lambda mt=mt, c=c, c4=c4, pb=pb: nc.tensor.transpose(
                            pb[:, c4 * 128:(c4 + 1) * 128], MS[:, mt, c * 128:(c + 1) * 128], identf[:]),
                           reads=[tMS, tC], writes=[tp], sig=(c4 == 3))
                    op(act, lambda mt=mt, hb=hb, pb=pb: nc.scalar.copy(
                        MTm[:, hb * 4:(hb + 1) * 4, mt * 128:(mt + 1) * 128],
                        pb[:].rearrange("p (c t) -> p c t", c=4)),
                       reads=[tp], writes=[tMTm])
            KM = sb(p2, "KM", [128, 4, 256], BF16)
            VM = sb(p2, "VM", [128, 2, 512], BF16)
            QM = sb(p2, "QM", [128, 4, S], BF16)
            tKM, tVM, tQM = Tok(), Tok(), toks(4)
            for h in range(4):
                pb, tp = PSA[h % 2], tPSA[h % 2]
                for kc in range(NKC):
                    op(pe, lambda h=h, kc=kc, pb=pb: nc.tensor.matmul(
                        pb[:, 0:256], lhsT=WKV[:, kc, h * 128:(h + 1) * 128], rhs=MTm[:, kc, :],
                        start=(kc == 0), stop=(kc == NKC - 1)),
                       reads=[tWKV, tMTm], writes=[tp], sig=(kc == NKC - 1))
                op(act, lambda h=h, pb=pb: nc.scalar.copy(KM[:, h, :], pb[:, 0:256]), reads=[tp], writes=[tKM])
            for mt in range(2):
                pb, tp = PSA[mt % 2], tPSA[mt % 2]
                for kc in range(NKC):
                    op(pe, lambda mt=mt, kc=kc, pb=pb: nc.tensor.matmul(
                        pb[:], lhsT=MTm[:, kc, mt * 128:(mt + 1) * 128], rhs=WKV[:, kc, 512:1024],
                        start=(kc == 0), stop=(kc == NKC - 1)),
                       reads=[tWKV, tMTm], writes=[tp], sig=(kc == NKC - 1))
                op(act, lambda mt=mt, pb=pb: nc.scalar.copy(VM[:, mt, :], pb[:]), reads=[tp], writes=[tVM])
            for h in range(4):
                for tg in range(4):
                    pb, tp = PSA[tg % 2], tPSA[tg % 2]
                    for kc in range(NKC):
                        op(pe, lambda h=h, tg=tg, kc=kc, pb=pb: nc.tensor.matmul(
                            pb[:], lhsT=WQ[:, kc, h * 128:(h + 1) * 128], rhs=XT[:, kc, tg * 512:(tg + 1) * 512],
                            start=(kc == 0), stop=(kc == NKC - 1)),
                           reads=[tWQ] + tXT[tg * 4:(tg + 1) * 4], writes=[tp], sig=(kc == NKC - 1))
                    op(act, lambda h=h, tg=tg, pb=pb: nc.scalar.copy(QM[:, h, tg * 512:(tg + 1) * 512], pb[:]),
                       reads=[tp], writes=[tQM[tg]])
            MX = [sb(p2, "MX%d" % k, [128, 4], F32) for k in range(2)]
            NMX = [sb(p2, "NMX%d" % k, [128, 4], F32) for k in range(2)]
            SS = [sb(p2, "SS%d" % k, [128, 4], F32) for k in range(2)]
            RSS = [sb(p2, "RSS%d" % k, [128, 4], F32) for k in range(2)]
            Pf = [sb(p2, "Pf%d" % k, [128, 4, 256], F32) for k in range(2)]
            Pn = [sb(p2, "Pn%d" % k, [128, 4, 256], BF16) for k in range(2)]
            PTm = [sb(p2, "PTm%d" % k, [128, 8, 128], BF16) for k in range(2)]
            OTm = [sb(p2, "OTm%d" % k, [128, 4, 128], BF16) for k in range(2)]
            tMX, tNMX, tSS, tRSS, tPf, tPn, tPTm, tOTm = (toks(2) for _ in range(8))
            PSL2 = [PSL, PSAf]
            tPSL2 = [tPSL, Tok()]

            def m_s1(i):
                q = i % 2
                psl, tpsl = PSL2[q], tPSL2[q]
                for h in range(4):
                    op(pe, lambda h=h: nc.tensor.matmul(
                        psl[:, h * 256:(h + 1) * 256], lhsT=QM[:, h, i * 128:(i + 1) * 128], rhs=KM[:, h, :],
                        start=True, stop=True),
                       reads=[tQM[i // 4], tKM], writes=[tpsl], sig=(h == 3))
                op(dve, lambda: nc.vector.tensor_reduce(MX[q][:], psl[:].rearrange("p (h m) -> p h m", h=4),
                                                        AX.X, ALU.max),
                   reads=[tpsl], writes=[tMX[q]])
                op(dve, lambda: nc.vector.tensor_scalar(NMX[q][:], MX[q][:], -MEM_SCALE, None, ALU.mult),
                   reads=[tMX[q]], writes=[tNMX[q]])
                for h in range(4):
                    op(act, lambda h=h: nc.scalar.activation(
                        Pf[q][:, h, :], psl[:, h * 256:(h + 1) * 256], AF.Exp, bias=NMX[q][:, h:h + 1],
                        scale=MEM_SCALE, accum_out=SS[q][:, h:h + 1]),
                       reads=[tpsl, tNMX[q]], writes=[tPf[q], tSS[q]])
                op(dve, lambda: nc.vector.reciprocal(RSS[q][:], SS[q][:]), reads=[tSS[q]], writes=[tRSS[q]])
                for h in range(4):
                    op(dve, lambda h=h: nc.vector.tensor_scalar(Pn[q][:, h, :], Pf[q][:, h, :], RSS[q][:, h:h + 1],
                                                                None, ALU.mult),
                       reads=[tPf[q], tRSS[q]], writes=[tPn[q]])

            def m_s2a(i):
                q = i % 2
                for h in range(4):
                    for mt in range(2):
                        j = h * 2 + mt
                        op(pe, lambda h=h, mt=mt, j=j: nc.tensor.transpose(
                            PSTr[:, j * 128:(j + 1) * 128], Pn[q][:, h, mt * 128:(mt + 1) * 128], identb[:]),
                           reads=[tPn[q], tC], writes=[tPSTr], sig=(j == 7))
                op(act, lambda: nc.scalar.copy(PTm[q][:], PSTr[:].rearrange("p (j t) -> p j t", j=8)),
                   reads=[tPSTr], writes=[tPTm[q]])

            def m_s2b(i):
                q = i % 2
                for h in range(4):
                    for mt in range(2):
                        op(pe, lambda h=h, mt=mt: nc.tensor.matmul(
                            PSO[:, h * 128:(h + 1) * 128], lhsT=VM[:, mt, h * 128:(h + 1) * 128],
                            rhs=PTm[q][:, h * 2 + mt, :], start=(mt == 0), stop=(mt == 1)),
                           reads=[tVM, tPTm[q]], writes=[tPSO], sig=(h == 3 and mt == 1))
                op(act, lambda: nc.scalar.copy(OTm[q][:], PSO[:].rearrange("p (h t) -> p h t", h=4)),
                   reads=[tPSO], writes=[tOTm[q]])

            def m_s3(i):
                q = i % 2
                for hf in range(2):
                    pb, tp = PSM[hf], tPSM[hf]
                    for h in range(4):
                        op(pe, lambda h=h, hf=hf, pb=pb: nc.tensor.matmul(
                            pb[:], lhsT=OTm[q][:, h, :], rhs=WO[:, h, hf * 512:(hf + 1) * 512],
                            start=(h == 0), stop=(h == 3)),
                           reads=[tOTm[q], tWO], writes=[tp], sig=(h == 3))
                    op(dve, lambda hf=hf, pb=pb: nc.vector.tensor_tensor(
                        X[:, i, hf * 512:(hf + 1) * 512], X[:, i, hf * 512:(hf + 1) * 512], pb[:], ALU.add),
                       reads=[tp, tX[i]], writes=[tX[i]])

            K.barrier()
            for s_ in range(NT + 3):
                if s_ < NT:
                    m_s1(s_)
                if 0 <= s_ - 1 < NT:
                    m_s2a(s_ - 1)
                if 0 <= s_ - 2 < NT:
                    m_s2b(s_ - 2)
                if 0 <= s_ - 3 < NT:
                    m_s3(s_ - 3)
            K.barrier()

        pmw.close()
        if stop == 4:
            dump_x()
            return nc
        with contextlib.ExitStack() as p3:
            pxb = contextlib.ExitStack()
            XB = sb(pxb, "XB", [128, NT, D], BF16)
            with contextlib.ExitStack() as p3a:
                PSt = [ps(p3a, "l2t%d" % k, [128, 512]) for k in range(2)]
                tPSt = toks(2)
                PSr = ps(p3a, "l2r", [128, 512])
                tPSr = Tok()
                PSpos = ps(p3a, "l2p", [128, 512])
                tPSpos = Tok()
                WR = sb(p3a, "WR", [128, NKC, NEXP], F32)
                RB = sb(p3a, "RB", [128, NEXP], F32)
                tWR, tRB = Tok(), Tok()
                dma(sp, WR[:], w_r.rearrange("(c p) n -> p c n", p=128), writes=[tWR])
                dma(sp, RB[:], r_b.partition_broadcast(128), writes=[tRB])
                tXB = toks(NT)
                MKB = sb(p3a, "MKB", [128, NT, NEXP], BF16)
                tMKB = toks(NT)
                LT = sb(p3a, "LT", [128, 128], BF16)
                ONESM = sb(p3a, "ONESM", [128, 128], BF16)
                EOFF = sb(p3a, "EOFF", [128, NEXP], F32)
                EOFFI = sb(p3a, "EOFFI", [128, NEXP], mybir.dt.int32)
                tK = Tok()
                op(pool, lambda: nc.gpsimd.affine_select(out=LT[:], in_=onesf[:], pattern=[[1, 128]],
                                                         compare_op=ALU.is_gt, fill=FILL0, base=0,
                                                         channel_multiplier=-1), reads=[tC], writes=[tK])
                op(dve, lambda: nc.vector.memset(ONESM[:], 1.0), writes=[tK])
                op(pool, lambda: nc.gpsimd.iota(EOFFI[:], pattern=[[CAP, NEXP]], base=0, channel_multiplier=0),
                   writes=[tK])
                op(dve, lambda: nc.vector.tensor_copy(EOFF[:], EOFFI[:]), reads=[tK], writes=[tK])
                SCA = sb(p3a, "SCA", [128, NT, NEXP], F32)
                tSCA = toks(NT)
                NR = 4
                PSposL = [PSpos] + [ps(p3a, "l2p%d" % k, [128, 512]) for k in range(NR - 1)]
                tPSposL = [tPSpos] + toks(NR - 1)

                def mkset(r):
                    d = {}
                    for nm, shp in (("SEL", [128, NEXP]), ("SELM", [128, NEXP]), ("T8", [128, 8, 8]), ("GS", [128, 8]),
                                    ("G8", [128, 8]), ("GM", [128, 8]), ("E8", [128, 8]), ("MK", [128, NEXP]),
                                    ("WGt", [128, NEXP]), ("SM", [128, 1]), ("GT_", [128, NEXP]),
                                    ("NMK", [128, NEXP]), ("SLOT", [128, NEXP]), ("N8", [128, 8]),
                                    ("SL8f", [128, 8]), ("JK", [128, NEXP])):
                        d[nm] = sb(p3a, "%s_%d" % (nm, r), shp, F32)
                    d["t"] = Tok()
                    return d
                RS_ = [mkset(r) for r in range(NR)]
                tXF = Tok()
                WRH = sb(p3a, "WRH", [128, NKC, NEXP], BF16)
                WRL = sb(p3a, "WRL", [128, NKC, NEXP], BF16)
                XL = sb(p3a, "XL", [128, NKC, 128], BF16)
                tWRH = Tok()
                op(dve, lambda: nc.vector.tensor_copy(WRH[:], WR[:]), reads=[tWR], writes=[tWRH])
                op(dve, lambda: nc.vector.tensor_tensor(WRL[:], WR[:], WRH[:], ALU.subtract),
                   reads=[tWR, tWRH], writes=[tWRH])

                def router(i, hb, pb, tp):
                    op(dve, lambda: nc.vector.tensor_tensor(
                        XL[:, hb * 4:(hb + 1) * 4, :], pb[:].rearrange("p (c t) -> p c t", c=4),
                        XT[:, hb * 4:(hb + 1) * 4, i * 128:(i + 1) * 128], ALU.subtract),
                       reads=[tp, tXT[i]], writes=[tXF])
                    if hb == 0:
                        op(act, lambda: nc.scalar.copy(XB[:, i, :], X[:, i, :]), reads=[tX[i]], writes=[tXB[i]])
                        return
                    n = 0
                    for (a_hi, wt) in ((True, WRH), (False, WRH), (True, WRL)):
                        for kc in range(NKC):
                            lhs = XT[:, kc, i * 128:(i + 1) * 128] if a_hi else XL[:, kc, :]
                            op(pe, lambda lhs=lhs, wt=wt, kc=kc, n=n: nc.tensor.matmul(
                                PSr[:, 0:NEXP], lhsT=lhs, rhs=wt[:, kc, :], start=(n == 0), stop=(n == 23)),
                               reads=[tXF, tXT[i], tWRH], writes=[tPSr], sig=(n == 23))
                            n += 1
                    op(act, lambda: nc.scalar.activation(SCA[:, i, :], PSr[:, 0:NEXP], AF.Sigmoid),
                       reads=[tPSr], writes=[tSCA[i]])

                def route_chain(i, r):
                    d = RS_[r]
                    SEL, SELM, T8, GS, G8, GM, E8, MK = (d[k] for k in ("SEL", "SELM", "T8", "GS", "G8", "GM", "E8", "MK"))
                    WGt, SM, GT_, NMK, SLOT, N8, SL8f, JK = (d[k] for k in ("WGt", "SM", "GT_", "NMK", "SLOT", "N8", "SL8f", "JK"))
                    tr = d["t"]
                    SC = SCA[:, i, :]
                    V = nc.vector
                    R = dict(reads=[tr], writes=[tr])
                    op(dve, lambda: V.tensor_tensor(SEL[:], SC, RB[:], ALU.add), reads=[tSCA[i], tRB], writes=[tr])
                    yield
                    for g in range(8):
                        op(dve, lambda g=g: V.max(out=T8[:, g, :], in_=SEL[:, g * 8:(g + 1) * 8]), **R)
                        yield
                    op(dve, lambda: V.tensor_tensor(GS[:], T8[:, :, 0], T8[:, :, 1], ALU.add), **R)
                    yield
                    op(dve, lambda: V.max(out=G8[:], in_=GS[:]), **R)
                    yield
                    op(dve, lambda: V.tensor_scalar(GM[:], GS[:], G8[:, 3:4], None, ALU.is_ge), **R)
                    yield
                    op(dve, lambda: V.tensor_scalar(GM[:], GM[:], 1.0, 1.0e4, ALU.subtract, ALU.mult), **R)
                    yield
                    for g in range(8):
                        op(dve, lambda g=g: V.tensor_scalar(SELM[:, g * 8:(g + 1) * 8], SEL[:, g * 8:(g + 1) * 8],
                                                            GM[:, g:g + 1], None, ALU.add), **R)
                        yield
                    op(dve, lambda: V.max(out=E8[:], in_=SELM[:]), **R)
                    yield
                    op(dve, lambda: V.tensor_scalar(MK[:], SELM[:], E8[:, 7:8], None, ALU.is_ge), **R)
                    yield
                    op(dve, lambda: V.tensor_copy(MKB[:, i, :], MK[:]), reads=[tr], writes=[tMKB[i]])
                    yield
                    op(dve, lambda: V.tensor_tensor(WGt[:], SC, MK[:], ALU.mult), **R)
                    yield
                    op(dve, lambda: V.tensor_reduce(SM[:], WGt[:], AX.X, ALU.add), **R)
                    yield
                    op(dve, lambda: V.reciprocal(SM[:], SM[:]), **R)
                    yield
                    op(dve, lambda: V.tensor_scalar(GT_[:], WGt[:], SM[:, 0:1], ROUTED_SCALE, ALU.mult, ALU.mult), **R)
                    yield
                    pp, tpp = PSposL[r], tPSposL[r]
                    op(pe, lambda: nc.tensor.matmul(pp[:, 0:NEXP], lhsT=LT[:], rhs=MKB[:, i, :],
                                                    start=True, stop=(i == 0)),
                       reads=[tK, tMKB[i]], writes=[tpp], sig=(i == 0))
                    for i2 in range(i):
                        op(pe, lambda i2=i2: nc.tensor.matmul(pp[:, 0:NEXP], lhsT=ONESM[:], rhs=MKB[:, i2, :],
                                                              start=False, stop=(i2 == i - 1)),
                           reads=[tK, tMKB[i2]], writes=[tpp], sig=(i2 == i - 1))
                    op(dve, lambda: V.tensor_scalar(NMK[:], MK[:], 1.0, -1.0e6, ALU.subtract, ALU.mult), **R)
                    yield
                    op(dve, lambda: V.tensor_tensor(SLOT[:], pp[:, 0:NEXP], EOFF[:], ALU.add),
                       reads=[tpp, tK, tr], writes=[tr])
                    yield
                    op(dve, lambda: V.tensor_tensor(SLOT[:], SLOT[:], NMK[:], ALU.add), **R)
                    yield
                    op(dve, lambda: V.tensor_scalar(SLOT[:], SLOT[:], -1.0, None, ALU.mult), **R)
                    yield
                    op(dve, lambda: V.max(out=N8[:], in_=SLOT[:]), **R)
                    yield
                    op(dve, lambda: V.tensor_scalar(SL8f[:], N8[:], -1.0, None, ALU.mult), **R)
                    yield
                    op(dve, lambda: V.tensor_copy(SL8I[:, i, :], SL8f[:]), reads=[tr], writes=[tSL[i]])
                    yield
                    for k in range(8):
                        dma(pool, None, None, reads=[tXB[i], tSL[i]] + tZ, writes=[Tok()],
                            fn=lambda k=k: nc.gpsimd.indirect_dma_start(
                                out=XG[:, :], out_offset=bass.IndirectOffsetOnAxis(ap=SL8I[:, i, k:k + 1], axis=0),
                                in_=XB[:, i, :], in_offset=None))
                    yield
                    tG8 = Tok()
                    for k in range(8):
                        op(dve, lambda k=k: V.scalar_tensor_tensor(
                            out=JK[:], in0=SLOT[:], scalar=N8[:, k:k + 1], in1=GT_[:], op0=ALU.is_equal,
                            op1=ALU.mult, accum_out=G8v[:, i, k:k + 1]),
                           reads=[tr], writes=[tr, tG8])
                        yield

                layer_norm_x(p3a, ln2_g, ln2_b, PSt, tPSt, per_tile=router, dbg_out=dbg_d.get("d_x2"))
                for base_ in range(0, NT, NR):
                    gens = [route_chain(base_ + r, r) for r in range(NR)]
                    while gens:
                        for g_ in list(gens):
                            try:
                                next(g_)
                            except StopIteration:
                                gens.remove(g_)
                for i in range(NT):
                    op(dve, lambda i=i: nc.vector.tensor_scalar(X[:, i, :], X[:, i, :], ALPHA, None, ALU.mult),
                       reads=[tX[i]], writes=[tX[i]])
                K.barrier(skip_pool_dma=True)

            if stop == 5:
                K.barrier()
                pxb.close()
                dump_x()
                return nc
            with contextlib.ExitStack() as p3s:
                PSg = [ps(p3s, "p3g%d" % k, [128, 512]) for k in range(2)]
                PSu = [ps(p3s, "p3u%d" % k, [128, 512]) for k in range(2)]
                PSd = [ps(p3s, "p3d%d" % k, [128, 512]) for k in range(4)]
                tPSg, tPSu, tPSd = toks(2), toks(2), toks(4)
                WG = sb(p3s, "sWG", [128, NKC, 256], BF16)
                WU = sb(p3s, "sWU", [128, NKC, 256], BF16)
                WD = sb(p3s, "sWD", [128, 2, D], BF16)
                tWG, tWU, tWD = Tok(), Tok(), Tok()
                HT = sb(p3s, "sHT", [128, 2, S], BF16)
                tHT = toks(4)
                SG = [sb(p3s, "sSG%d" % k, [128, 512], BF16) for k in range(2)]
                tSG = toks(2)
                sWGs = sb(p3s, "sWGs", [128, NKC, 256], F32)
                sWUs = sb(p3s, "sWUs", [128, NKC, 256], F32)
                sWDs = sb(p3s, "sWDs", [128, 2, D], F32)
                tsW = toks(3)
                dma(sp, sWGs[:], w_sg.rearrange("(c p) n -> p c n", p=128), writes=[tsW[0]])
                dma(sp, sWUs[:], w_su.rearrange("(c p) n -> p c n", p=128), writes=[tsW[1]])
                dma(sp, sWDs[:], w_sd.rearrange("(c p) n -> p c n", p=128), writes=[tsW[2]])
                op(act, lambda: nc.scalar.copy(WG[:], sWGs[:]), reads=[tsW[0]], writes=[tWG])
                op(dve, lambda: nc.vector.tensor_copy(WU[:], sWUs[:]), reads=[tsW[1]], writes=[tWU])
                op(act, lambda: nc.scalar.copy(WD[:], sWDs[:]), reads=[tsW[2]], writes=[tWD])
                cnt = 0
                for tg in range(4):
                    for hc in range(2):
                        k = cnt % 2
                        cnt += 1
                        for kc in range(NKC):
                            op(pe, lambda kc=kc, tg=tg, hc=hc, k=k: nc.tensor.matmul(
                                PSg[k][:], lhsT=WG[:, kc, hc * 128:(hc + 1) * 128],
                                rhs=XT[:, kc, tg * 512:(tg + 1) * 512], start=(kc == 0), stop=(kc == NKC - 1)),
                               reads=[tWG] + tXT[tg * 4:(tg + 1) * 4], writes=[tPSg[k]], sig=(kc == NKC - 1))
                        for kc in range(NKC):
                            op(pe, lambda kc=kc, tg=tg, hc=hc, k=k: nc.tensor.matmul(
                                PSu[k][:], lhsT=WU[:, kc, hc * 128:(hc + 1) * 128],
                                rhs=XT[:, kc, tg * 512:(tg + 1) * 512], start=(kc == 0), stop=(kc == NKC - 1)),
                               reads=[tWU] + tXT[tg * 4:(tg + 1) * 4], writes=[tPSu[k]], sig=(kc == NKC - 1))
                        op(act, lambda k=k: nc.scalar.activation(SG[k][:], PSg[k][:], AF.Silu),
                           reads=[tPSg[k]], writes=[tSG[k]])
                        op(dve, lambda k=k, tg=tg, hc=hc: nc.vector.tensor_tensor(
                            HT[:, hc, tg * 512:(tg + 1) * 512], SG[k][:], PSu[k][:], ALU.mult),
                           reads=[tSG[k], tPSu[k]], writes=[tHT[tg]])
                for i in range(NT):
                    for hf in range(2):
                        k = (i * 2 + hf) % 4
                        for hc in range(2):
                            op(pe, lambda i=i, hf=hf, hc=hc, k=k: nc.tensor.matmul(
                                PSd[k][:], lhsT=HT[:, hc, i * 128:(i + 1) * 128],
                                rhs=WD[:, hc, hf * 512:(hf + 1) * 512], start=(hc == 0), stop=(hc == 1)),
                               reads=[tHT[i // 4], tWD], writes=[tPSd[k]], sig=(hc == 1))
                        op(dve, lambda i=i, hf=hf, k=k: nc.vector.tensor_tensor(
                            X[:, i, hf * 512:(hf + 1) * 512], X[:, i, hf * 512:(hf + 1) * 512], PSd[k][:], ALU.add),
                           reads=[tPSd[k], tX[i]], writes=[tX[i]])
                K.barrier()

            pxb.close()
            stXT.close()
            with contextlib.ExitStack() as p3b:
                PSTr = [ps(p3b, "p3t%d" % k, [128, 1024], BF16) for k in range(2)]
                PSg = [ps(p3b, "p3g%d" % k, [128, 512]) for k in range(2)]
                PSu = [ps(p3b, "p3u%d" % k, [128, 512]) for k in range(2)]
                PSd = [ps(p3b, "p3d%d" % k, [128, 512]) for k in range(2)]
                tPSTr, tPSg, tPSu, tPSd = toks(2), toks(2), toks(2), toks(2)
                NJ = CAP // 128
                XS = [sb(p3b, "XS%d" % k, [128, NJ, D], BF16) for k in range(2)]
                XGT = [sb(p3b, "XGT%d" % k, [128, NKC, CAP], BF16) for k in range(2)]
                WGs = [sb(p3b, "WGs%d" % k, [128, NKC, 256], F32) for k in range(3)]
                WUs = [sb(p3b, "WUs%d" % k, [128, NKC, 256], F32) for k in range(3)]
                WDs = [sb(p3b, "WDs%d" % k, [128, 2, D], F32) for k in range(3)]
                WG = [sb(p3b, "WG%d" % k, [128, NKC, 256], BF16) for k in range(2)]
                WU = [sb(p3b, "WU%d" % k, [128, NKC, 256], BF16) for k in range(2)]
                WD = [sb(p3b, "WD%d" % k, [128, 2, D], BF16) for k in range(2)]
                HT = [sb(p3b, "HT%d" % k, [128, 2, CAP], BF16) for k in range(2)]
                SG1 = sb(p3b, "SG", [128, CAP], BF16)
                SG = [SG1, SG1]
                YS1 = sb(p3b, "YS", [128, NJ, D], BF16)
                YS = [YS1, YS1]
                tXS, tXGT, tWG, tWU, tWD, tHT = (toks(2) for _ in range(6))
                tSG1, tYS1 = Tok(), Tok()
                tSG, tYS = [tSG1, tSG1], [tYS1, tYS1]
                tWGs, tWUs, tWDs = toks(3), toks(3), toks(3)

                def prefetch_xs(e):
                    par = e % 2
                    dma(sp, XS[par][:], XG[e * CAP:(e + 1) * CAP, :].rearrange("(j p) n -> p j n", p=128),
                        writes=[tXS[par]])

                def prefetch_w(e):
                    p3_ = e % 3
                    dma(sp, WGs[p3_][:], w_eg[e].rearrange("(c p) n -> p c n", p=128), writes=[tWGs[p3_]])
                    dma(sp, WUs[p3_][:], w_eu[e].rearrange("(c p) n -> p c n", p=128), writes=[tWUs[p3_]])
                    dma(sp, WDs[p3_][:], w_ed[e].rearrange("(c p) n -> p c n", p=128), writes=[tWDs[p3_]])

                def cast_w(e):
                    p2_, p3_ = e % 2, e % 3
                    op(act, lambda: nc.scalar.copy(WG[p2_][:], WGs[p3_][:]), reads=[tWGs[p3_]], writes=[tWG[p2_]])
                    op(dve, lambda: nc.vector.tensor_copy(WU[p2_][:], WUs[p3_][:]), reads=[tWUs[p3_]],
                       writes=[tWU[p2_]])
                    op(pool, lambda: nc.gpsimd.tensor_copy(WD[p2_][:], WDs[p3_][:]), reads=[tWDs[p3_]],
                       writes=[tWD[p2_]])

                ev = [0]

                def transposes(e):
                    par = e % 2
                    for j in range(NJ):
                        pt, tpt = PSTr[j % 2], tPSTr[j % 2]
                        for c in range(NKC):
                            op(pe, lambda j=j, c=c, pt=pt: nc.tensor.transpose(
                                pt[:, c * 128:(c + 1) * 128], XS[par][:, j, c * 128:(c + 1) * 128], identb[:]),
                               reads=[tXS[par], tC], writes=[tpt], sig=(c == NKC - 1))
                        ev[0] += 1
                        if ev[0] % 2 == 0:
                            op(act, lambda j=j, pt=pt: nc.scalar.copy(
                                XGT[par][:, :, j * 128:(j + 1) * 128], pt[:].rearrange("p (c t) -> p c t", c=NKC)),
                               reads=[tpt], writes=[tXGT[par]])
                        else:
                            op(dve, lambda j=j, pt=pt: nc.vector.tensor_copy(
                                XGT[par][:, :, j * 128:(j + 1) * 128], pt[:].rearrange("p (c t) -> p c t", c=NKC)),
                               reads=[tpt], writes=[tXGT[par]])

                prefetch_xs(0)
                prefetch_w(0)
                prefetch_w(1)
                prefetch_xs(1)
                cast_w(0)
                transposes(0)
                for e in range(NEXP):
                    par = e % 2
                    if e + 2 < NEXP:
                        prefetch_xs(e + 2)
                        prefetch_w(e + 2)
                    if e + 1 < NEXP:
                        cast_w(e + 1)
                    for hc in range(2):
                        k = hc
                        for kc in range(NKC):
                            op(pe, lambda kc=kc, hc=hc, k=k: nc.tensor.matmul(
                                PSg[k][:], lhsT=WG[par][:, kc, hc * 128:(hc + 1) * 128], rhs=XGT[par][:, kc, :],
                                start=(kc == 0), stop=(kc == NKC - 1)),
                               reads=[tWG[par], tXGT[par]], writes=[tPSg[k]], sig=(kc == NKC - 1))
                        for kc in range(NKC):
                            op(pe, lambda kc=kc, hc=hc, k=k: nc.tensor.matmul(
                                PSu[k][:], lhsT=WU[par][:, kc, hc * 128:(hc + 1) * 128], rhs=XGT[par][:, kc, :],
                                start=(kc == 0), stop=(kc == NKC - 1)),
                               reads=[tWU[par], tXGT[par]], writes=[tPSu[k]], sig=(kc == NKC - 1))
                        op(act, lambda k=k: nc.scalar.activation(SG[k][:], PSg[k][:], AF.Silu),
                           reads=[tPSg[k]], writes=[tSG[k]])
                        op(dve, lambda k=k, hc=hc: nc.vector.tensor_tensor(
                            HT[par][:, hc, :], SG[k][:], PSu[k][:], ALU.mult),
                           reads=[tSG[k], tPSu[k]], writes=[tHT[par]])
                    if e + 1 < NEXP:
                        transposes(e + 1)
                    for j in range(NJ):
                        for hf in range(2):
                            k = (j * 2 + hf) % 2
                            for hc in range(2):
                                op(pe, lambda j=j, hf=hf, hc=hc, k=k: nc.tensor.matmul(
                                    PSd[k][:], lhsT=HT[par][:, hc, j * 128:(j + 1) * 128],
                                    rhs=WD[par][:, hc, hf * 512:(hf + 1) * 512], start=(hc == 0), stop=(hc == 1)),
                                   reads=[tHT[par], tWD[par]], writes=[tPSd[k]], sig=(hc == 1))
                            ev[0] += 1
                            if ev[0] % 2 == 0:
                                op(act, lambda j=j, hf=hf, k=k: nc.scalar.copy(
                                    YS[par][:, j, hf * 512:(hf + 1) * 512], PSd[k][:]),
                                   reads=[tPSd[k]], writes=[tYS[par]])
                            else:
                                op(dve, lambda j=j, hf=hf, k=k: nc.vector.tensor_copy(
                                    YS[par][:, j, hf * 512:(hf + 1) * 512], PSd[k][:]),
                                   reads=[tPSd[k]], writes=[tYS[par]])
                    dma(pool, YG[e * CAP:(e + 1) * CAP, :].rearrange("(j p) n -> p j n", p=128), YS[par][:],
                        reads=[tYS[par]])
                K.barrier()

            with contextlib.ExitStack() as p3c:
                NB = 6
                YR = [sb(p3c, "YR%d" % k, [128, D], BF16) for k in range(NB)]
                tYR = toks(NB)
                n = 0
                for i in range(NT):
                    for k in range(8):
                        bfi = n % NB
                        n += 1
                        dma(pool, None, None, reads=[tSL[i]], writes=[tYR[bfi]],
                            fn=lambda i=i, k=k, bfi=bfi: nc.gpsimd.indirect_dma_start(
                                out=YR[bfi][:], out_offset=None, in_=YG[:, :],
                                in_offset=bass.IndirectOffsetOnAxis(ap=SL8I[:, i, k:k + 1], axis=0)))
                        op(dve, lambda i=i, k=k, bfi=bfi: nc.vector.scalar_tensor_tensor(
                            out=X[:, i, :], in0=YR[bfi][:], scalar=G8v[:, i, k:k + 1], in1=X[:, i, :],
                            op0=ALU.mult, op1=ALU.add),
                           reads=[tYR[bfi], tSL[i], tX[i]], writes=[tX[i]])
                K.barrier()

        with contextlib.ExitStack() as pl3:
            layer_norm_x(pl3, ln3_g, ln3_b, None, None, want_xt=False, out_dram=out_d)
            K.barrier()
    return nc


_NC_CACHE = {}


def _prep_inputs(inputs, b):
    f = lambda a: np.ascontiguousarray(np.asarray(a, dtype=np.float32))
    m = {
        "x": f(inputs["x"][b]),
        "mem": f(inputs["mem"][b]),
        "ln_in_g": f(inputs["ln_in_g"]).reshape(1, D),
        "ln_in_b": f(inputs["ln_in_b"]).reshape(1, D),
        "w_in": f(inputs["w_in"][0]),
        "b_in": f(inputs["b_in"][0]).reshape(56, 128),
        "ln_v_g": f(inputs["ln_v_g"][0]).reshape(1, D),
        "ln_v_b": f(inputs["ln_v_b"][0]).reshape(1, D),
        "w_spatial": f(inputs["w_spatial"][0]),
        "b_spatial": f(inputs["b_spatial"][0]),
        "w_out": f(inputs["w_out"][0]),
        "ln1_g": f(inputs["ln1_g"][0]).reshape(1, D),
        "ln1_b": f(inputs["ln1_b"][0]).reshape(1, D),
        "w_mem_q": f(inputs["w_mem_q"][0]),
        "w_mem_kv": f(inputs["w_mem_kv"][0]),
        "w_mem_o": f(inputs["w_mem_o"][0]),
        "ln2_g": f(inputs["ln2_g"][0]).reshape(1, D),
        "ln2_b": f(inputs["ln2_b"][0]).reshape(1, D),
        "w_router": f(inputs["w_router"][0]),
        "router_bias": f(inputs["router_bias"][0]).reshape(1, NEXP),
        "w_exp_gate": f(inputs["w_exp_gate"][0]),
        "w_exp_up": f(inputs["w_exp_up"][0]),
        "w_exp_down": f(inputs["w_exp_down"][0]),
        "w_sh_gate": f(inputs["w_sh_gate"][0]),
        "w_sh_up": f(inputs["w_sh_up"][0]),
        "w_sh_down": f(inputs["w_sh_down"][0]),
        "ln3_g": f(inputs["ln3_g"][0]).reshape(1, D),
        "ln3_b": f(inputs["ln3_b"][0]).reshape(1, D),
    }
    return m


def kernel(**inputs):
    dbg = bool(os.environ.get("MK_DEBUG"))
    if dbg not in _NC_CACHE:
        _NC_CACHE[dbg] = build(dbg, int(os.environ.get("MK_STOP", "99")))
    nc = _NC_CACHE[dbg]
    shared = _prep_inputs(inputs, 0)
    in_maps = []
    for b in range(8):
        m = dict(shared)
        m["x"] = np.ascontiguousarray(np.asarray(inputs["x"][b], dtype=np.float32))
        m["mem"] = np.ascontiguousarray(np.asarray(inputs["mem"][b], dtype=np.float32))
        in_maps.append(m)
    res = run_bass_kernel_spmd(nc, in_maps, core_ids=list(range(8)))
    out = np.stack([np.asarray(r["out"], dtype=np.float32) for r in res.results], axis=0)
    if dbg:
        kernel.debug = [{k: np.asarray(v) for k, v in r.items()} for r in res.results]
    return out
```

```python
import os
import contextlib
import numpy as np
import ml_dtypes
import concourse.bass as bass
import concourse.mybir as mybir
from concourse.bass_utils import run_bass_kernel_spmd

F32 = mybir.dt.float32
BF16 = mybir.dt.bfloat16
AF = mybir.ActivationFunctionType
ALU = mybir.AluOpType
AX = mybir.AxisListType

S = 2048
D = 1024
NT = 16
NKC = 8
ALPHA = 2.0 ** 0.25
LN_EPS = 1e-5
SB_SCALE = 128.0 ** -0.5
MEM_SCALE = 128.0 ** -0.5
NEXP = 64
ROUTED_SCALE = 2.5
CAP = 512
NSLOT = NEXP * CAP
U32 = mybir.dt.uint32


class Tok:
    __slots__ = ("w", "r")

    def __init__(self):
        self.w = None
        self.r = {}


def toks(n):
    return [Tok() for _ in range(n)]


class Eng:
    def __init__(self, name, h, sem):
        self.name = name
        self.h = h
        self.sem = sem
        self.cnt = 0
        self.known = {}
        self.pool = []
        self.dma_i = 0


class Sched:
    def __init__(self, nc, st, ndma=12):
        self.nc = nc
        mk = lambda n: st.enter_context(nc.semaphore(n))
        self.pe = Eng("pe", nc.tensor, mk("s_pe"))
        self.act = Eng("act", nc.scalar, mk("s_act"))
        self.dve = Eng("dve", nc.vector, mk("s_dve"))
        self.pool = Eng("pool", nc.gpsimd, mk("s_pool"))
        self.sp = Eng("sp", nc.sync, mk("s_sp"))
        self.engs = [self.pe, self.act, self.dve, self.pool, self.sp]
        for q in (self.sp, self.pool, self.act):
            q.pool = [[mk("d_%s_%d" % (q.name, i)), 0] for i in range(ndma)]

    def _emit_waits(self, eng, deps):
        for sem, val in deps.items():
            if eng.known.get(sem, 0) < val:
                if sem is eng.sem:
                    assert val <= eng.cnt, "self-wait on future count"
                eng.h.wait_ge(sem, val)
                eng.known[sem] = val

    def _deps(self, eng, reads, writes):
        deps = {}

        def add(d, raw):
            sem, val = d
            if sem is eng.sem and not raw:
                return
            if deps.get(sem, 0) < val:
                deps[sem] = val

        for t in reads:
            if t.w is not None:
                add(t.w, True)
        for t in writes:
            if t.w is not None:
                add(t.w, False)
            for sem, val in t.r.items():
                add((sem, val), False)
        return deps

    def _mark(self, mark, reads, writes):
        for t in reads:
            if t.r.get(mark[0], 0) < mark[1]:
                t.r[mark[0]] = mark[1]
        for t in writes:
            t.w = mark
            t.r = {}

    def op(self, eng, fn, reads=(), writes=(), sig=True):
        self._emit_waits(eng, self._deps(eng, reads, writes))
        ins = fn()
        if sig:
            ins.then_inc(eng.sem, 1)
            eng.cnt += 1
            mark = (eng.sem, eng.cnt)
        else:
            mark = (eng.sem, eng.cnt + 1)
        self._mark(mark, reads, writes)
        return ins

    def dma(self, q, out, in_, reads=(), writes=(), fn=None, **kw):
        slot = q.pool[q.dma_i % len(q.pool)]
        q.dma_i += 1
        deps = self._deps(q, reads, writes)
        if slot[1] > 0:
            deps[slot[0]] = max(deps.get(slot[0], 0), 16 * slot[1])
        self._emit_waits(q, deps)
        ins = fn() if fn is not None else q.h.dma_start(out=out, in_=in_, **kw)
        ins.then_inc(slot[0], 16)
        slot[1] += 1
        self._mark((slot[0], 16 * slot[1]), reads, writes)
        return ins

    def barrier(self, skip_pool_dma=False):
        targets = {}
        for e in (self.pe, self.act, self.dve, self.pool):
            if e.cnt > 0:
                targets[e.sem] = e.cnt
        for q in ((self.sp, self.act) if skip_pool_dma else (self.sp, self.pool, self.act)):
            for sem, used in q.pool:
                if used > 0:
                    targets[sem] = 16 * used
        for e in self.engs:
            d = {s: v for s, v in targets.items() if not (s is e.sem and e.name == "pe")}
            self._emit_waits(e, d)


def build(dbg=False, stop=99):
    nc = bass.Bass("TRN2", target_bir_lowering=False)

    def din(name, shape):
        return nc.dram_tensor(name, list(shape), F32, kind="ExternalInput").ap()

    x_d = din("x", [S, D])
    mem_d = din("mem", [256, D])
    ln_in_g = din("ln_in_g", [1, D])
    ln_in_b = din("ln_in_b", [1, D])
    w_in = din("w_in", [D, 7168])
    b_in = din("b_in", [56, 128])
    ln_v_g = din("ln_v_g", [1, D])
    ln_v_b = din("ln_v_b", [1, D])
    w_sp = din("w_spatial", [8, 128, 128])
    b_sp = din("b_spatial", [8, 128])
    w_out = din("w_out", [D, D])
    ln1_g = din("ln1_g", [1, D])
    ln1_b = din("ln1_b", [1, D])
    w_mq = din("w_mem_q", [D, 512])
    w_mkv = din("w_mem_kv", [D, 1024])
    w_mo = din("w_mem_o", [512, D])
    ln2_g = din("ln2_g", [1, D])
    ln2_b = din("ln2_b", [1, D])
    w_r = din("w_router", [D, NEXP])
    r_b = din("router_bias", [1, NEXP])
    w_eg = din("w_exp_gate", [NEXP, D, 256])
    w_eu = din("w_exp_up", [NEXP, D, 256])
    w_ed = din("w_exp_down", [NEXP, 256, D])
    w_sg = din("w_sh_gate", [D, 256])
    w_su = din("w_sh_up", [D, 256])
    w_sd = din("w_sh_down", [256, D])
    ln3_g = din("ln3_g", [1, D])
    ln3_b = din("ln3_b", [1, D])
    out_d = nc.dram_tensor("out", [S, D], F32, kind="ExternalOutput").ap()
    XG = nc.dram_tensor("XG_scratch", [NSLOT, D], BF16, kind="Internal").ap()
    YG = nc.dram_tensor("YG_scratch", [NSLOT, D], BF16, kind="Internal").ap()
    dbg_d = {}
    if dbg:
        for nm in ("d_x0", "d_x1", "d_x2"):
            dbg_d[nm] = nc.dram_tensor(nm, [S, D], F32, kind="ExternalOutput").ap()

    w_in_v = w_in.rearrange("(c p) n -> p c n", p=128)

    with contextlib.ExitStack() as st:
        K = Sched(nc, st)
        pe, act, dve, pool, sp = K.pe, K.act, K.dve, K.pool, K.sp
        op, dma = K.op, K.dma

        uid = [0]

        def sb(stk, name, shape, dt):
            uid[0] += 1
            return stk.enter_context(nc.sbuf_tensor("%s_%d" % (name, uid[0]), list(shape), dt))

        def ps(stk, name, shape, dt=F32):
            uid[0] += 1
            return stk.enter_context(nc.psum_tensor("%s_%d" % (name, uid[0]), list(shape), dt))

        X = sb(st, "X", [128, NT, D], F32)
        SL8I = sb(st, "SL8I", [128, NT, 8], U32)
        G8v = sb(st, "G8v", [128, NT, 8], F32)
        tSL = toks(NT)
        tZ = toks(NEXP)
        tX = toks(NT)
        tXT = toks(NT)
        identf = sb(st, "identf", [128, 128], F32)
        identb = sb(st, "identb", [128, 128], BF16)
        onesf = sb(st, "onesf", [128, 128], F32)
        BC = sb(st, "BC", [128, 56], F32)
        ONESB = sb(st, "ONESB", [1, 128], BF16)
        NEGH = sb(st, "NEGH", [128, NT], F32)
        tC = Tok()
        stXT = contextlib.ExitStack()
        XT = sb(stXT, "XT", [128, NKC, S], BF16)

        FILL0 = nc.gpsimd.to_reg(0.0)
        FILL1 = nc.gpsimd.to_reg(1.0)
        op(dve, lambda: nc.vector.memset(onesf[:], 1.0), writes=[tC])
        op(dve, lambda: nc.vector.memset(ONESB[:], 1.0), writes=[tC])
        op(dve, lambda: nc.vector.memset(NEGH[:], -0.5), writes=[tC])
        op(pool, lambda: nc.gpsimd.affine_select(out=identf[:], in_=onesf[:], pattern=[[1, 128]],
                                                 compare_op=ALU.is_equal, fill=FILL0, base=0,
                                                 channel_multiplier=-1), reads=[tC], writes=[tC])
        op(pool, lambda: nc.gpsimd.affine_select(out=identb[:], in_=onesf[:], pattern=[[1, 128]],
                                                 compare_op=ALU.is_equal, fill=FILL0, base=0,
                                                 channel_multiplier=-1), reads=[tC], writes=[tC])

        def layer_norm_x(stk, g_d, b_d, PSt, tPSt, want_xt=True, per_tile=None, out_dram=None, dbg_out=None,
                         post_scale=None):
            G = sb(stk, "lnG", [128, D], F32)
            Bt = sb(stk, "lnB", [128, D], F32)
            ST = sb(stk, "lnST", [128, NT, 12], F32)
            MV = sb(stk, "lnMV", [128, NT, 2], F32)
            RS = sb(stk, "lnRS", [128, NT], F32)
            tG, tB, tS, tM, tR = Tok(), Tok(), Tok(), Tok(), Tok()
            dma(sp, G[:], g_d.partition_broadcast(128), writes=[tG])
            dma(sp, Bt[:], b_d.partition_broadcast(128), writes=[tB])
            for i in range(NT):
                for hf in range(2):
                    op(dve, lambda i=i, hf=hf: nc.vector.bn_stats(ST[:, i, hf * 6:(hf + 1) * 6],
                                                                  X[:, i, hf * 512:(hf + 1) * 512]),
                       reads=[tX[i]], writes=[tS])
                op(dve, lambda i=i: nc.vector.bn_aggr(MV[:, i, :], ST[:, i, :]), reads=[tS], writes=[tM])
            op(dve, lambda: nc.vector.tensor_scalar(RS[:], MV[:, :, 1], LN_EPS, None, ALU.add),
               reads=[tM], writes=[tR])
            op(pool, lambda: nc.gpsimd.tensor_tensor(RS[:], RS[:], NEGH[:], ALU.pow), reads=[tR, tC], writes=[tR])
            for i in range(NT):
                op(dve, lambda i=i: nc.vector.tensor_scalar(X[:, i, :], X[:, i, :], MV[:, i, 0:1], RS[:, i:i + 1],
                                                            ALU.subtract, ALU.mult),
                   reads=[tX[i], tM, tR], writes=[tX[i]])
                op(dve, lambda i=i: nc.vector.tensor_tensor(X[:, i, :], X[:, i, :], G[:], ALU.mult),
                   reads=[tX[i], tG], writes=[tX[i]])
                op(pool, lambda i=i: nc.gpsimd.tensor_tensor(X[:, i, :], X[:, i, :], Bt[:], ALU.add),
                   reads=[tX[i], tB], writes=[tX[i]])
                if dbg_out is not None:
                    dma(sp, dbg_out[i * 128:(i + 1) * 128, :], X[:, i, :], reads=[tX[i]])
                if out_dram is not None:
                    dma(sp, out_dram[i * 128:(i + 1) * 128, :], X[:, i, :], reads=[tX[i]])
                if want_xt:
                    for hb in range(2):
                        pb = PSt[hb]
                        for c4 in range(4):
                            c = hb * 4 + c4
                            op(pe, lambda i=i, c=c, c4=c4, pb=pb: nc.tensor.transpose(
                                pb[:, c4 * 128:(c4 + 1) * 128], X[:, i, c * 128:(c + 1) * 128], identf[:]),
                               reads=[tX[i], tC], writes=[tPSt[hb]], sig=(c4 == 3))
                        op(act, lambda i=i, hb=hb, pb=pb: nc.scalar.copy(
                            XT[:, hb * 4:(hb + 1) * 4, i * 128:(i + 1) * 128],
                            pb[:].rearrange("p (c t) -> p c t", c=4)),
                           reads=[tPSt[hb]], writes=[tXT[i]])
                        if per_tile is not None:
                            per_tile(i, hb, pb, tPSt[hb])
                    if post_scale is not None:
                        op(act, lambda i=i: nc.scalar.mul(X[:, i, :], X[:, i, :], post_scale),
                           reads=[tX[i]], writes=[tX[i]])

        def dump_x():
            for i in range(NT):
                dma(sp, out_d[i * 128:(i + 1) * 128, :], X[:, i, :], reads=[tX[i]])
            K.barrier()
            stXT.close()

        with contextlib.ExitStack() as p0:
            PSt = [ps(p0, "p0t%d" % k, [128, 512]) for k in range(2)]
            tPSt = toks(2)
            for i in range(NT):
                dma(sp, X[:, i, :], x_d[i * 128:(i + 1) * 128, :], writes=[tX[i]])
            BI = sb(p0, "BI", [56, 128], F32)
            tBI = Tok()
            dma(sp, BI[:], b_in[:, :], writes=[tBI])
            PSb = ps(p0, "p0b", [128, 512])
            tPSb = Tok()
            op(pe, lambda: nc.tensor.transpose(PSb[:, 0:56], BI[:], identf[0:56, 0:56]),
               reads=[tBI, tC], writes=[tPSb])
            op(act, lambda: nc.scalar.copy(BC[:], PSb[:, 0:56]), reads=[tPSb], writes=[tC])
            layer_norm_x(p0, ln_in_g, ln_in_b, PSt, tPSt, dbg_out=dbg_d.get("d_x0"), post_scale=ALPHA)
            K.barrier()

        def proj_fm(Wblk, tW, PSbanks, tPS, consume):
            for tg in range(4):
                pb = PSbanks[tg % len(PSbanks)]
                tp = tPS[tg % len(PSbanks)]
                for kc in range(NKC):
                    op(pe, lambda kc=kc, tg=tg, pb=pb: nc.tensor.matmul(
                        pb[:], lhsT=Wblk[:, kc, :], rhs=XT[:, kc, tg * 512:(tg + 1) * 512],
                        start=(kc == 0), stop=(kc == NKC - 1)),
                       reads=[tW] + tXT[tg * 4:(tg + 1) * 4], writes=[tp], sig=(kc == NKC - 1))
                consume(tg, pb, tp)

        def out_proj_accum(MT2, tMT2, WO2, tWO2, PSo, tPSo, nu=2):
            for i in range(NT):
                for hf in range(2):
                    k = (i * 2 + hf) % len(PSo)
                    pb, tp = PSo[k], tPSo[k]
                    for u in range(nu):
                        op(pe, lambda i=i, hf=hf, u=u, pb=pb: nc.tensor.matmul(
                            pb[:, 0:512], lhsT=MT2[:, u, i * 128:(i + 1) * 128], rhs=WO2[:, u, hf * 512:(hf + 1) * 512],
                            start=(u == 0), stop=(u == nu - 1)),
                           reads=[tMT2[u], tWO2], writes=[tp], sig=(u == nu - 1))
                    op(dve, lambda i=i, hf=hf, pb=pb: nc.vector.tensor_tensor(
                        X[:, i, hf * 512:(hf + 1) * 512], X[:, i, hf * 512:(hf + 1) * 512], pb[:, 0:512], ALU.add),
                       reads=[tp, tX[i]], writes=[tX[i]])

        if stop == 0:
            dump_x()
            return nc
        with contextlib.ExitStack() as p1:
            VN = sb(p1, "VN", [128, NT, D], BF16)
            tVN = toks(NT)
            PSa = [ps(p1, "p1a%d" % k, [128, 512]) for k in range(4)]
            tPSa = toks(4)
            PSm = [ps(p1, "p1m%d" % k, [128, 512]) for k in range(2)]
            tPSm = toks(2)
            PSo = [ps(p1, "p1o%d" % k, [128, 512]) for k in range(2)]
            tPSo = toks(2)
            with contextlib.ExitStack() as p1a:
                W2 = [sb(p1a, "Wv%d" % k, [128, NKC, 512], BF16) for k in range(2)]
                BR = sb(p1a, "BRv", [1, D], BF16)
                tW2, tBR = toks(2), Tok()
                G = sb(p1a, "vG", [128, D], F32)
                Bt = sb(p1a, "vB", [128, D], F32)
                ST = sb(p1a, "vST", [128, NT, 12], F32)
                MV = sb(p1a, "vMV", [128, NT, 2], F32)
                RS = sb(p1a, "vRS", [128, NT], F32)
                tG, tB = Tok(), Tok()
                tLv = toks(NT)
                dma(sp, G[:], ln_v_g.partition_broadcast(128), writes=[tG])
                dma(sp, Bt[:], ln_v_b.partition_broadcast(128), writes=[tB])
                dma(pool, BR[:], b_in[8:16, :].rearrange("a b -> (a b)").rearrange("(o n) -> o n", o=1), writes=[tBR])
                for hf in range(2):
                    dma(pool, W2[hf][:], w_in_v[:, :, 1024 + hf * 512:1024 + (hf + 1) * 512], writes=[tW2[hf]])
                for hf in range(2):
                    W, tW = W2[hf], tW2[hf]
                    for i in range(NT):
                        k = i % 4
                        pb, tp = PSa[k], tPSa[k]
                        for kc in range(NKC):
                            op(pe, lambda kc=kc, i=i, pb=pb, W=W: nc.tensor.matmul(
                                pb[:], lhsT=XT[:, kc, i * 128:(i + 1) * 128], rhs=W[:, kc, :],
                                start=(kc == 0), stop=False),
                               reads=[tW, tXT[i]], writes=[tp], sig=False)
                        op(pe, lambda hf=hf, pb=pb: nc.tensor.matmul(
                            pb[:], lhsT=ONESB[0:1, :], rhs=BR[0:1, hf * 512:(hf + 1) * 512], start=False, stop=True),
                           reads=[tBR, tC], writes=[tp])
                        op(act, lambda i=i, hf=hf, pb=pb: nc.scalar.activation(
                            VN[:, i, hf * 512:(hf + 1) * 512], pb[:], AF.Gelu),
                           reads=[tp], writes=[tVN[i]])
                        if hf == 1:
                            tl = tLv[i]
                            for h2 in range(2):
                                op(dve, lambda i=i, h2=h2: nc.vector.bn_stats(ST[:, i, h2 * 6:(h2 + 1) * 6],
                                                                              VN[:, i, h2 * 512:(h2 + 1) * 512]),
                                   reads=[tVN[i]], writes=[tl])
                            op(dve, lambda i=i: nc.vector.bn_aggr(MV[:, i, :], ST[:, i, :]), reads=[tl], writes=[tl])
                            op(dve, lambda i=i: nc.vector.tensor_scalar(RS[:, i:i + 1], MV[:, i, 1:2], LN_EPS, None,
                                                                        ALU.add),
                               reads=[tl], writes=[tl])
                            op(pool, lambda i=i: nc.gpsimd.tensor_tensor(RS[:, i:i + 1], RS[:, i:i + 1], NEGH[:, 0:1],
                                                                         ALU.pow),
                               reads=[tl, tC], writes=[tl])
                            op(dve, lambda i=i: nc.vector.tensor_scalar(VN[:, i, :], VN[:, i, :], MV[:, i, 0:1],
                                                                        RS[:, i:i + 1], ALU.subtract, ALU.mult),
                               reads=[tVN[i], tl], writes=[tVN[i]])
                            op(dve, lambda i=i: nc.vector.tensor_tensor(VN[:, i, :], VN[:, i, :], G[:], ALU.mult),
                               reads=[tVN[i], tG], writes=[tVN[i]])
                            op(pool, lambda i=i: nc.gpsimd.tensor_tensor(VN[:, i, :], VN[:, i, :], Bt[:], ALU.add),
                               reads=[tVN[i], tB], writes=[tVN[i]])
                K.barrier()

            with contextlib.ExitStack() as p1b:
                WT = sb(p1b, "WT", [128, 8, 128], BF16)
                WS = sb(p1b, "WS", [128, 8, 128], F32)
                BS = sb(p1b, "BS", [128, 8, 128], F32)
                tWT, tWS, tBS = Tok(), Tok(), Tok()
                dma(sp, WS[:], w_sp.rearrange("g t s -> t g s"), writes=[tWS])
                dma(sp, BS[:].rearrange("p g t -> p (g t)"),
                    b_sp.rearrange("g t -> (g t)").rearrange("(o n) -> o n", o=1).partition_broadcast(128),
                    writes=[tBS])
                ZT = sb(p1b, "ZT", [128, CAP // 128, D], BF16)
                tZT = Tok()
                op(pool, lambda: nc.gpsimd.memset(ZT[:], 0.0), writes=[tZT])
                for g in range(8):
                    op(pool, lambda g=g: nc.gpsimd.affine_select(out=WS[:, g, :], in_=WS[:, g, :], pattern=[[-1, 128]],
                                                                 compare_op=ALU.is_ge, fill=FILL0, base=0,
                                                                 channel_multiplier=1),
                       reads=[tWS], writes=[tWS])
                for g4 in range(2):
                    pb, tp = PSm[g4], tPSm[g4]
                    for gg in range(4):
                        g = g4 * 4 + gg
                        op(pe, lambda g=g, gg=gg, pb=pb: nc.tensor.transpose(
                            pb[:, gg * 128:(gg + 1) * 128], WS[:, g, :], identf[:]),
                           reads=[tWS, tC], writes=[tp], sig=(gg == 3))
                    op(act, lambda g4=g4, pb=pb: nc.scalar.copy(
                        WT[:, g4 * 4:(g4 + 1) * 4, :], pb[:].rearrange("p (g t) -> p g t", g=4)),
                       reads=[tp], writes=[tWT])

                WB = [sb(p1b, "WB%d" % k, [128, 2, NKC, 128], BF16) for k in range(2)]
                tWB = toks(2)
                WO2 = sb(p1b, "WO4", [128, 4, D], BF16)
                tWO2 = Tok()
                U2 = [sb(p1b, "U%d" % k, [128, S], BF16) for k in range(2)]
                GA2 = [sb(p1b, "GA%d" % k, [128, S], BF16) for k in range(2)]
                T1 = [sb(p1b, "T1%d" % k, [128, 512], BF16) for k in range(2)]
                tU2, tGA2, tT1 = [toks(4), toks(4)], [toks(4), toks(4)], toks(2)
                MT2 = sb(p1b, "MT4", [128, 4, S], BF16)
                tMT2 = toks(4)
                def load_group_w(g):
                    par = g % 2
                    dma(pool, WB[par][:, 0, :, :], w_in_v[:, :, g * 128:(g + 1) * 128], writes=[tWB[par]])
                    dma(pool, WB[par][:, 1, :, :], w_in_v[:, :, 5120 + g * 128:5120 + (g + 1) * 128],
                        writes=[tWB[par]])

                load_group_w(0)
                for g in range(8):
                    par = g % 2
                    g4_ = g % 4
                    U, GA, tU, tGA = U2[par], GA2[par], tU2[par], tGA2[par]
                    if g + 1 < 8:
                        load_group_w(g + 1)
                    if g4_ == 0:
                        dma(pool, WO2[:], w_out[g * 128:(g + 4) * 128, :].rearrange("(u p) n -> p u n", p=128),
                            writes=[tWO2])

                    def cons_u(tg, pb, tp, g=g, U=U, tU=tU):
                        op(act, lambda: nc.scalar.activation(U[:, tg * 512:(tg + 1) * 512], pb[:], AF.Gelu,
                                                             bias=BC[:, g:g + 1], scale=1.0),
                           reads=[tp, tC], writes=[tU[tg]])
                    proj_fm(WB[par][:, 0, :, :], tWB[par], PSa, tPSa, cons_u)
                    for e in range(g * 8, (g + 1) * 8):
                        dma(sp, XG[e * CAP:(e + 1) * CAP, :].rearrange("(j p) n -> p j n", p=128), ZT[:],
                            reads=[tZT, tU[3]], writes=[tZ[e]])

                    def cons_ga(tg, pb, tp, g=g, GA=GA, tGA=tGA):
                        op(act, lambda: nc.scalar.activation(GA[:, tg * 512:(tg + 1) * 512], pb[:], AF.Sigmoid,
                                                             bias=BC[:, 40 + g:41 + g], scale=1.0),
                           reads=[tp, tC], writes=[tGA[tg]])
                    proj_fm(WB[par][:, 1, :, :], tWB[par], PSa, tPSa, cons_ga)

                    for tg in range(4):
                        pb, tp = PSm[tg % 2], tPSm[tg % 2]
                        for c4 in range(4):
                            c = tg * 4 + c4
                            op(pe, lambda c=c, c4=c4, pb=pb, g=g: nc.tensor.matmul(
                                pb[:, c4 * 128:(c4 + 1) * 128], lhsT=VN[:, c, g * 128:(g + 1) * 128],
                                rhs=WT[:, g, :], start=True, stop=True),
                               reads=[tVN[c], tWT], writes=[tp], sig=(c4 == 3))
                        t1 = T1[tg % 2]
                        for c4 in range(4):
                            op(dve, lambda c4=c4, pb=pb, t1=t1, g=g: nc.vector.tensor_tensor(
                                t1[:, c4 * 128:(c4 + 1) * 128], pb[:, c4 * 128:(c4 + 1) * 128], BS[:, g, :], ALU.add),
                               reads=[tp, tBS], writes=[tT1[tg % 2]])
                        op(dve, lambda tg=tg, t1=t1, U=U: nc.vector.tensor_tensor(
                            t1[:], t1[:], U[:, tg * 512:(tg + 1) * 512], ALU.mult),
                           reads=[tT1[tg % 2], tU[tg]], writes=[tT1[tg % 2]])
                        op(pool, lambda tg=tg, t1=t1, g4_=g4_, GA=GA: nc.gpsimd.tensor_tensor(
                            MT2[:, g4_, tg * 512:(tg + 1) * 512], t1[:], GA[:, tg * 512:(tg + 1) * 512], ALU.mult),
                           reads=[tT1[tg % 2], tGA[tg]], writes=[tMT2[g4_]])
                    if g4_ == 3:
                        out_proj_accum(MT2, tMT2, WO2, tWO2, PSo, tPSo, nu=4)
                K.barrier()

        if stop == 1:
            dump_x()
            return nc
        with contextlib.ExitStack() as p1c:
            PSz = [ps(p1c, "pz%d" % k, [128, 512]) for k in range(4)]
            tPSz = toks(4)
            zbase = {}
            PSTr = [ps(p1c, "ptr%d" % k, [128, 1024], BF16) for k in range(2)]
            tPSTr = toks(2)
            PSy = ps(p1c, "py", [128, 512])
            tPSy = Tok()
            PSp = [ps(p1c, "pp", [128, 512])]
            tPSp = toks(1)
            WH = [sb(p1c, "WH%d" % k, [128, 4, NKC, 128], BF16) for k in range(2)]
            tWH = toks(2)
            WO2 = sb(p1c, "WO2c", [128, 2, D], BF16)
            tWO2 = Tok()
            BRV = [sb(p1c, "BRV%d" % k, [1, 128], BF16) for k in range(2)]
            tBRV = toks(2)
            QT = [sb(p1c, "QT%d" % k, [128, S], BF16) for k in range(2)]
            KT = [sb(p1c, "KT%d" % k, [128, S], BF16) for k in range(2)]
            GB = [sb(p1c, "GB%d" % k, [128, S], BF16) for k in range(2)]
            VH = [sb(p1c, "VH%d" % k, [128, NT, 128], BF16) for k in range(2)]
            tQT, tKT, tGB, tVH = [toks(4), toks(4)], [toks(4), toks(4)], [toks(4), toks(4)], [toks(4), toks(4)]
            Rb = [sb(p1c, "Rb%d" % k, [128, S], F32) for k in range(2)]
            Bb = [sb(p1c, "Bb%d" % k, [128, S], BF16) for k in range(4)]
            Pb = [sb(p1c, "Pb%d" % k, [128, S + 2], BF16) for k in range(2)]
            ATb = [sb(p1c, "ATb%d" % k, [128, S], BF16) for k in range(2)]
            tRb, tBb, tATb, tPb = toks(2), toks(4), toks(2), toks(2)
            MT2 = sb(p1c, "MT2c", [128, 2, S], BF16)
            tMT2 = toks(2)

            def load_head_w(h, part=None):
                par = h % 2
                for j, off in enumerate((2048, 3072, 4096, 6144)):
                    if part is None or part == j:
                        dma(pool, WH[par][:, j, :, :], w_in_v[:, :, off + h * 128:off + (h + 1) * 128],
                            writes=[tWH[par]])
                if part is None or part == 4:
                    dma(pool, BRV[par][:], b_in[32 + h:33 + h, :], writes=[tBRV[par]])

            def emit_proj(h):
                par = h % 2
                specs = ((0, QT, tQT, AF.Identity, 16), (1, KT, tKT, AF.Identity, 24), (3, GB, tGB, AF.Sigmoid, 48))
                for (j, DST, tDST, fn_, bcol) in specs:
                    for tg in range(4):
                        pb, tp = PSp[0], tPSp[0]
                        for kc in range(NKC):
                            op(pe, lambda kc=kc, tg=tg, pb=pb, j=j: nc.tensor.matmul(
                                pb[:], lhsT=WH[par][:, j, kc, :], rhs=XT[:, kc, tg * 512:(tg + 1) * 512],
                                start=(kc == 0), stop=(kc == NKC - 1)),
                               reads=[tWH[par]] + tXT[tg * 4:(tg + 1) * 4], writes=[tp], sig=(kc == NKC - 1))
                        op(act, lambda tg=tg, pb=pb, DST=DST, fn_=fn_, bcol=bcol: nc.scalar.activation(
                            DST[par][:, tg * 512:(tg + 1) * 512], pb[:], fn_,
                            bias=BC[:, bcol + h:bcol + h + 1], scale=1.0),
                           reads=[tp, tC], writes=[tDST[par][tg]])
                        yield
                for tg in range(4):
                    pb, tp = PSp[0], tPSp[0]
                    for c4 in range(4):
                        i = tg * 4 + c4
                        for kc in range(NKC):
                            op(pe, lambda kc=kc, i=i, c4=c4, pb=pb: nc.tensor.matmul(
                                pb[:, c4 * 128:(c4 + 1) * 128], lhsT=XT[:, kc, i * 128:(i + 1) * 128],
                                rhs=WH[par][:, 2, kc, :], start=(kc == 0), stop=False),
                               reads=[tWH[par], tXT[i]], writes=[tp], sig=False)
                        op(pe, lambda c4=c4, pb=pb: nc.tensor.matmul(
                            pb[:, c4 * 128:(c4 + 1) * 128], lhsT=ONESB[0:1, :],
                            rhs=BRV[par][0:1, :], start=False, stop=True),
                           reads=[tBRV[par], tC], writes=[tp], sig=(c4 == 3))
                    op(act, lambda tg=tg, pb=pb: nc.scalar.copy(
                        VH[par][:, tg * 4:(tg + 1) * 4, :], pb[:].rearrange("p (c d) -> p c d", c=4)),
                       reads=[tp], writes=[tVH[par][tg]])
                    yield

            def stage_a1_pe(h, i, s_):
                par = h % 2
                nk = 128 * (i + 1)
                nch = (nk + 511) // 512
                base = zbase.get(s_ - 1, (0, 0))
                base = (base[0] + base[1]) % 4
                zbase[s_] = (base, nch)
                for ch in range(nch):
                    k0 = ch * 512
                    w_ = min(512, nk - k0)
                    zi = (base + ch) % 4
                    op(pe, lambda zi=zi, w_=w_, k0=k0: nc.tensor.matmul(
                        PSz[zi][:, 0:w_], lhsT=QT[par][:, i * 128:(i + 1) * 128],
                        rhs=KT[par][:, k0:k0 + w_], start=True, stop=True),
                       reads=[tQT[par][i // 4], tKT[par][ch]], writes=[tPSz[zi]])

            def stage_a1_act(h, i, s_):
                bp, bq = s_ % 2, s_ % 4
                nk = 128 * (i + 1)
                base, nch = zbase[s_]
                for ch in range(nch):
                    k0 = ch * 512
                    w_ = min(512, nk - k0)
                    zi = (base + ch) % 4
                    op(act, lambda zi=zi, k0=k0, w_=w_: nc.scalar.activation(
                        Rb[bp][:, k0:k0 + w_], PSz[zi][:, 0:w_], AF.Sigmoid, scale=-SB_SCALE),
                       reads=[tPSz[zi]], writes=[tRb[bp]])
                    op(act, lambda zi=zi, k0=k0, w_=w_: nc.scalar.activation(
                        Bb[bq][:, k0:k0 + w_], PSz[zi][:, 0:w_], AF.Sigmoid, scale=SB_SCALE),
                       reads=[tPSz[zi]], writes=[tBb[bq]])

            def stage_a2a(h, i, s_):
                bp, bq = s_ % 2, s_ % 4
                nk = 128 * (i + 1)
                d0 = i * 128
                op(pool, lambda: nc.gpsimd.affine_select(
                    out=Rb[bp][:, d0:d0 + 128], in_=Rb[bp][:, d0:d0 + 128], pattern=[[-1, 128]],
                    compare_op=ALU.is_gt, fill=FILL1, base=0, channel_multiplier=1),
                   reads=[tRb[bp]], writes=[tRb[bp]])
                op(pool, lambda: nc.gpsimd.affine_select(
                    out=Bb[bq][:, d0:d0 + 128], in_=Bb[bq][:, d0:d0 + 128], pattern=[[-1, 128]],
                    compare_op=ALU.is_gt, fill=FILL0, base=0, channel_multiplier=1),
                   reads=[tBb[bq]], writes=[tBb[bq]])
                op(pool, lambda: nc.gpsimd.memset(Pb[bp][:, nk + 1:nk + 2], 1.0), writes=[tPb[bp]])
                op(dve, lambda: nc.vector.tensor_tensor_scan(
                    out=Pb[bp][:, 1:nk + 1][:, ::-1], data0=Rb[bp][:, 0:nk][:, ::-1], data1=Rb[bp][:, 0:nk][:, ::-1],
                    initial=1.0, op0=ALU.mult, op1=ALU.min),
                   reads=[tRb[bp]], writes=[tPb[bp]])

            def stage_a2b(h, i, s_):
                bp, bq = s_ % 2, s_ % 4
                nk = 128 * (i + 1)
                op(dve, lambda: nc.vector.tensor_tensor(
                    Bb[bq][:, 0:nk], Bb[bq][:, 0:nk], Pb[bp][:, 2:nk + 2], ALU.mult),
                   reads=[tBb[bq], tPb[bp]], writes=[tBb[bq]])

            def stage_b(h, i, s_):
                par = h % 2
                bp, bq = s_ % 2, s_ % 4
                nb = i + 1
                for bk in range((nb + 7) // 8):
                    b0 = bk * 8
                    nbb = min(8, nb - b0)
                    pt, tpt = PSTr[bk % 2], tPSTr[bk % 2]
                    for b_ in range(nbb):
                        op(pe, lambda b_=b_, b0=b0, pt=pt: nc.tensor.transpose(
                            pt[:, b_ * 128:(b_ + 1) * 128], Bb[bq][:, (b0 + b_) * 128:(b0 + b_ + 1) * 128],
                            identb[:]),
                           reads=[tBb[bq], tC], writes=[tpt], sig=(b_ == nbb - 1))
                    op(act, lambda b0=b0, nbb=nbb, pt=pt: nc.scalar.copy(
                        ATb[bp][:, b0 * 128:(b0 + nbb) * 128], pt[:, 0:nbb * 128]),
                       reads=[tpt], writes=[tATb[bp]])

            def stage_b2(h, i, s_):
                par = h % 2
                bp, bq = s_ % 2, s_ % 4
                nb = i + 1
                c4 = i % 4
                for b_ in range(nb):
                    op(pe, lambda b_=b_: nc.tensor.matmul(
                        PSy[:, c4 * 128:(c4 + 1) * 128], lhsT=VH[par][:, b_, :],
                        rhs=ATb[bp][:, b_ * 128:(b_ + 1) * 128], start=(b_ == 0), stop=(b_ == nb - 1)),
                       reads=[tVH[par][b_ // 4], tATb[bp]], writes=[tPSy], sig=(b_ == nb - 1))
                if c4 == 3:
                    tg = i // 4
                    op(dve, lambda: nc.vector.tensor_tensor(
                        MT2[:, par, tg * 512:(tg + 1) * 512], PSy[:], GB[par][:, tg * 512:(tg + 1) * 512], ALU.mult),
                       reads=[tPSy, tGB[par][tg]], writes=[tMT2[par]])
                if par == 1 and i == NT - 1:
                    out_proj_accum(MT2, tMT2, WO2, tWO2, [PSp[0], PSy], [tPSp[0], tPSy])
                    if h + 1 < 8:
                        dma(pool, WO2[:], w_out[(h + 1) * 128:(h + 3) * 128, :].rearrange("(u p) n -> p u n", p=128),
                            writes=[tWO2])

            tiles = [(h, i) for h in range(8) for i in range(NT)]
            dma(pool, WO2[:], w_out[0:256, :].rearrange("(u p) n -> p u n", p=128), writes=[tWO2])
            load_head_w(0)
            load_head_w(1)
            for _ in emit_proj(0):
                pass
            NTL = len(tiles)
            stage_a1_pe(*tiles[0], 0)
            gen = None
            for s_ in range(NTL + 4):
                if s_ < NTL:
                    h, i = tiles[s_]
                    if 9 <= i <= 13 and h + 2 < 8:
                        load_head_w(h + 2, part=i - 9)
                    if i == 4 and h + 1 < 8:
                        gen = emit_proj(h + 1)
                        gen_n = 0
                    stage_a1_act(h, i, s_)
                if s_ + 1 < NTL:
                    stage_a1_pe(*tiles[s_ + 1], s_ + 1)
                if 0 <= s_ - 1 < NTL:
                    stage_a2a(*tiles[s_ - 1], s_ - 1)
                if 0 <= s_ - 2 < NTL:
                    stage_a2b(*tiles[s_ - 2], s_ - 2)
                if 0 <= s_ - 3 < NTL:
                    stage_b(*tiles[s_ - 3], s_ - 3)
                if 0 <= s_ - 4 < NTL:
                    stage_b2(*tiles[s_ - 4], s_ - 4)
                if gen is not None:
                    for _ in range(2 if gen_n < 12 else 1):
                        try:
                            next(gen)
                            gen_n += 1
                        except StopIteration:
                            gen = None
                            break
            K.barrier()

        if stop == 2:
            dump_x()
            return nc
        pmw = contextlib.ExitStack()
        WKV = sb(pmw, "WKV", [128, NKC, 1024], BF16)
        WQ = sb(pmw, "WQ", [128, NKC, 512], BF16)
        WO = sb(pmw, "WOm", [128, 4, D], BF16)
        MS = sb(pmw, "MS", [128, 2, D], F32)
        tWKV, tWQ, tWO, tMS = Tok(), Tok(), Tok(), Tok()
        dma(pool, WKV[:], w_mkv.rearrange("(c p) n -> p c n", p=128), writes=[tWKV])
        dma(pool, WQ[:], w_mq.rearrange("(c p) n -> p c n", p=128), writes=[tWQ])
        dma(pool, WO[:], w_mo.rearrange("(c p) n -> p c n", p=128), writes=[tWO])
        dma(sp, MS[:], mem_d.rearrange("(m p) n -> p m n", p=128), writes=[tMS])
        with contextlib.ExitStack() as pl1:
            PSt = [ps(pl1, "l1t%d" % k, [128, 512]) for k in range(2)]
            tPSt = toks(2)
            layer_norm_x(pl1, ln1_g, ln1_b, PSt, tPSt, dbg_out=dbg_d.get("d_x1"), post_scale=ALPHA)
            K.barrier()

        if stop == 3:
            pmw.close()
            dump_x()
            return nc
        with contextlib.ExitStack() as p2:
            PSAf = ps(p2, "p2a", [128, 1024])
            PSA = [PSAf[:, 0:512], PSAf[:, 512:1024]]
            tPSA = toks(2)
            PSL = ps(p2, "p2l", [128, 1024])
            tPSL = Tok()
            PSTr = ps(p2, "p2tr", [128, 1024], BF16)
            tPSTr = Tok()
            PSO = ps(p2, "p2o", [128, 512])
            tPSO = Tok()
            PSM = [ps(p2, "p2m%d" % k, [128, 512]) for k in range(2)]
            tPSM = toks(2)
            MTm = sb(p2, "MTm", [128, NKC, 256], BF16)
            tMTm = Tok()
            for mt in range(2):
                for hb in range(2):
                    pb, tp = PSA[hb], tPSA[hb]
                    for c4 in range(4):
                        c = hb * 4 + c4
                        op(pe, lambda mt=mt, c=c, c4=c4, pb=pb: nc.tensor.transpose(
                            pb[:, c4 * 128:(c4 + 1) * 128], MS[:, mt, c * 128:(c + 1) * 128], identf[:]),
                           reads=[tMS, tC], writes=[tp], sig=(c4 == 3))
                    op(act, lambda mt=mt, hb=hb, pb=pb: nc.scalar.copy(
                        MTm[:, hb * 4:(hb + 1) * 4, mt * 128:(mt + 1) * 128],
                        pb[:].rearrange("p (c t) -> p c t", c=4)),
                       reads=[tp], writes=[tMTm])
            KM = sb(p2, "KM", [128, 4, 256], BF16)
            VM = sb(p2, "VM", [128, 2, 512], BF16)
            QM = sb(p2, "QM", [128, 4, S], BF16)
            tKM, tVM, tQM = Tok(), Tok(), toks(4)
            for h in range(4):
                pb, tp = PSA[h % 2], tPSA[h % 2]
                for kc in range(NKC):
                    op(pe, lambda h=h, kc=kc, pb=pb: nc.tensor.matmul(
                        pb[:, 0:256], lhsT=WKV[:, kc, h * 128:(h + 1) * 128], rhs=MTm[:, kc, :],
                        start=(kc == 0), stop=(kc == NKC - 1)),
                       reads=[tWKV, tMTm], writes=[tp], sig=(kc == NKC - 1))
                op(act, lambda h=h, pb=pb: nc.scalar.copy(KM[:, h, :], pb[:, 0:256]), reads=[tp], writes=[tKM])
            for mt in range(2):
                pb, tp = PSA[mt % 2], tPSA[mt % 2]
                for kc in range(NKC):
                    op(pe, lambda mt=mt, kc=kc, pb=pb: nc.tensor.matmul(
                        pb[:], lhsT=MTm[:, kc, mt * 128:(mt + 1) * 128], rhs=WKV[:, kc, 512:1024],
                        start=(kc == 0), stop=(kc == NKC - 1)),
                       reads=[tWKV, tMTm], writes=[tp], sig=(kc == NKC - 1))
                op(act, lambda mt=mt, pb=pb: nc.scalar.copy(VM[:, mt, :], pb[:]), reads=[tp], writes=[tVM])
            for h in range(4):
                for tg in range(4):
                    pb, tp = PSA[tg % 2], tPSA[tg % 2]
                    for kc in range(NKC):
                        op(pe, lambda h=h, tg=tg, kc=kc, pb=pb: nc.tensor.matmul(
                            pb[:], lhsT=WQ[:, kc, h * 128:(h + 1) * 128], rhs=XT[:, kc, tg * 512:(tg + 1) * 512],
                            start=(kc == 0), stop=(kc == NKC - 1)),
                           reads=[tWQ] + tXT[tg * 4:(tg + 1) * 4], writes=[tp], sig=(kc == NKC - 1))
                    op(act, lambda h=h, tg=tg, pb=pb: nc.scalar.copy(QM[:, h, tg * 512:(tg + 1) * 512], pb[:]),
                       reads=[tp], writes=[tQM[tg]])
            MX = [sb(p2, "MX%d" % k, [128, 4], F32) for k in range(2)]
            NMX = [sb(p2, "NMX%d" % k, [128, 4], F32) for k in range(2)]
            SS = [sb(p2, "SS%d" % k, [128, 4], F32) for k in range(2)]
            RSS = [sb(p2, "RSS%d" % k, [128, 4], F32) for k in range(2)]
            Pf = [sb(p2, "Pf%d" % k, [128, 4, 256], F32) for k in range(2)]
            Pn = [sb(p2, "Pn%d" % k, [128, 4, 256], BF16) for k in range(2)]
            PTm = [sb(p2, "PTm%d" % k, [128, 8, 128], BF16) for k in range(2)]
            OTm = [sb(p2, "OTm%d" % k, [128, 4, 128], BF16) for k in range(2)]
            tMX, tNMX, tSS, tRSS, tPf, tPn, tPTm, tOTm = (toks(2) for _ in range(8))
            PSL2 = [PSL, PSAf]
            tPSL2 = [tPSL, Tok()]

            def m_s1(i):
                q = i % 2
                psl, tpsl = PSL2[q], tPSL2[q]
                for h in range(4):
                    op(pe, lambda h=h: nc.tensor.matmul(
                        psl[:, h * 256:(h + 1) * 256], lhsT=QM[:, h, i * 128:(i + 1) * 128], rhs=KM[:, h, :],
                        start=True, stop=True),
                       reads=[tQM[i // 4], tKM], writes=[tpsl], sig=(h == 3))
                op(dve, lambda: nc.vector.tensor_reduce(MX[q][:], psl[:].rearrange("p (h m) -> p h m", h=4),
                                                        AX.X, ALU.max),
                   reads=[tpsl], writes=[tMX[q]])
                op(dve, lambda: nc.vector.tensor_scalar(NMX[q][:], MX[q][:], -MEM_SCALE, None, ALU.mult),
                   reads=[tMX[q]], writes=[tNMX[q]])
                for h in range(4):
                    op(act, lambda h=h: nc.scalar.activation(
                        Pf[q][:, h, :], psl[:, h * 256:(h + 1) * 256], AF.Exp, bias=NMX[q][:, h:h + 1],
                        scale=MEM_SCALE, accum_out=SS[q][:, h:h + 1]),
                       reads=[tpsl, tNMX[q]], writes=[tPf[q], tSS[q]])
                op(dve, lambda: nc.vector.reciprocal(RSS[q][:], SS[q][:]), reads=[tSS[q]], writes=[tRSS[q]])
                for h in range(4):
                    op(dve, lambda h=h: nc.vector.tensor_scalar(Pn[q][:, h, :], Pf[q][:, h, :], RSS[q][:, h:h + 1],
                                                                None, ALU.mult),
                       reads=[tPf[q], tRSS[q]], writes=[tPn[q]])

            def m_s2a(i):
                q = i % 2
                for h in range(4):
                    for mt in range(2):
                        j = h * 2 + mt
                        op(pe, lambda h=h, mt=mt, j=j: nc.tensor.transpose(
                            PSTr[:, j * 128:(j + 1) * 128], Pn[q][:, h, mt * 128:(mt + 1) * 128], identb[:]),
                           reads=[tPn[q], tC], writes=[tPSTr], sig=(j == 7))
                op(act, lambda: nc.scalar.copy(PTm[q][:], PSTr[:].rearrange("p (j t) -> p j t", j=8)),
                   reads=[tPSTr], writes=[tPTm[q]])

            def m_s2b(i):
                q = i % 2
                for h in range(4):
                    for mt in range(2):
                        op(pe, lambda h=h, mt=mt: nc.tensor.matmul(
                            PSO[:, h * 128:(h + 1) * 128], lhsT=VM[:, mt, h * 128:(h + 1) * 128],
                            rhs=PTm[q][:, h * 2 + mt, :], start=(mt == 0), stop=(mt == 1)),
                           reads=[tVM, tPTm[q]], writes=[tPSO], sig=(h == 3 and mt == 1))
                op(act, lambda: nc.scalar.copy(OTm[q][:], PSO[:].rearrange("p (h t) -> p h t", h=4)),
                   reads=[tPSO], writes=[tOTm[q]])

            def m_s3(i):
                q = i % 2
                for hf in range(2):
                    pb, tp = PSM[hf], tPSM[hf]
                    for h in range(4):
                        op(pe, lambda h=h, hf=hf, pb=pb: nc.tensor.matmul(
                            pb[:], lhsT=OTm[q][:, h, :], rhs=WO[:, h, hf * 512:(hf + 1) * 512],
                            start=(h == 0), stop=(h == 3)),
                           reads=[tOTm[q], tWO], writes=[tp], sig=(h == 3))
                    op(dve, lambda hf=hf, pb=pb: nc.vector.tensor_tensor(
                        X[:, i, hf * 512:(hf + 1) * 512], X[:, i, hf * 512:(hf + 1) * 512], pb[:], ALU.add),
                       reads=[tp, tX[i]], writes=[tX[i]])

            K.barrier()
            for s_ in range(NT + 3):
                if s_ < NT:
                    m_s1(s_)
                if 0 <= s_ - 1 < NT:
                    m_s2a(s_ - 1)
                if 0 <= s_ - 2 < NT:
                    m_s2b(s_ - 2)
                if 0 <= s_ - 3 < NT:
                    m_s3(s_ - 3)
            K.barrier()

        pmw.close()
        if stop == 4:
            dump_x()
            return nc
        with contextlib.ExitStack() as p3:
            pxb = contextlib.ExitStack()
            XB = sb(pxb, "XB", [128, NT, D], BF16)
            with contextlib.ExitStack() as p3a:
                PSt = [ps(p3a, "l2t%d" % k, [128, 512]) for k in range(2)]
                tPSt = toks(2)
                PSr = ps(p3a, "l2r", [128, 512])
                tPSr = Tok()
                PSpos = ps(p3a, "l2p", [128, 512])
                tPSpos = Tok()
                WR = sb(p3a, "WR", [128, NKC, NEXP], F32)
                RB = sb(p3a, "RB", [128, NEXP], F32)
                tWR, tRB = Tok(), Tok()
                dma(sp, WR[:], w_r.rearrange("(c p) n -> p c n", p=128), writes=[tWR])
                dma(sp, RB[:], r_b.partition_broadcast(128), writes=[tRB])
                tXB = toks(NT)
                MKB = sb(p3a, "MKB", [128, NT, NEXP], BF16)
                tMKB = toks(NT)
                LT = sb(p3a, "LT", [128, 128], BF16)
                ONESM = sb(p3a, "ONESM", [128, 128], BF16)
                EOFF = sb(p3a, "EOFF", [128, NEXP], F32)
                EOFFI = sb(p3a, "EOFFI", [128, NEXP], mybir.dt.int32)
                tK = Tok()
                op(pool, lambda: nc.gpsimd.affine_select(out=LT[:], in_=onesf[:], pattern=[[1, 128]],
                                                         compare_op=ALU.is_gt, fill=FILL0, base=0,
                                                         channel_multiplier=-1), reads=[tC], writes=[tK])
                op(dve, lambda: nc.vector.memset(ONESM[:], 1.0), writes=[tK])
                op(pool, lambda: nc.gpsimd.iota(EOFFI[:], pattern=[[CAP, NEXP]], base=0, channel_multiplier=0),
                   writes=[tK])
                op(dve, lambda: nc.vector.tensor_copy(EOFF[:], EOFFI[:]), reads=[tK], writes=[tK])
                SCA = sb(p3a, "SCA", [128, NT, NEXP], F32)
                tSCA = toks(NT)
                NR = 4
                PSposL = [PSpos] + [ps(p3a, "l2p%d" % k, [128, 512]) for k in range(NR - 1)]
                tPSposL = [tPSpos] + toks(NR - 1)

                def mkset(r):
                    d = {}
                    for nm, shp in (("SEL", [128, NEXP]), ("SELM", [128, NEXP]), ("T8", [128, 8, 8]), ("GS", [128, 8]),
                                    ("G8", [128, 8]), ("GM", [128, 8]), ("E8", [128, 8]), ("MK", [128, NEXP]),
                                    ("WGt", [128, NEXP]), ("SM", [128, 1]), ("GT_", [128, NEXP]),
                                    ("NMK", [128, NEXP]), ("SLOT", [128, NEXP]), ("N8", [128, 8]),
                                    ("SL8f", [128, 8]), ("JK", [128, NEXP])):
                        d[nm] = sb(p3a, "%s_%d" % (nm, r), shp, F32)
                    d["t"] = Tok()
                    return d
                RS_ = [mkset(r) for r in range(NR)]
                tXF = Tok()
                WRH = sb(p3a, "WRH", [128, NKC, NEXP], BF16)
                WRL = sb(p3a, "WRL", [128, NKC, NEXP], BF16)
                XL = sb(p3a, "XL", [128, NKC, 128], BF16)
                tWRH = Tok()
                op(dve, lambda: nc.vector.tensor_copy(WRH[:], WR[:]), reads=[tWR], writes=[tWRH])
                op(dve, lambda: nc.vector.tensor_tensor(WRL[:], WR[:], WRH[:], ALU.subtract),
                   reads=[tWR, tWRH], writes=[tWRH])

                def router(i, hb, pb, tp):
                    op(dve, lambda: nc.vector.tensor_tensor(
                        XL[:, hb * 4:(hb + 1) * 4, :], pb[:].rearrange("p (c t) -> p c t", c=4),
                        XT[:, hb * 4:(hb + 1) * 4, i * 128:(i + 1) * 128], ALU.subtract),
                       reads=[tp, tXT[i]], writes=[tXF])
                    if hb == 0:
                        op(act, lambda: nc.scalar.copy(XB[:, i, :], X[:, i, :]), reads=[tX[i]], writes=[tXB[i]])
                        return
                    n = 0
                    for (a_hi, wt) in ((True, WRH), (False, WRH), (True, WRL)):
                        for kc in range(NKC):
                            lhs = XT[:, kc, i * 128:(i + 1) * 128] if a_hi else XL[:, kc, :]
                            op(pe, lambda lhs=lhs, wt=wt, kc=kc, n=n: nc.tensor.matmul(
                                PSr[:, 0:NEXP], lhsT=lhs, rhs=wt[:, kc, :], start=(n == 0), stop=(n == 23)),
                               reads=[tXF, tXT[i], tWRH], writes=[tPSr], sig=(n == 23))
                            n += 1
                    op(act, lambda: nc.scalar.activation(SCA[:, i, :], PSr[:, 0:NEXP], AF.Sigmoid),
                       reads=[tPSr], writes=[tSCA[i]])

                def route_chain(i, r):
                    d = RS_[r]
                    SEL, SELM, T8, GS, G8, GM, E8, MK = (d[k] for k in ("SEL", "SELM", "T8", "GS", "G8", "GM", "E8", "MK"))
                    WGt, SM, GT_, NMK, SLOT, N8, SL8f, JK = (d[k] for k in ("WGt", "SM", "GT_", "NMK", "SLOT", "N8", "SL8f", "JK"))
                    tr = d["t"]
                    SC = SCA[:, i, :]
                    V = nc.vector
                    R = dict(reads=[tr], writes=[tr])
                    op(dve, lambda: V.tensor_tensor(SEL[:], SC, RB[:], ALU.add), reads=[tSCA[i], tRB], writes=[tr])
                    yield
                    for g in range(8):
                        op(dve, lambda g=g: V.max(out=T8[:, g, :], in_=SEL[:, g * 8:(g + 1) * 8]), **R)
                        yield
                    op(dve, lambda: V.tensor_tensor(GS[:], T8[:, :, 0], T8[:, :, 1], ALU.add), **R)
                    yield
                    op(dve, lambda: V.max(out=G8[:], in_=GS[:]), **R)
                    yield
                    op(dve, lambda: V.tensor_scalar(GM[:], GS[:], G8[:, 3:4], None, ALU.is_ge), **R)
                    yield
                    op(dve, lambda: V.tensor_scalar(GM[:], GM[:], 1.0, 1.0e4, ALU.subtract, ALU.mult), **R)
                    yield
                    for g in range(8):
                        op(dve, lambda g=g: V.tensor_scalar(SELM[:, g * 8:(g + 1) * 8], SEL[:, g * 8:(g + 1) * 8],
                                                            GM[:, g:g + 1], None, ALU.add), **R)
                        yield
                    op(dve, lambda: V.max(out=E8[:], in_=SELM[:]), **R)
                    yield
                    op(dve, lambda: V.tensor_scalar(MK[:], SELM[:], E8[:, 7:8], None, ALU.is_ge), **R)
                    yield
                    op(dve, lambda: V.tensor_copy(MKB[:, i, :], MK[:]), reads=[tr], writes=[tMKB[i]])
                    yield
                    op(dve, lambda: V.tensor_tensor(WGt[:], SC, MK[:], ALU.mult), **R)
                    yield
                    op(dve, lambda: V.tensor_reduce(SM[:], WGt[:], AX.X, ALU.add), **R)
                    yield
                    op(dve, lambda: V.reciprocal(SM[:], SM[:]), **R)
                    yield
                    op(dve, lambda: V.tensor_scalar(GT_[:], WGt[:], SM[:, 0:1], ROUTED_SCALE, ALU.mult, ALU.mult), **R)
                    yield
                    pp, tpp = PSposL[r], tPSposL[r]
                    op(pe, lambda: nc.tensor.matmul(pp[:, 0:NEXP], lhsT=LT[:], rhs=MKB[:, i, :],
                                                    start=True, stop=(i == 0)),
                       reads=[tK, tMKB[i]], writes=[tpp], sig=(i == 0))
                    for i2 in range(i):
                        op(pe, lambda i2=i2: nc.tensor.matmul(pp[:, 0:NEXP], lhsT=ONESM[:], rhs=MKB[:, i2, :],
                                                              start=False, stop=(i2 == i - 1)),
                           reads=[tK, tMKB[i2]], writes=[tpp], sig=(i2 == i - 1))
                    op(dve, lambda: V.tensor_scalar(NMK[:], MK[:], 1.0, -1.0e6, ALU.subtract, ALU.mult), **R)
                    yield
                    op(dve, lambda: V.tensor_tensor(SLOT[:], pp[:, 0:NEXP], EOFF[:], ALU.add),
                       reads=[tpp, tK, tr], writes=[tr])
                    yield
                    op(dve, lambda: V.tensor_tensor(SLOT[:], SLOT[:], NMK[:], ALU.add), **R)
                    yield
                    op(dve, lambda: V.tensor_scalar(SLOT[:], SLOT[:], -1.0, None, ALU.mult), **R)
                    yield
                    op(dve, lambda: V.max(out=N8[:], in_=SLOT[:]), **R)
                    yield
                    op(dve, lambda: V.tensor_scalar(SL8f[:], N8[:], -1.0, None, ALU.mult), **R)
                    yield
                    op(dve, lambda: V.tensor_copy(SL8I[:, i, :], SL8f[:]), reads=[tr], writes=[tSL[i]])
                    yield
                    for k in range(8):
                        op(dve, lambda k=k: V.scalar_tensor_tensor(
                            out=JK[:], in0=SLOT[:], scalar=N8[:, k:k + 1], in1=GT_[:], op0=ALU.is_equal,
                            op1=ALU.mult, accum_out=G8v[:, i, k:k + 1]),
                           reads=[tr], writes=[tr, tSL[i]])
                        yield
                    for k in range(8):
                        dma(pool, None, None, reads=[tXB[i], tSL[i]] + tZ, writes=[Tok()],
                            fn=lambda k=k: nc.gpsimd.indirect_dma_start(
                                out=XG[:, :], out_offset=bass.IndirectOffsetOnAxis(ap=SL8I[:, i, k:k + 1], axis=0),
                                in_=XB[:, i, :], in_offset=None))
                    yield

                layer_norm_x(p3a, ln2_g, ln2_b, PSt, tPSt, per_tile=router, dbg_out=dbg_d.get("d_x2"), post_scale=ALPHA)
                for base_ in range(0, NT, NR):
                    gens = [route_chain(base_ + r, r) for r in range(NR)]
                    while gens:
                        for g_ in list(gens):
                            try:
                                next(g_)
                            except StopIteration:
                                gens.remove(g_)
                K.barrier(skip_pool_dma=True)

            if stop == 5:
                K.barrier()
                pxb.close()
                dump_x()
                return nc
            with contextlib.ExitStack() as p3s:
                PSg = [ps(p3s, "p3g%d" % k, [128, 512]) for k in range(2)]
                PSu = [ps(p3s, "p3u%d" % k, [128, 512]) for k in range(2)]
                PSd = [ps(p3s, "p3d%d" % k, [128, 512]) for k in range(4)]
                tPSg, tPSu, tPSd = toks(2), toks(2), toks(4)
                WG = sb(p3s, "sWG", [128, NKC, 256], BF16)
                WU = sb(p3s, "sWU", [128, NKC, 256], BF16)
                WD = sb(p3s, "sWD", [128, 2, D], BF16)
                tWG, tWU, tWD = Tok(), Tok(), Tok()
                HT = sb(p3s, "sHT", [128, 2, S], BF16)
                tHT = toks(4)
                SG = [sb(p3s, "sSG%d" % k, [128, 512], BF16) for k in range(2)]
                tSG = toks(2)
                sWGs = sb(p3s, "sWGs", [128, NKC, 256], F32)
                sWUs = sb(p3s, "sWUs", [128, NKC, 256], F32)
                sWDs = sb(p3s, "sWDs", [128, 2, D], F32)
                tsW = toks(3)
                dma(sp, sWGs[:], w_sg.rearrange("(c p) n -> p c n", p=128), writes=[tsW[0]])
                dma(sp, sWUs[:], w_su.rearrange("(c p) n -> p c n", p=128), writes=[tsW[1]])
                dma(sp, sWDs[:], w_sd.rearrange("(c p) n -> p c n", p=128), writes=[tsW[2]])
                op(act, lambda: nc.scalar.copy(WG[:], sWGs[:]), reads=[tsW[0]], writes=[tWG])
                op(dve, lambda: nc.vector.tensor_copy(WU[:], sWUs[:]), reads=[tsW[1]], writes=[tWU])
                op(act, lambda: nc.scalar.copy(WD[:], sWDs[:]), reads=[tsW[2]], writes=[tWD])
                cnt = 0
                for tg in range(4):
                    for hc in range(2):
                        k = cnt % 2
                        cnt += 1
                        for kc in range(NKC):
                            op(pe, lambda kc=kc, tg=tg, hc=hc, k=k: nc.tensor.matmul(
                                PSg[k][:], lhsT=WG[:, kc, hc * 128:(hc + 1) * 128],
                                rhs=XT[:, kc, tg * 512:(tg + 1) * 512], start=(kc == 0), stop=(kc == NKC - 1)),
                               reads=[tWG] + tXT[tg * 4:(tg + 1) * 4], writes=[tPSg[k]], sig=(kc == NKC - 1))
                        for kc in range(NKC):
                            op(pe, lambda kc=kc, tg=tg, hc=hc, k=k: nc.tensor.matmul(
                                PSu[k][:], lhsT=WU[:, kc, hc * 128:(hc + 1) * 128],
                                rhs=XT[:, kc, tg * 512:(tg + 1) * 512], start=(kc == 0), stop=(kc == NKC - 1)),
                               reads=[tWU] + tXT[tg * 4:(tg + 1) * 4], writes=[tPSu[k]], sig=(kc == NKC - 1))
                        op(act, lambda k=k: nc.scalar.activation(SG[k][:], PSg[k][:], AF.Silu),
                           reads=[tPSg[k]], writes=[tSG[k]])
                        op(dve, lambda k=k, tg=tg, hc=hc: nc.vector.tensor_tensor(
                            HT[:, hc, tg * 512:(tg + 1) * 512], SG[k][:], PSu[k][:], ALU.mult),
                           reads=[tSG[k], tPSu[k]], writes=[tHT[tg]])
                for i in range(NT):
                    for hf in range(2):
                        k = (i * 2 + hf) % 4
                        for hc in range(2):
                            op(pe, lambda i=i, hf=hf, hc=hc, k=k: nc.tensor.matmul(
                                PSd[k][:], lhsT=HT[:, hc, i * 128:(i + 1) * 128],
                                rhs=WD[:, hc, hf * 512:(hf + 1) * 512], start=(hc == 0), stop=(hc == 1)),
                               reads=[tHT[i // 4], tWD], writes=[tPSd[k]], sig=(hc == 1))
                        op(dve, lambda i=i, hf=hf, k=k: nc.vector.tensor_tensor(
                            X[:, i, hf * 512:(hf + 1) * 512], X[:, i, hf * 512:(hf + 1) * 512], PSd[k][:], ALU.add),
                           reads=[tPSd[k], tX[i]], writes=[tX[i]])
                K.barrier()

            pxb.close()
            stXT.close()
            with contextlib.ExitStack() as p3b:
                PSTr = [ps(p3b, "p3t%d" % k, [128, 1024], BF16) for k in range(2)]
                PSg = [ps(p3b, "p3g%d" % k, [128, 512]) for k in range(2)]
                PSu = [ps(p3b, "p3u%d" % k, [128, 512]) for k in range(2)]
                PSd = [ps(p3b, "p3d%d" % k, [128, 512]) for k in range(2)]
                tPSTr, tPSg, tPSu, tPSd = toks(2), toks(2), toks(2), toks(2)
                NJ = CAP // 128
                XS = [sb(p3b, "XS%d" % k, [128, NJ, D], BF16) for k in range(2)]
                XGT = [sb(p3b, "XGT%d" % k, [128, NKC, CAP], BF16) for k in range(2)]
                WGs = [sb(p3b, "WGs%d" % k, [128, NKC, 256], F32) for k in range(3)]
                WUs = [sb(p3b, "WUs%d" % k, [128, NKC, 256], F32) for k in range(3)]
                WDs = [sb(p3b, "WDs%d" % k, [128, 2, D], F32) for k in range(3)]
                WG = [sb(p3b, "WG%d" % k, [128, NKC, 256], BF16) for k in range(2)]
                WU = [sb(p3b, "WU%d" % k, [128, NKC, 256], BF16) for k in range(2)]
                WD = [sb(p3b, "WD%d" % k, [128, 2, D], BF16) for k in range(2)]
                HT = [sb(p3b, "HT%d" % k, [128, 2, CAP], BF16) for k in range(2)]
                SG1 = sb(p3b, "SG", [128, CAP], BF16)
                SG = [SG1, SG1]
                YS1 = sb(p3b, "YS", [128, NJ, D], BF16)
                YS = [YS1, YS1]
                tXS, tXGT, tWG, tWU, tWD, tHT = (toks(2) for _ in range(6))
                tSG1, tYS1 = Tok(), Tok()
                tSG, tYS = [tSG1, tSG1], [tYS1, tYS1]
                tWGs, tWUs, tWDs = toks(3), toks(3), toks(3)

                def prefetch_xs(e):
                    par = e % 2
                    dma(sp, XS[par][:], XG[e * CAP:(e + 1) * CAP, :].rearrange("(j p) n -> p j n", p=128),
                        writes=[tXS[par]])

                def prefetch_w(e):
                    p3_ = e % 3
                    dma(sp, WGs[p3_][:], w_eg[e].rearrange("(c p) n -> p c n", p=128), writes=[tWGs[p3_]])
                    dma(sp, WUs[p3_][:], w_eu[e].rearrange("(c p) n -> p c n", p=128), writes=[tWUs[p3_]])
                    dma(sp, WDs[p3_][:], w_ed[e].rearrange("(c p) n -> p c n", p=128), writes=[tWDs[p3_]])

                def cast_w(e):
                    p2_, p3_ = e % 2, e % 3
                    op(act, lambda: nc.scalar.copy(WG[p2_][:], WGs[p3_][:]), reads=[tWGs[p3_]], writes=[tWG[p2_]])
                    op(dve, lambda: nc.vector.tensor_copy(WU[p2_][:], WUs[p3_][:]), reads=[tWUs[p3_]],
                       writes=[tWU[p2_]])
                    op(pool, lambda: nc.gpsimd.tensor_copy(WD[p2_][:], WDs[p3_][:]), reads=[tWDs[p3_]],
                       writes=[tWD[p2_]])

                ev = [0]

                def transposes(e):
                    par = e % 2
                    for j in range(NJ):
                        pt, tpt = PSTr[j % 2], tPSTr[j % 2]
                        for c in range(NKC):
                            op(pe, lambda j=j, c=c, pt=pt: nc.tensor.transpose(
                                pt[:, c * 128:(c + 1) * 128], XS[par][:, j, c * 128:(c + 1) * 128], identb[:]),
                               reads=[tXS[par], tC], writes=[tpt], sig=(c == NKC - 1))
                        ev[0] += 1
                        if ev[0] % 2 == 0:
                            op(act, lambda j=j, pt=pt: nc.scalar.copy(
                                XGT[par][:, :, j * 128:(j + 1) * 128], pt[:].rearrange("p (c t) -> p c t", c=NKC)),
                               reads=[tpt], writes=[tXGT[par]])
                        else:
                            op(dve, lambda j=j, pt=pt: nc.vector.tensor_copy(
                                XGT[par][:, :, j * 128:(j + 1) * 128], pt[:].rearrange("p (c t) -> p c t", c=NKC)),
                               reads=[tpt], writes=[tXGT[par]])

                prefetch_xs(0)
                prefetch_w(0)
                prefetch_w(1)
                prefetch_xs(1)
                cast_w(0)
                transposes(0)
                for e in range(NEXP):
                    par = e % 2
                    if e + 2 < NEXP:
                        prefetch_xs(e + 2)
                        prefetch_w(e + 2)
                    if e + 1 < NEXP:
                        cast_w(e + 1)
                    for hc in range(2):
                        k = hc
                        for kc in range(NKC):
                            op(pe, lambda kc=kc, hc=hc, k=k: nc.tensor.matmul(
                                PSg[k][:], lhsT=WG[par][:, kc, hc * 128:(hc + 1) * 128], rhs=XGT[par][:, kc, :],
                                start=(kc == 0), stop=(kc == NKC - 1)),
                               reads=[tWG[par], tXGT[par]], writes=[tPSg[k]], sig=(kc == NKC - 1))
                        for kc in range(NKC):
                            op(pe, lambda kc=kc, hc=hc, k=k: nc.tensor.matmul(
                                PSu[k][:], lhsT=WU[par][:, kc, hc * 128:(hc + 1) * 128], rhs=XGT[par][:, kc, :],
                                start=(kc == 0), stop=(kc == NKC - 1)),
                               reads=[tWU[par], tXGT[par]], writes=[tPSu[k]], sig=(kc == NKC - 1))
                        op(act, lambda k=k: nc.scalar.activation(SG[k][:], PSg[k][:], AF.Silu),
                           reads=[tPSg[k]], writes=[tSG[k]])
                        op(dve, lambda k=k, hc=hc: nc.vector.tensor_tensor(
                            HT[par][:, hc, :], SG[k][:], PSu[k][:], ALU.mult),
                           reads=[tSG[k], tPSu[k]], writes=[tHT[par]])
                    if e + 1 < NEXP:
                        transposes(e + 1)
                    for j in range(NJ):
                        for hf in range(2):
                            k = (j * 2 + hf) % 2
                            for hc in range(2):
                                op(pe, lambda j=j, hf=hf, hc=hc, k=k: nc.tensor.matmul(
                                    PSd[k][:], lhsT=HT[par][:, hc, j * 128:(j + 1) * 128],
                                    rhs=WD[par][:, hc, hf * 512:(hf + 1) * 512], start=(hc == 0), stop=(hc == 1)),
                                   reads=[tHT[par], tWD[par]], writes=[tPSd[k]], sig=(hc == 1))
                            ev[0] += 1
                            if ev[0] % 2 == 0:
                                op(act, lambda j=j, hf=hf, k=k: nc.scalar.copy(
                                    YS[par][:, j, hf * 512:(hf + 1) * 512], PSd[k][:]),
                                   reads=[tPSd[k]], writes=[tYS[par]])
                            else:
                                op(dve, lambda j=j, hf=hf, k=k: nc.vector.tensor_copy(
                                    YS[par][:, j, hf * 512:(hf + 1) * 512], PSd[k][:]),
                                   reads=[tPSd[k]], writes=[tYS[par]])
                    dma(pool, YG[e * CAP:(e + 1) * CAP, :].rearrange("(j p) n -> p j n", p=128), YS[par][:],
                        reads=[tYS[par]])
                K.barrier()

            with contextlib.ExitStack() as p3c:
                NB = 6
                YR = [sb(p3c, "YR%d" % k, [128, D], BF16) for k in range(NB)]
                tYR = toks(NB)
                n = 0
                for i in range(NT):
                    for k in range(8):
                        bfi = n % NB
                        n += 1
                        dma(pool, None, None, reads=[tSL[i]], writes=[tYR[bfi]],
                            fn=lambda i=i, k=k, bfi=bfi: nc.gpsimd.indirect_dma_start(
                                out=YR[bfi][:], out_offset=None, in_=YG[:, :],
                                in_offset=bass.IndirectOffsetOnAxis(ap=SL8I[:, i, k:k + 1], axis=0)))
                        op(dve, lambda i=i, k=k, bfi=bfi: nc.vector.scalar_tensor_tensor(
                            out=X[:, i, :], in0=YR[bfi][:], scalar=G8v[:, i, k:k + 1], in1=X[:, i, :],
                            op0=ALU.mult, op1=ALU.add),
                           reads=[tYR[bfi], tSL[i], tX[i]], writes=[tX[i]])
                K.barrier()

        with contextlib.ExitStack() as pl3:
            layer_norm_x(pl3, ln3_g, ln3_b, None, None, want_xt=False, out_dram=out_d)
            K.barrier()
    return nc


_NC_CACHE = {}


def _prep_inputs(inputs, b):
    f = lambda a: np.ascontiguousarray(np.asarray(a, dtype=np.float32))
    m = {
        "x": f(inputs["x"][b]),
        "mem": f(inputs["mem"][b]),
        "ln_in_g": f(inputs["ln_in_g"]).reshape(1, D),
        "ln_in_b": f(inputs["ln_in_b"]).reshape(1, D),
        "w_in": f(inputs["w_in"][0]),
        "b_in": f(inputs["b_in"][0]).reshape(56, 128),
        "ln_v_g": f(inputs["ln_v_g"][0]).reshape(1, D),
        "ln_v_b": f(inputs["ln_v_b"][0]).reshape(1, D),
        "w_spatial": f(inputs["w_spatial"][0]),
        "b_spatial": f(inputs["b_spatial"][0]),
        "w_out": f(inputs["w_out"][0]),
        "ln1_g": f(inputs["ln1_g"][0]).reshape(1, D),
        "ln1_b": f(inputs["ln1_b"][0]).reshape(1, D),
        "w_mem_q": f(inputs["w_mem_q"][0]),
        "w_mem_kv": f(inputs["w_mem_kv"][0]),
        "w_mem_o": f(inputs["w_mem_o"][0]),
        "ln2_g": f(inputs["ln2_g"][0]).reshape(1, D),
        "ln2_b": f(inputs["ln2_b"][0]).reshape(1, D),
        "w_router": f(inputs["w_router"][0]),
        "router_bias": f(inputs["router_bias"][0]).reshape(1, NEXP),
        "w_exp_gate": f(inputs["w_exp_gate"][0]),
        "w_exp_up": f(inputs["w_exp_up"][0]),
        "w_exp_down": f(inputs["w_exp_down"][0]),
        "w_sh_gate": f(inputs["w_sh_gate"][0]),
        "w_sh_up": f(inputs["w_sh_up"][0]),
        "w_sh_down": f(inputs["w_sh_down"][0]),
        "ln3_g": f(inputs["ln3_g"][0]).reshape(1, D),
        "ln3_b": f(inputs["ln3_b"][0]).reshape(1, D),
    }
    return m


def kernel(**inputs):
    dbg = bool(os.environ.get("MK_DEBUG"))
    if dbg not in _NC_CACHE:
        _NC_CACHE[dbg] = build(dbg, int(os.environ.get("MK_STOP", "99")))
    nc = _NC_CACHE[dbg]
    shared = _prep_inputs(inputs, 0)
    in_maps = []
    for b in range(8):
        m = dict(shared)
        m["x"] = np.ascontiguousarray(np.asarray(inputs["x"][b], dtype=np.float32))
        m["mem"] = np.ascontiguousarray(np.asarray(inputs["mem"][b], dtype=np.float32))
        in_maps.append(m)
    res = run_bass_kernel_spmd(nc, in_maps, core_ids=list(range(8)))
    out = np.stack([np.asarray(r["out"], dtype=np.float32) for r in res.results], axis=0)
    if dbg:
        kernel.debug = [{k: np.asarray(v) for k, v in r.items()} for r in res.results]
    return out
```

```python
import os
import contextlib
import numpy as np
import ml_dtypes
import concourse.bass as bass
import concourse.mybir as mybir
from concourse.bass_utils import run_bass_kernel_spmd

F32 = mybir.dt.float32
BF16 = mybir.dt.bfloat16
AF = mybir.ActivationFunctionType
ALU = mybir.AluOpType
AX = mybir.AxisListType

S = 2048
D = 1024
NT = 16
NKC = 8
ALPHA = 2.0 ** 0.25
LN_EPS = 1e-5
SB_SCALE = 128.0 ** -0.5
MEM_SCALE = 128.0 ** -0.5
NEXP = 64
ROUTED_SCALE = 2.5
CAP = 512
NSLOT = NEXP * CAP
U32 = mybir.dt.uint32


class Tok:
    __slots__ = ("w", "r")

    def __init__(self):
        self.w = None
        self.r = {}


def toks(n):
    return [Tok() for _ in range(n)]


class Eng:
    def __init__(self, name, h, sem):
        self.name = name
        self.h = h
        self.sem = sem
        self.cnt = 0
        self.known = {}
        self.pool = []
        self.dma_i = 0


class Sched:
    def __init__(self, nc, st, ndma=12):
        self.nc = nc
        mk = lambda n: st.enter_context(nc.semaphore(n))
        self.pe = Eng("pe", nc.tensor, mk("s_pe"))
        self.act = Eng("act", nc.scalar, mk("s_act"))
        self.dve = Eng("dve", nc.vector, mk("s_dve"))
        self.pool = Eng("pool", nc.gpsimd, mk("s_pool"))
        self.sp = Eng("sp", nc.sync, mk("s_sp"))
        self.engs = [self.pe, self.act, self.dve, self.pool, self.sp]
        for q in (self.sp, self.pool, self.act):
            q.pool = [[mk("d_%s_%d" % (q.name, i)), 0] for i in range(ndma)]

    def _emit_waits(self, eng, deps):
        for sem, val in deps.items():
            if eng.known.get(sem, 0) < val:
                if sem is eng.sem:
                    assert val <= eng.cnt, "self-wait on future count"
                eng.h.wait_ge(sem, val)
                eng.known[sem] = val

    def _deps(self, eng, reads, writes):
        deps = {}

        def add(d, raw):
            sem, val = d
            if sem is eng.sem and not raw:
                return
            if deps.get(sem, 0) < val:
                deps[sem] = val

        for t in reads:
            if t.w is not None:
                add(t.w, True)
        for t in writes:
            if t.w is not None:
                add(t.w, False)
            for sem, val in t.r.items():
                add((sem, val), False)
        return deps

    def _mark(self, mark, reads, writes):
        for t in reads:
            if t.r.get(mark[0], 0) < mark[1]:
                t.r[mark[0]] = mark[1]
        for t in writes:
            t.w = mark
            t.r = {}

    def op(self, eng, fn, reads=(), writes=(), sig=True):
        self._emit_waits(eng, self._deps(eng, reads, writes))
        ins = fn()
        if sig:
            ins.then_inc(eng.sem, 1)
            eng.cnt += 1
            mark = (eng.sem, eng.cnt)
        else:
            mark = (eng.sem, eng.cnt + 1)
        self._mark(mark, reads, writes)
        return ins

    def dma(self, q, out, in_, reads=(), writes=(), fn=None, **kw):
        slot = q.pool[q.dma_i % len(q.pool)]
        q.dma_i += 1
        deps = self._deps(q, reads, writes)
        if slot[1] > 0:
            deps[slot[0]] = max(deps.get(slot[0], 0), 16 * slot[1])
        self._emit_waits(q, deps)
        ins = fn() if fn is not None else q.h.dma_start(out=out, in_=in_, **kw)
        ins.then_inc(slot[0], 16)
        slot[1] += 1
        self._mark((slot[0], 16 * slot[1]), reads, writes)
        return ins

    def barrier(self, skip_pool_dma=False):
        targets = {}
        for e in (self.pe, self.act, self.dve, self.pool):
            if e.cnt > 0:
                targets[e.sem] = e.cnt
        for q in ((self.sp, self.act) if skip_pool_dma else (self.sp, self.pool, self.act)):
            for sem, used in q.pool:
                if used > 0:
                    targets[sem] = 16 * used
        for e in self.engs:
            d = {s: v for s, v in targets.items() if not (s is e.sem and e.name == "pe")}
            self._emit_waits(e, d)


def build(dbg=False, stop=99):
    nc = bass.Bass("TRN2", target_bir_lowering=False)

    def din(name, shape):
        return nc.dram_tensor(name, list(shape), F32, kind="ExternalInput").ap()

    x_d = din("x", [S, D])
    mem_d = din("mem", [256, D])
    ln_in_g = din("ln_in_g", [1, D])
    ln_in_b = din("ln_in_b", [1, D])
    w_in = din("w_in", [D, 7168])
    b_in = din("b_in", [56, 128])
    ln_v_g = din("ln_v_g", [1, D])
    ln_v_b = din("ln_v_b", [1, D])
    w_sp = din("w_spatial", [8, 128, 128])
    b_sp = din("b_spatial", [8, 128])
    w_out = din("w_out", [D, D])
    ln1_g = din("ln1_g", [1, D])
    ln1_b = din("ln1_b", [1, D])
    w_mq = din("w_mem_q", [D, 512])
    w_mkv = din("w_mem_kv", [D, 1024])
    w_mo = din("w_mem_o", [512, D])
    ln2_g = din("ln2_g", [1, D])
    ln2_b = din("ln2_b", [1, D])
    w_r = din("w_router", [D, NEXP])
    r_b = din("router_bias", [1, NEXP])
    w_eg = din("w_exp_gate", [NEXP, D, 256])
    w_eu = din("w_exp_up", [NEXP, D, 256])
    w_ed = din("w_exp_down", [NEXP, 256, D])
    w_sg = din("w_sh_gate", [D, 256])
    w_su = din("w_sh_up", [D, 256])
    w_sd = din("w_sh_down", [256, D])
    ln3_g = din("ln3_g", [1, D])
    ln3_b = din("ln3_b", [1, D])
    out_d = nc.dram_tensor("out", [S, D], F32, kind="ExternalOutput").ap()
    XG = nc.dram_tensor("XG_scratch", [NSLOT, D], BF16, kind="Internal").ap()
    YG = nc.dram_tensor("YG_scratch", [NSLOT, D], BF16, kind="Internal").ap()
    dbg_d = {}
    if dbg:
        for nm in ("d_x0", "d_x1", "d_x2"):
            dbg_d[nm] = nc.dram_tensor(nm, [S, D], F32, kind="ExternalOutput").ap()

    w_in_v = w_in.rearrange("(c p) n -> p c n", p=128)

    with contextlib.ExitStack() as st:
        K = Sched(nc, st)
        pe, act, dve, pool, sp = K.pe, K.act, K.dve, K.pool, K.sp
        op, dma = K.op, K.dma

        uid = [0]

        def sb(stk, name, shape, dt):
            uid[0] += 1
            return stk.enter_context(nc.sbuf_tensor("%s_%d" % (name, uid[0]), list(shape), dt))

        def ps(stk, name, shape, dt=F32):
            uid[0] += 1
            return stk.enter_context(nc.psum_tensor("%s_%d" % (name, uid[0]), list(shape), dt))

        X = sb(st, "X", [128, NT, D], F32)
        SL8I = sb(st, "SL8I", [128, NT, 8], U32)
        G8v = sb(st, "G8v", [128, NT, 8], F32)
        tSL = toks(NT)
        tZ = toks(NEXP)
        tX = toks(NT)
        tXT = toks(NT)
        identf = sb(st, "identf", [128, 128], F32)
        identb = sb(st, "identb", [128, 128], BF16)
        onesf = sb(st, "onesf", [128, 128], F32)
        BC = sb(st, "BC", [128, 56], F32)
        ONESB = sb(st, "ONESB", [1, 128], BF16)
        NEGH = sb(st, "NEGH", [128, NT], F32)
        tC = Tok()
        stXT = contextlib.ExitStack()
        XT = sb(stXT, "XT", [128, NKC, S], BF16)

        FILL0 = nc.gpsimd.to_reg(0.0)
        FILL1 = nc.gpsimd.to_reg(1.0)
        op(dve, lambda: nc.vector.memset(onesf[:], 1.0), writes=[tC])
        op(dve, lambda: nc.vector.memset(ONESB[:], 1.0), writes=[tC])
        op(dve, lambda: nc.vector.memset(NEGH[:], -0.5), writes=[tC])
        op(pool, lambda: nc.gpsimd.affine_select(out=identf[:], in_=onesf[:], pattern=[[1, 128]],
                                                 compare_op=ALU.is_equal, fill=FILL0, base=0,
                                                 channel_multiplier=-1), reads=[tC], writes=[tC])
        op(pool, lambda: nc.gpsimd.affine_select(out=identb[:], in_=onesf[:], pattern=[[1, 128]],
                                                 compare_op=ALU.is_equal, fill=FILL0, base=0,
                                                 channel_multiplier=-1), reads=[tC], writes=[tC])

        def layer_norm_x(stk, g_d, b_d, PSt, tPSt, want_xt=True, per_tile=None, out_dram=None, dbg_out=None,
                         post_scale=None):
            G = sb(stk, "lnG", [128, D], F32)
            Bt = sb(stk, "lnB", [128, D], F32)
            ST = sb(stk, "lnST", [128, NT, 12], F32)
            MV = sb(stk, "lnMV", [128, NT, 2], F32)
            RS = sb(stk, "lnRS", [128, NT], F32)
            tG, tB, tS, tM, tR = Tok(), Tok(), Tok(), Tok(), Tok()
            dma(sp, G[:], g_d.partition_broadcast(128), writes=[tG])
            dma(sp, Bt[:], b_d.partition_broadcast(128), writes=[tB])
            for i in range(NT):
                for hf in range(2):
                    op(dve, lambda i=i, hf=hf: nc.vector.bn_stats(ST[:, i, hf * 6:(hf + 1) * 6],
                                                                  X[:, i, hf * 512:(hf + 1) * 512]),
                       reads=[tX[i]], writes=[tS])
                op(dve, lambda i=i: nc.vector.bn_aggr(MV[:, i, :], ST[:, i, :]), reads=[tS], writes=[tM])
            op(dve, lambda: nc.vector.tensor_scalar(RS[:], MV[:, :, 1], LN_EPS, None, ALU.add),
               reads=[tM], writes=[tR])
            op(pool, lambda: nc.gpsimd.tensor_tensor(RS[:], RS[:], NEGH[:], ALU.pow), reads=[tR, tC], writes=[tR])
            for i in range(NT):
                op(dve, lambda i=i: nc.vector.scalar_tensor_tensor(
                    out=X[:, i, :], in0=X[:, i, :], scalar=MV[:, i, 0:1], in1=G[:], op0=ALU.subtract, op1=ALU.mult),
                   reads=[tX[i], tM, tG], writes=[tX[i]])
                op(dve, lambda i=i: nc.vector.scalar_tensor_tensor(
                    out=X[:, i, :], in0=X[:, i, :], scalar=RS[:, i:i + 1], in1=Bt[:], op0=ALU.mult, op1=ALU.add),
                   reads=[tX[i], tR, tB], writes=[tX[i]])
                if dbg_out is not None:
                    dma(sp, dbg_out[i * 128:(i + 1) * 128, :], X[:, i, :], reads=[tX[i]])
                if out_dram is not None:
                    dma(sp, out_dram[i * 128:(i + 1) * 128, :], X[:, i, :], reads=[tX[i]])
                if want_xt:
                    for hb in range(2):
                        pb = PSt[hb]
                        for c4 in range(4):
                            c = hb * 4 + c4
                            op(pe, lambda i=i, c=c, c4=c4, pb=pb: nc.tensor.transpose(
                                pb[:, c4 * 128:(c4 + 1) * 128], X[:, i, c * 128:(c + 1) * 128], identf[:]),
                               reads=[tX[i], tC], writes=[tPSt[hb]], sig=(c4 == 3))
                        op(act, lambda i=i, hb=hb, pb=pb: nc.scalar.copy(
                            XT[:, hb * 4:(hb + 1) * 4, i * 128:(i + 1) * 128],
                            pb[:].rearrange("p (c t) -> p c t", c=4)),
                           reads=[tPSt[hb]], writes=[tXT[i]])
                        if per_tile is not None:
                            per_tile(i, hb, pb, tPSt[hb])
                    if post_scale is not None:
                        op(act, lambda i=i: nc.scalar.mul(X[:, i, :], X[:, i, :], post_scale),
                           reads=[tX[i]], writes=[tX[i]])

        def dump_x():
            for i in range(NT):
                dma(sp, out_d[i * 128:(i + 1) * 128, :], X[:, i, :], reads=[tX[i]])
            K.barrier()
            stXT.close()

        with contextlib.ExitStack() as p0:
            PSt = [ps(p0, "p0t%d" % k, [128, 512]) for k in range(2)]
            tPSt = toks(2)
            for i in range(NT):
                dma(sp, X[:, i, :], x_d[i * 128:(i + 1) * 128, :], writes=[tX[i]])
            BI = sb(p0, "BI", [56, 128], F32)
            tBI = Tok()
            dma(sp, BI[:], b_in[:, :], writes=[tBI])
            PSb = ps(p0, "p0b", [128, 512])
            tPSb = Tok()
            op(pe, lambda: nc.tensor.transpose(PSb[:, 0:56], BI[:], identf[0:56, 0:56]),
               reads=[tBI, tC], writes=[tPSb])
            op(act, lambda: nc.scalar.copy(BC[:], PSb[:, 0:56]), reads=[tPSb], writes=[tC])
            layer_norm_x(p0, ln_in_g, ln_in_b, PSt, tPSt, dbg_out=dbg_d.get("d_x0"), post_scale=ALPHA)
            K.barrier()

        def proj_fm(Wblk, tW, PSbanks, tPS, consume):
            for tg in range(4):
                pb = PSbanks[tg % len(PSbanks)]
                tp = tPS[tg % len(PSbanks)]
                for kc in range(NKC):
                    op(pe, lambda kc=kc, tg=tg, pb=pb: nc.tensor.matmul(
                        pb[:], lhsT=Wblk[:, kc, :], rhs=XT[:, kc, tg * 512:(tg + 1) * 512],
                        start=(kc == 0), stop=(kc == NKC - 1)),
                       reads=[tW] + tXT[tg * 4:(tg + 1) * 4], writes=[tp], sig=(kc == NKC - 1))
                consume(tg, pb, tp)

        def out_proj_accum(MT2, tMT2, WO2, tWO2, PSo, tPSo, nu=2):
            for i in range(NT):
                for hf in range(2):
                    k = (i * 2 + hf) % len(PSo)
                    pb, tp = PSo[k], tPSo[k]
                    for u in range(nu):
                        op(pe, lambda i=i, hf=hf, u=u, pb=pb: nc.tensor.matmul(
                            pb[:, 0:512], lhsT=MT2[:, u, i * 128:(i + 1) * 128], rhs=WO2[:, u, hf * 512:(hf + 1) * 512],
                            start=(u == 0), stop=(u == nu - 1)),
                           reads=[tMT2[u], tWO2], writes=[tp], sig=(u == nu - 1))
                    op(dve, lambda i=i, hf=hf, pb=pb: nc.vector.tensor_tensor(
                        X[:, i, hf * 512:(hf + 1) * 512], X[:, i, hf * 512:(hf + 1) * 512], pb[:, 0:512], ALU.add),
                       reads=[tp, tX[i]], writes=[tX[i]])

        if stop == 0:
            dump_x()
            return nc
        with contextlib.ExitStack() as p1:
            VN = sb(p1, "VN", [128, NT, D], BF16)
            tVN = toks(NT)
            PSa = [ps(p1, "p1a%d" % k, [128, 512]) for k in range(4)]
            tPSa = toks(4)
            PSm = [ps(p1, "p1m%d" % k, [128, 512]) for k in range(2)]
            tPSm = toks(2)
            PSo = [ps(p1, "p1o%d" % k, [128, 512]) for k in range(2)]
            tPSo = toks(2)
            with contextlib.ExitStack() as p1a:
                W2 = [sb(p1a, "Wv%d" % k, [128, NKC, 512], BF16) for k in range(2)]
                BR = sb(p1a, "BRv", [1, D], BF16)
                tW2, tBR = toks(2), Tok()
                G = sb(p1a, "vG", [128, D], F32)
                Bt = sb(p1a, "vB", [128, D], F32)
                ST = sb(p1a, "vST", [128, NT, 12], F32)
                MV = sb(p1a, "vMV", [128, NT, 2], F32)
                RS = sb(p1a, "vRS", [128, NT], F32)
                tG, tB = Tok(), Tok()
                tLv = toks(NT)
                dma(sp, G[:], ln_v_g.partition_broadcast(128), writes=[tG])
                dma(sp, Bt[:], ln_v_b.partition_broadcast(128), writes=[tB])
                dma(pool, BR[:], b_in[8:16, :].rearrange("a b -> (a b)").rearrange("(o n) -> o n", o=1), writes=[tBR])
                for hf in range(2):
                    dma(pool, W2[hf][:], w_in_v[:, :, 1024 + hf * 512:1024 + (hf + 1) * 512], writes=[tW2[hf]])
                for hf in range(2):
                    W, tW = W2[hf], tW2[hf]
                    for i in range(NT):
                        k = i % 4
                        pb, tp = PSa[k], tPSa[k]
                        for kc in range(NKC):
                            op(pe, lambda kc=kc, i=i, pb=pb, W=W: nc.tensor.matmul(
                                pb[:], lhsT=XT[:, kc, i * 128:(i + 1) * 128], rhs=W[:, kc, :],
                                start=(kc == 0), stop=False),
                               reads=[tW, tXT[i]], writes=[tp], sig=False)
                        op(pe, lambda hf=hf, pb=pb: nc.tensor.matmul(
                            pb[:], lhsT=ONESB[0:1, :], rhs=BR[0:1, hf * 512:(hf + 1) * 512], start=False, stop=True),
                           reads=[tBR, tC], writes=[tp])
                        op(act, lambda i=i, hf=hf, pb=pb: nc.scalar.activation(
                            VN[:, i, hf * 512:(hf + 1) * 512], pb[:], AF.Gelu),
                           reads=[tp], writes=[tVN[i]])
                        if hf == 1:
                            tl = tLv[i]
                            for h2 in range(2):
                                op(dve, lambda i=i, h2=h2: nc.vector.bn_stats(ST[:, i, h2 * 6:(h2 + 1) * 6],
                                                                              VN[:, i, h2 * 512:(h2 + 1) * 512]),
                                   reads=[tVN[i]], writes=[tl])
                            op(dve, lambda i=i: nc.vector.bn_aggr(MV[:, i, :], ST[:, i, :]), reads=[tl], writes=[tl])
                            op(dve, lambda i=i: nc.vector.tensor_scalar(RS[:, i:i + 1], MV[:, i, 1:2], LN_EPS, None,
                                                                        ALU.add),
                               reads=[tl], writes=[tl])
                            op(pool, lambda i=i: nc.gpsimd.tensor_tensor(RS[:, i:i + 1], RS[:, i:i + 1], NEGH[:, 0:1],
                                                                         ALU.pow),
                               reads=[tl, tC], writes=[tl])
                            op(dve, lambda i=i: nc.vector.tensor_scalar(VN[:, i, :], VN[:, i, :], MV[:, i, 0:1],
                                                                        RS[:, i:i + 1], ALU.subtract, ALU.mult),
                               reads=[tVN[i], tl], writes=[tVN[i]])
                            op(dve, lambda i=i: nc.vector.tensor_tensor(VN[:, i, :], VN[:, i, :], G[:], ALU.mult),
                               reads=[tVN[i], tG], writes=[tVN[i]])
                            op(pool, lambda i=i: nc.gpsimd.tensor_tensor(VN[:, i, :], VN[:, i, :], Bt[:], ALU.add),
                               reads=[tVN[i], tB], writes=[tVN[i]])
                K.barrier()

            with contextlib.ExitStack() as p1b:
                WT = sb(p1b, "WT", [128, 8, 128], BF16)
                WS = sb(p1b, "WS", [128, 8, 128], F32)
                BS = sb(p1b, "BS", [128, 8, 128], F32)
                tWT, tWS, tBS = Tok(), Tok(), Tok()
                dma(sp, WS[:], w_sp.rearrange("g t s -> t g s"), writes=[tWS])
                dma(sp, BS[:].rearrange("p g t -> p (g t)"),
                    b_sp.rearrange("g t -> (g t)").rearrange("(o n) -> o n", o=1).partition_broadcast(128),
                    writes=[tBS])
                ZT = sb(p1b, "ZT", [128, CAP // 128, D], BF16)
                tZT = Tok()
                op(pool, lambda: nc.gpsimd.memset(ZT[:], 0.0), writes=[tZT])
                for g in range(8):
                    op(pool, lambda g=g: nc.gpsimd.affine_select(out=WS[:, g, :], in_=WS[:, g, :], pattern=[[-1, 128]],
                                                                 compare_op=ALU.is_ge, fill=FILL0, base=0,
                                                                 channel_multiplier=1),
                       reads=[tWS], writes=[tWS])
                for g4 in range(2):
                    pb, tp = PSm[g4], tPSm[g4]
                    for gg in range(4):
                        g = g4 * 4 + gg
                        op(pe, lambda g=g, gg=gg, pb=pb: nc.tensor.transpose(
                            pb[:, gg * 128:(gg + 1) * 128], WS[:, g, :], identf[:]),
                           reads=[tWS, tC], writes=[tp], sig=(gg == 3))
                    op(act, lambda g4=g4, pb=pb: nc.scalar.copy(
                        WT[:, g4 * 4:(g4 + 1) * 4, :], pb[:].rearrange("p (g t) -> p g t", g=4)),
                       reads=[tp], writes=[tWT])

                WB = [sb(p1b, "WB%d" % k, [128, 2, NKC, 128], BF16) for k in range(2)]
                tWB = toks(2)
                WO2 = sb(p1b, "WO4", [128, 4, D], BF16)
                tWO2 = Tok()
                U2 = [sb(p1b, "U%d" % k, [128, S], BF16) for k in range(2)]
                GA2 = [sb(p1b, "GA%d" % k, [128, S], BF16) for k in range(2)]
                T1 = [sb(p1b, "T1%d" % k, [128, 512], BF16) for k in range(2)]
                tU2, tGA2, tT1 = [toks(4), toks(4)], [toks(4), toks(4)], toks(2)
                MT2 = sb(p1b, "MT4", [128, 4, S], BF16)
                tMT2 = toks(4)
                def load_group_w(g):
                    par = g % 2
                    dma(pool, WB[par][:, 0, :, :], w_in_v[:, :, g * 128:(g + 1) * 128], writes=[tWB[par]])
                    dma(pool, WB[par][:, 1, :, :], w_in_v[:, :, 5120 + g * 128:5120 + (g + 1) * 128],
                        writes=[tWB[par]])

                load_group_w(0)
                for g in range(8):
                    par = g % 2
                    g4_ = g % 4
                    U, GA, tU, tGA = U2[par], GA2[par], tU2[par], tGA2[par]
                    if g + 1 < 8:
                        load_group_w(g + 1)
                    if g4_ == 0:
                        dma(pool, WO2[:], w_out[g * 128:(g + 4) * 128, :].rearrange("(u p) n -> p u n", p=128),
                            writes=[tWO2])

                    def cons_u(tg, pb, tp, g=g, U=U, tU=tU):
                        op(act, lambda: nc.scalar.activation(U[:, tg * 512:(tg + 1) * 512], pb[:], AF.Gelu,
                                                             bias=BC[:, g:g + 1], scale=1.0),
                           reads=[tp, tC], writes=[tU[tg]])
                    proj_fm(WB[par][:, 0, :, :], tWB[par], PSa, tPSa, cons_u)
                    for e in range(g * 8, (g + 1) * 8):
                        dma(sp, XG[e * CAP:(e + 1) * CAP, :].rearrange("(j p) n -> p j n", p=128), ZT[:],
                            reads=[tZT, tU[3]], writes=[tZ[e]])

                    def cons_ga(tg, pb, tp, g=g, GA=GA, tGA=tGA):
                        op(act, lambda: nc.scalar.activation(GA[:, tg * 512:(tg + 1) * 512], pb[:], AF.Sigmoid,
                                                             bias=BC[:, 40 + g:41 + g], scale=1.0),
                           reads=[tp, tC], writes=[tGA[tg]])
                    proj_fm(WB[par][:, 1, :, :], tWB[par], PSa, tPSa, cons_ga)

                    for tg in range(4):
                        pb, tp = PSm[tg % 2], tPSm[tg % 2]
                        for c4 in range(4):
                            c = tg * 4 + c4
                            op(pe, lambda c=c, c4=c4, pb=pb, g=g: nc.tensor.matmul(
                                pb[:, c4 * 128:(c4 + 1) * 128], lhsT=VN[:, c, g * 128:(g + 1) * 128],
                                rhs=WT[:, g, :], start=True, stop=True),
                               reads=[tVN[c], tWT], writes=[tp], sig=(c4 == 3))
                        t1 = T1[tg % 2]
                        for c4 in range(4):
                            op(dve, lambda c4=c4, pb=pb, t1=t1, g=g: nc.vector.tensor_tensor(
                                t1[:, c4 * 128:(c4 + 1) * 128], pb[:, c4 * 128:(c4 + 1) * 128], BS[:, g, :], ALU.add),
                               reads=[tp, tBS], writes=[tT1[tg % 2]])
                        op(dve, lambda tg=tg, t1=t1, U=U: nc.vector.tensor_tensor(
                            t1[:], t1[:], U[:, tg * 512:(tg + 1) * 512], ALU.mult),
                           reads=[tT1[tg % 2], tU[tg]], writes=[tT1[tg % 2]])
                        op(pool, lambda tg=tg, t1=t1, g4_=g4_, GA=GA: nc.gpsimd.tensor_tensor(
                            MT2[:, g4_, tg * 512:(tg + 1) * 512], t1[:], GA[:, tg * 512:(tg + 1) * 512], ALU.mult),
                           reads=[tT1[tg % 2], tGA[tg]], writes=[tMT2[g4_]])
                    if g4_ == 3:
                        out_proj_accum(MT2, tMT2, WO2, tWO2, PSo, tPSo, nu=4)
                K.barrier()

        if stop == 1:
            dump_x()
            return nc
        with contextlib.ExitStack() as p1c:
            PSz = [ps(p1c, "pz%d" % k, [128, 512]) for k in range(4)]
            tPSz = toks(4)
            zbase = {}
            PSTr = [ps(p1c, "ptr%d" % k, [128, 1024], BF16) for k in range(2)]
            tPSTr = toks(2)
            PSy = ps(p1c, "py", [128, 512])
            tPSy = Tok()
            PSp = [ps(p1c, "pp", [128, 512])]
            tPSp = toks(1)
            WH = [sb(p1c, "WH%d" % k, [128, 4, NKC, 128], BF16) for k in range(2)]
            tWH = toks(2)
            WO2 = sb(p1c, "WO2c", [128, 2, D], BF16)
            tWO2 = Tok()
            BRV = [sb(p1c, "BRV%d" % k, [1, 128], BF16) for k in range(2)]
            tBRV = toks(2)
            QT = [sb(p1c, "QT%d" % k, [128, S], BF16) for k in range(2)]
            KT = [sb(p1c, "KT%d" % k, [128, S], BF16) for k in range(2)]
            GB = [sb(p1c, "GB%d" % k, [128, S], BF16) for k in range(2)]
            VH = [sb(p1c, "VH%d" % k, [128, NT, 128], BF16) for k in range(2)]
            tQT, tKT, tGB, tVH = [toks(4), toks(4)], [toks(4), toks(4)], [toks(4), toks(4)], [toks(4), toks(4)]
            Rb = [sb(p1c, "Rb%d" % k, [128, S], F32) for k in range(2)]
            Bb = [sb(p1c, "Bb%d" % k, [128, S], BF16) for k in range(4)]
            Pb = [sb(p1c, "Pb%d" % k, [128, S + 2], BF16) for k in range(2)]
            ATb = [sb(p1c, "ATb%d" % k, [128, S], BF16) for k in range(2)]
            tRb, tBb, tATb, tPb = toks(2), toks(4), toks(2), toks(2)
            MT2 = sb(p1c, "MT2c", [128, 2, S], BF16)
            tMT2 = toks(2)

            def load_head_w(h, part=None):
                par = h % 2
                for j, off in enumerate((2048, 3072, 4096, 6144)):
                    if part is None or part == j:
                        dma(pool, WH[par][:, j, :, :], w_in_v[:, :, off + h * 128:off + (h + 1) * 128],
                            writes=[tWH[par]])
                if part is None or part == 4:
                    dma(pool, BRV[par][:], b_in[32 + h:33 + h, :], writes=[tBRV[par]])

            def emit_proj(h):
                par = h % 2
                specs = ((0, QT, tQT, AF.Identity, 16), (1, KT, tKT, AF.Identity, 24), (3, GB, tGB, AF.Sigmoid, 48))
                for (j, DST, tDST, fn_, bcol) in specs:
                    for tg in range(4):
                        pb, tp = PSp[0], tPSp[0]
                        for kc in range(NKC):
                            op(pe, lambda kc=kc, tg=tg, pb=pb, j=j: nc.tensor.matmul(
                                pb[:], lhsT=WH[par][:, j, kc, :], rhs=XT[:, kc, tg * 512:(tg + 1) * 512],
                                start=(kc == 0), stop=(kc == NKC - 1)),
                               reads=[tWH[par]] + tXT[tg * 4:(tg + 1) * 4], writes=[tp], sig=(kc == NKC - 1))
                        op(act, lambda tg=tg, pb=pb, DST=DST, fn_=fn_, bcol=bcol: nc.scalar.activation(
                            DST[par][:, tg * 512:(tg + 1) * 512], pb[:], fn_,
                            bias=BC[:, bcol + h:bcol + h + 1], scale=1.0),
                           reads=[tp, tC], writes=[tDST[par][tg]])
                        yield
                for tg in range(4):
                    pb, tp = PSp[0], tPSp[0]
                    for c4 in range(4):
                        i = tg * 4 + c4
                        for kc in range(NKC):
                            op(pe, lambda kc=kc, i=i, c4=c4, pb=pb: nc.tensor.matmul(
                                pb[:, c4 * 128:(c4 + 1) * 128], lhsT=XT[:, kc, i * 128:(i + 1) * 128],
                                rhs=WH[par][:, 2, kc, :], start=(kc == 0), stop=False),
                               reads=[tWH[par], tXT[i]], writes=[tp], sig=False)
                        op(pe, lambda c4=c4, pb=pb: nc.tensor.matmul(
                            pb[:, c4 * 128:(c4 + 1) * 128], lhsT=ONESB[0:1, :],
                            rhs=BRV[par][0:1, :], start=False, stop=True),
                           reads=[tBRV[par], tC], writes=[tp], sig=(c4 == 3))
                    op(act, lambda tg=tg, pb=pb: nc.scalar.copy(
                        VH[par][:, tg * 4:(tg + 1) * 4, :], pb[:].rearrange("p (c d) -> p c d", c=4)),
                       reads=[tp], writes=[tVH[par][tg]])
                    yield

            def stage_a1_pe(h, i, s_):
                par = h % 2
                nk = 128 * (i + 1)
                nch = (nk + 511) // 512
                base = zbase.get(s_ - 1, (0, 0))
                base = (base[0] + base[1]) % 4
                zbase[s_] = (base, nch)
                for ch in range(nch):
                    k0 = ch * 512
                    w_ = min(512, nk - k0)
                    zi = (base + ch) % 4
                    op(pe, lambda zi=zi, w_=w_, k0=k0: nc.tensor.matmul(
                        PSz[zi][:, 0:w_], lhsT=QT[par][:, i * 128:(i + 1) * 128],
                        rhs=KT[par][:, k0:k0 + w_], start=True, stop=True),
                       reads=[tQT[par][i // 4], tKT[par][ch]], writes=[tPSz[zi]])

            def stage_a1_act(h, i, s_):
                bp, bq = s_ % 2, s_ % 4
                nk = 128 * (i + 1)
                base, nch = zbase[s_]
                for ch in range(nch):
                    k0 = ch * 512
                    w_ = min(512, nk - k0)
                    zi = (base + ch) % 4
                    op(act, lambda zi=zi, k0=k0, w_=w_: nc.scalar.activation(
                        Rb[bp][:, k0:k0 + w_], PSz[zi][:, 0:w_], AF.Sigmoid, scale=-SB_SCALE),
                       reads=[tPSz[zi]], writes=[tRb[bp]])
                    op(act, lambda zi=zi, k0=k0, w_=w_: nc.scalar.activation(
                        Bb[bq][:, k0:k0 + w_], PSz[zi][:, 0:w_], AF.Sigmoid, scale=SB_SCALE),
                       reads=[tPSz[zi]], writes=[tBb[bq]])

            def stage_a2a(h, i, s_):
                bp, bq = s_ % 2, s_ % 4
                nk = 128 * (i + 1)
                d0 = i * 128
                op(pool, lambda: nc.gpsimd.affine_select(
                    out=Rb[bp][:, d0:d0 + 128], in_=Rb[bp][:, d0:d0 + 128], pattern=[[-1, 128]],
                    compare_op=ALU.is_gt, fill=FILL1, base=0, channel_multiplier=1),
                   reads=[tRb[bp]], writes=[tRb[bp]])
                op(pool, lambda: nc.gpsimd.affine_select(
                    out=Bb[bq][:, d0:d0 + 128], in_=Bb[bq][:, d0:d0 + 128], pattern=[[-1, 128]],
                    compare_op=ALU.is_gt, fill=FILL0, base=0, channel_multiplier=1),
                   reads=[tBb[bq]], writes=[tBb[bq]])
                op(pool, lambda: nc.gpsimd.memset(Pb[bp][:, nk + 1:nk + 2], 1.0), writes=[tPb[bp]])
                op(dve, lambda: nc.vector.tensor_tensor_scan(
                    out=Pb[bp][:, 1:nk + 1][:, ::-1], data0=Rb[bp][:, 0:nk][:, ::-1], data1=Rb[bp][:, 0:nk][:, ::-1],
                    initial=1.0, op0=ALU.mult, op1=ALU.min),
                   reads=[tRb[bp]], writes=[tPb[bp]])

            def stage_a2b(h, i, s_):
                bp, bq = s_ % 2, s_ % 4
                nk = 128 * (i + 1)
                op(dve, lambda: nc.vector.tensor_tensor(
                    Bb[bq][:, 0:nk], Bb[bq][:, 0:nk], Pb[bp][:, 2:nk + 2], ALU.mult),
                   reads=[tBb[bq], tPb[bp]], writes=[tBb[bq]])

            def stage_b(h, i, s_):
                par = h % 2
                bp, bq = s_ % 2, s_ % 4
                nb = i + 1
                for bk in range((nb + 7) // 8):
                    b0 = bk * 8
                    nbb = min(8, nb - b0)
                    pt, tpt = PSTr[bk % 2], tPSTr[bk % 2]
                    for b_ in range(nbb):
                        op(pe, lambda b_=b_, b0=b0, pt=pt: nc.tensor.transpose(
                            pt[:, b_ * 128:(b_ + 1) * 128], Bb[bq][:, (b0 + b_) * 128:(b0 + b_ + 1) * 128],
                            identb[:]),
                           reads=[tBb[bq], tC], writes=[tpt], sig=(b_ == nbb - 1))
                    op(act, lambda b0=b0, nbb=nbb, pt=pt: nc.scalar.copy(
                        ATb[bp][:, b0 * 128:(b0 + nbb) * 128], pt[:, 0:nbb * 128]),
                       reads=[tpt], writes=[tATb[bp]])

            def stage_b2(h, i, s_):
                par = h % 2
                bp, bq = s_ % 2, s_ % 4
                nb = i + 1
                c4 = i % 4
                for b_ in range(nb):
                    op(pe, lambda b_=b_: nc.tensor.matmul(
                        PSy[:, c4 * 128:(c4 + 1) * 128], lhsT=VH[par][:, b_, :],
                        rhs=ATb[bp][:, b_ * 128:(b_ + 1) * 128], start=(b_ == 0), stop=(b_ == nb - 1)),
                       reads=[tVH[par][b_ // 4], tATb[bp]], writes=[tPSy], sig=(b_ == nb - 1))
                if c4 == 3:
                    tg = i // 4
                    op(dve, lambda: nc.vector.tensor_tensor(
                        MT2[:, par, tg * 512:(tg + 1) * 512], PSy[:], GB[par][:, tg * 512:(tg + 1) * 512], ALU.mult),
                       reads=[tPSy, tGB[par][tg]], writes=[tMT2[par]])
                if par == 1 and i == NT - 1:
                    out_proj_accum(MT2, tMT2, WO2, tWO2, [PSp[0], PSy], [tPSp[0], tPSy])
                    if h + 1 < 8:
                        dma(pool, WO2[:], w_out[(h + 1) * 128:(h + 3) * 128, :].rearrange("(u p) n -> p u n", p=128),
                            writes=[tWO2])

            tiles = [(h, i) for h in range(8) for i in range(NT)]
            dma(pool, WO2[:], w_out[0:256, :].rearrange("(u p) n -> p u n", p=128), writes=[tWO2])
            load_head_w(0)
            load_head_w(1)
            for _ in emit_proj(0):
                pass
            NTL = len(tiles)
            stage_a1_pe(*tiles[0], 0)
            gen = None
            for s_ in range(NTL + 4):
                if s_ < NTL:
                    h, i = tiles[s_]
                    if 9 <= i <= 13 and h + 2 < 8:
                        load_head_w(h + 2, part=i - 9)
                    if i == 4 and h + 1 < 8:
                        gen = emit_proj(h + 1)
                        gen_n = 0
                    stage_a1_act(h, i, s_)
                if s_ + 1 < NTL:
                    stage_a1_pe(*tiles[s_ + 1], s_ + 1)
                if 0 <= s_ - 1 < NTL:
                    stage_a2a(*tiles[s_ - 1], s_ - 1)
                if 0 <= s_ - 2 < NTL:
                    stage_a2b(*tiles[s_ - 2], s_ - 2)
                if 0 <= s_ - 3 < NTL:
                    stage_b(*tiles[s_ - 3], s_ - 3)
                if 0 <= s_ - 4 < NTL:
                    stage_b2(*tiles[s_ - 4], s_ - 4)
                if gen is not None:
                    for _ in range(2 if gen_n < 12 else 1):
                        try:
                            next(gen)
                            gen_n += 1
                        except StopIteration:
                            gen = None
                            break
            K.barrier()

        if stop == 2:
            dump_x()
            return nc
        pmw = contextlib.ExitStack()
        WKV = sb(pmw, "WKV", [128, NKC, 1024], BF16)
        WQ = sb(pmw, "WQ", [128, NKC, 512], BF16)
        WO = sb(pmw, "WOm", [128, 4, D], BF16)
        MS = sb(pmw, "MS", [128, 2, D], F32)
        tWKV, tWQ, tWO, tMS = Tok(), Tok(), Tok(), Tok()
        dma(pool, WKV[:], w_mkv.rearrange("(c p) n -> p c n", p=128), writes=[tWKV])
        dma(pool, WQ[:], w_mq.rearrange("(c p) n -> p c n", p=128), writes=[tWQ])
        dma(pool, WO[:], w_mo.rearrange("(c p) n -> p c n", p=128), writes=[tWO])
        dma(sp, MS[:], mem_d.rearrange("(m p) n -> p m n", p=128), writes=[tMS])
        with contextlib.ExitStack() as pl1:
            PSt = [ps(pl1, "l1t%d" % k, [128, 512]) for k in range(2)]
            tPSt = toks(2)
            layer_norm_x(pl1, ln1_g, ln1_b, PSt, tPSt, dbg_out=dbg_d.get("d_x1"), post_scale=ALPHA)
            K.barrier()

        if stop == 3:
            pmw.close()
            dump_x()
            return nc
        with contextlib.ExitStack() as p2:
            PSAf = ps(p2, "p2a", [128, 1024])
            PSA = [PSAf[:, 0:512], PSAf[:, 512:1024]]
            tPSA = toks(2)
            PSL = ps(p2, "p2l", [128, 1024])
            tPSL = Tok()
            PSTr = ps(p2, "p2tr", [128, 1024], BF16)
            tPSTr = Tok()
            PSO = ps(p2, "p2o", [128, 512])
            tPSO = Tok()
            PSM = [ps(p2, "p2m%d" % k, [128, 512]) for k in range(2)]
            tPSM = toks(2)
            MTm = sb(p2, "MTm", [128, NKC, 256], BF16)
            tMTm = Tok()
            for mt in range(2):
                for hb in range(2):
                    pb, tp = PSA[hb], tPSA[hb]
                    for c4 in range(4):
                        c = hb * 4 + c4
                        op(pe, lambda mt=mt, c=c, c4=c4, pb=pb: nc.tensor.transpose(
                            pb[:, c4 * 128:(c4 + 1) * 128], MS[:, mt, c * 128:(c + 1) * 128], identf[:]),
                           reads=[tMS, tC], writes=[tp], sig=(c4 == 3))
                    op(act, lambda mt=mt, hb=hb, pb=pb: nc.scalar.copy(
                        MTm[:, hb * 4:(hb + 1) * 4, mt * 128:(mt + 1) * 128],
                        pb[:].rearrange("p (c t) -> p c t", c=4)),
                       reads=[tp], writes=[tMTm])
            KM = sb(p2, "KM", [128, 4, 256], BF16)
            VM = sb(p2, "VM", [128, 2, 512], BF16)
            QM = sb(p2, "QM", [128, 4, S], BF16)
            tKM, tVM, tQM = Tok(), Tok(), toks(4)
            for h in range(4):
                pb, tp = PSA[h % 2], tPSA[h % 2]
                for kc in range(NKC):
                    op(pe, lambda h=h, kc=kc, pb=pb: nc.tensor.matmul(
                        pb[:, 0:256], lhsT=WKV[:, kc, h * 128:(h + 1) * 128], rhs=MTm[:, kc, :],
                        start=(kc == 0), stop=(kc == NKC - 1)),
                       reads=[tWKV, tMTm], writes=[tp], sig=(kc == NKC - 1))
                op(act, lambda h=h, pb=pb: nc.scalar.copy(KM[:, h, :], pb[:, 0:256]), reads=[tp], writes=[tKM])
            for mt in range(2):
                pb, tp = PSA[mt % 2], tPSA[mt % 2]
                for kc in range(NKC):
                    op(pe, lambda mt=mt, kc=kc, pb=pb: nc.tensor.matmul(
                        pb[:], lhsT=MTm[:, kc, mt * 128:(mt + 1) * 128], rhs=WKV[:, kc, 512:1024],
                        start=(kc == 0), stop=(kc == NKC - 1)),
                       reads=[tWKV, tMTm], writes=[tp], sig=(kc == NKC - 1))
                op(act, lambda mt=mt, pb=pb: nc.scalar.copy(VM[:, mt, :], pb[:]), reads=[tp], writes=[tVM])
            for h in range(4):
                for tg in range(4):
                    pb, tp = PSA[tg % 2], tPSA[tg % 2]
                    for kc in range(NKC):
                        op(pe, lambda h=h, tg=tg, kc=kc, pb=pb: nc.tensor.matmul(
                            pb[:], lhsT=WQ[:, kc, h * 128:(h + 1) * 128], rhs=XT[:, kc, tg * 512:(tg + 1) * 512],
                            start=(kc == 0), stop=(kc == NKC - 1)),
                           reads=[tWQ] + tXT[tg * 4:(tg + 1) * 4], writes=[tp], sig=(kc == NKC - 1))
                    op(act, lambda h=h, tg=tg, pb=pb: nc.scalar.copy(QM[:, h, tg * 512:(tg + 1) * 512], pb[:]),
                       reads=[tp], writes=[tQM[tg]])
            MX = [sb(p2, "MX%d" % k, [128, 4], F32) for k in range(2)]
            NMX = [sb(p2, "NMX%d" % k, [128, 4], F32) for k in range(2)]
            SS = [sb(p2, "SS%d" % k, [128, 4], F32) for k in range(2)]
            RSS = [sb(p2, "RSS%d" % k, [128, 4], F32) for k in range(2)]
            Pf = [sb(p2, "Pf%d" % k, [128, 4, 256], F32) for k in range(2)]
            Pn = [sb(p2, "Pn%d" % k, [128, 4, 256], BF16) for k in range(2)]
            PTm = [sb(p2, "PTm%d" % k, [128, 8, 128], BF16) for k in range(2)]
            OTm = [sb(p2, "OTm%d" % k, [128, 4, 128], BF16) for k in range(2)]
            tMX, tNMX, tSS, tRSS, tPf, tPn, tPTm, tOTm = (toks(2) for _ in range(8))
            PSL2 = [PSL, PSAf]
            tPSL2 = [tPSL, Tok()]

            def m_s1(i):
                q = i % 2
                psl, tpsl = PSL2[q], tPSL2[q]
                for h in range(4):
                    op(pe, lambda h=h: nc.tensor.matmul(
                        psl[:, h * 256:(h + 1) * 256], lhsT=QM[:, h, i * 128:(i + 1) * 128], rhs=KM[:, h, :],
                        start=True, stop=True),
                       reads=[tQM[i // 4], tKM], writes=[tpsl], sig=(h == 3))
                op(dve, lambda: nc.vector.tensor_reduce(MX[q][:], psl[:].rearrange("p (h m) -> p h m", h=4),
                                                        AX.X, ALU.max),
                   reads=[tpsl], writes=[tMX[q]])
                op(dve, lambda: nc.vector.tensor_scalar(NMX[q][:], MX[q][:], -MEM_SCALE, None, ALU.mult),
                   reads=[tMX[q]], writes=[tNMX[q]])
                for h in range(4):
                    op(act, lambda h=h: nc.scalar.activation(
                        Pf[q][:, h, :], psl[:, h * 256:(h + 1) * 256], AF.Exp, bias=NMX[q][:, h:h + 1],
                        scale=MEM_SCALE, accum_out=SS[q][:, h:h + 1]),
                       reads=[tpsl, tNMX[q]], writes=[tPf[q], tSS[q]])
                op(dve, lambda: nc.vector.reciprocal(RSS[q][:], SS[q][:]), reads=[tSS[q]], writes=[tRSS[q]])
                for h in range(4):
                    op(dve, lambda h=h: nc.vector.tensor_scalar(Pn[q][:, h, :], Pf[q][:, h, :], RSS[q][:, h:h + 1],
                                                                None, ALU.mult),
                       reads=[tPf[q], tRSS[q]], writes=[tPn[q]])

            def m_s2a(i):
                q = i % 2
                for h in range(4):
                    for mt in range(2):
                        j = h * 2 + mt
                        op(pe, lambda h=h, mt=mt, j=j: nc.tensor.transpose(
                            PSTr[:, j * 128:(j + 1) * 128], Pn[q][:, h, mt * 128:(mt + 1) * 128], identb[:]),
                           reads=[tPn[q], tC], writes=[tPSTr], sig=(j == 7))
                op(act, lambda: nc.scalar.copy(PTm[q][:], PSTr[:].rearrange("p (j t) -> p j t", j=8)),
                   reads=[tPSTr], writes=[tPTm[q]])

            def m_s2b(i):
                q = i % 2
                for h in range(4):
                    for mt in range(2):
                        op(pe, lambda h=h, mt=mt: nc.tensor.matmul(
                            PSO[:, h * 128:(h + 1) * 128], lhsT=VM[:, mt, h * 128:(h + 1) * 128],
                            rhs=PTm[q][:, h * 2 + mt, :], start=(mt == 0), stop=(mt == 1)),
                           reads=[tVM, tPTm[q]], writes=[tPSO], sig=(h == 3 and mt == 1))
                op(act, lambda: nc.scalar.copy(OTm[q][:], PSO[:].rearrange("p (h t) -> p h t", h=4)),
                   reads=[tPSO], writes=[tOTm[q]])

            def m_s3(i):
                q = i % 2
                for hf in range(2):
                    pb, tp = PSM[hf], tPSM[hf]
                    for h in range(4):
                        op(pe, lambda h=h, hf=hf, pb=pb: nc.tensor.matmul(
                            pb[:], lhsT=OTm[q][:, h, :], rhs=WO[:, h, hf * 512:(hf + 1) * 512],
                            start=(h == 0), stop=(h == 3)),
                           reads=[tOTm[q], tWO], writes=[tp], sig=(h == 3))
                    op(dve, lambda hf=hf, pb=pb: nc.vector.tensor_tensor(
                        X[:, i, hf * 512:(hf + 1) * 512], X[:, i, hf * 512:(hf + 1) * 512], pb[:], ALU.add),
                       reads=[tp, tX[i]], writes=[tX[i]])

            K.barrier()
            for s_ in range(NT + 3):
                if s_ < NT:
                    m_s1(s_)
                if 0 <= s_ - 1 < NT:
                    m_s2a(s_ - 1)
                if 0 <= s_ - 2 < NT:
                    m_s2b(s_ - 2)
                if 0 <= s_ - 3 < NT:
                    m_s3(s_ - 3)
            K.barrier()

        pmw.close()
        if stop == 4:
            dump_x()
            return nc
        with contextlib.ExitStack() as p3:
            pxb = contextlib.ExitStack()
            XB = sb(pxb, "XB", [128, NT, D], BF16)
            with contextlib.ExitStack() as p3a:
                PSt = [ps(p3a, "l2t%d" % k, [128, 512]) for k in range(2)]
                tPSt = toks(2)
                PSr = ps(p3a, "l2r", [128, 512])
                tPSr = Tok()
                PSpos = ps(p3a, "l2p", [128, 512])
                tPSpos = Tok()
                WR = sb(p3a, "WR", [128, NKC, NEXP], F32)
                RB = sb(p3a, "RB", [128, NEXP], F32)
                tWR, tRB = Tok(), Tok()
                dma(sp, WR[:], w_r.rearrange("(c p) n -> p c n", p=128), writes=[tWR])
                dma(sp, RB[:], r_b.partition_broadcast(128), writes=[tRB])
                tXB = toks(NT)
                MKB = sb(p3a, "MKB", [128, NT, NEXP], BF16)
                tMKB = toks(NT)
                LT = sb(p3a, "LT", [128, 128], BF16)
                ONESM = sb(p3a, "ONESM", [128, 128], BF16)
                EOFF = sb(p3a, "EOFF", [128, NEXP], F32)
                EOFFI = sb(p3a, "EOFFI", [128, NEXP], mybir.dt.int32)
                tK = Tok()
                op(pool, lambda: nc.gpsimd.affine_select(out=LT[:], in_=onesf[:], pattern=[[1, 128]],
                                                         compare_op=ALU.is_gt, fill=FILL0, base=0,
                                                         channel_multiplier=-1), reads=[tC], writes=[tK])
                op(dve, lambda: nc.vector.memset(ONESM[:], 1.0), writes=[tK])
                op(pool, lambda: nc.gpsimd.iota(EOFFI[:], pattern=[[CAP, NEXP]], base=0, channel_multiplier=0),
                   writes=[tK])
                op(dve, lambda: nc.vector.tensor_copy(EOFF[:], EOFFI[:]), reads=[tK], writes=[tK])
                SCA = sb(p3a, "SCA", [128, NT, NEXP], F32)
                tSCA = toks(NT)
                NR = 4
                PSposL = [PSpos] + [ps(p3a, "l2p%d" % k, [128, 512]) for k in range(NR - 1)]
                tPSposL = [tPSpos] + toks(NR - 1)

                def mkset(r):
                    d = {}
                    for nm, shp in (("SEL", [128, NEXP]), ("SELM", [128, NEXP]), ("T8", [128, 8, 8]), ("GS", [128, 8]),
                                    ("G8", [128, 8]), ("GM", [128, 8]), ("E8", [128, 8]), ("MK", [128, NEXP]),
                                    ("WGt", [128, NEXP]), ("SM", [128, 1]), ("GT_", [128, NEXP]),
                                    ("NMK", [128, NEXP]), ("SLOT", [128, NEXP]), ("N8", [128, 8]),
                                    ("SL8f", [128, 8]), ("JK", [128, NEXP])):
                        d[nm] = sb(p3a, "%s_%d" % (nm, r), shp, F32)
                    d["t"] = Tok()
                    return d
                RS_ = [mkset(r) for r in range(NR)]
                tXF = Tok()
                WRH = sb(p3a, "WRH", [128, NKC, NEXP], BF16)
                WRL = sb(p3a, "WRL", [128, NKC, NEXP], BF16)
                XL = sb(p3a, "XL", [128, NKC, 128], BF16)
                tWRH = Tok()
                op(dve, lambda: nc.vector.tensor_copy(WRH[:], WR[:]), reads=[tWR], writes=[tWRH])
                op(dve, lambda: nc.vector.tensor_tensor(WRL[:], WR[:], WRH[:], ALU.subtract),
                   reads=[tWR, tWRH], writes=[tWRH])

                def router(i, hb, pb, tp):
                    op(dve, lambda: nc.vector.tensor_tensor(
                        XL[:, hb * 4:(hb + 1) * 4, :], pb[:].rearrange("p (c t) -> p c t", c=4),
                        XT[:, hb * 4:(hb + 1) * 4, i * 128:(i + 1) * 128], ALU.subtract),
                       reads=[tp, tXT[i]], writes=[tXF])
                    if hb == 0:
                        op(act, lambda: nc.scalar.copy(XB[:, i, :], X[:, i, :]), reads=[tX[i]], writes=[tXB[i]])
                        return
                    n = 0
                    for (a_hi, wt) in ((True, WRH), (False, WRH), (True, WRL)):
                        for kc in range(NKC):
                            lhs = XT[:, kc, i * 128:(i + 1) * 128] if a_hi else XL[:, kc, :]
                            op(pe, lambda lhs=lhs, wt=wt, kc=kc, n=n: nc.tensor.matmul(
                                PSr[:, 0:NEXP], lhsT=lhs, rhs=wt[:, kc, :], start=(n == 0), stop=(n == 23)),
                               reads=[tXF, tXT[i], tWRH], writes=[tPSr], sig=(n == 23))
                            n += 1
                    op(act, lambda: nc.scalar.activation(SCA[:, i, :], PSr[:, 0:NEXP], AF.Sigmoid),
                       reads=[tPSr], writes=[tSCA[i]])

                def route_chain(i, r):
                    d = RS_[r]
                    SEL, SELM, T8, GS, G8, GM, E8, MK = (d[k] for k in ("SEL", "SELM", "T8", "GS", "G8", "GM", "E8", "MK"))
                    WGt, SM, GT_, NMK, SLOT, N8, SL8f, JK = (d[k] for k in ("WGt", "SM", "GT_", "NMK", "SLOT", "N8", "SL8f", "JK"))
                    tr = d["t"]
                    SC = SCA[:, i, :]
                    V = nc.vector
                    R = dict(reads=[tr], writes=[tr])
                    op(dve, lambda: V.tensor_tensor(SEL[:], SC, RB[:], ALU.add), reads=[tSCA[i], tRB], writes=[tr])
                    yield
                    for g in range(8):
                        op(dve, lambda g=g: V.max(out=T8[:, g, :], in_=SEL[:, g * 8:(g + 1) * 8]), **R)
                        yield
                    op(dve, lambda: V.tensor_tensor(GS[:], T8[:, :, 0], T8[:, :, 1], ALU.add), **R)
                    yield
                    op(dve, lambda: V.max(out=G8[:], in_=GS[:]), **R)
                    yield
                    op(dve, lambda: V.tensor_scalar(GM[:], GS[:], G8[:, 3:4], None, ALU.is_ge), **R)
                    yield
                    op(dve, lambda: V.tensor_scalar(GM[:], GM[:], 1.0, 1.0e4, ALU.subtract, ALU.mult), **R)
                    yield
                    for g in range(8):
                        op(dve, lambda g=g: V.tensor_scalar(SELM[:, g * 8:(g + 1) * 8], SEL[:, g * 8:(g + 1) * 8],
                                                            GM[:, g:g + 1], None, ALU.add), **R)
                        yield
                    op(dve, lambda: V.max(out=E8[:], in_=SELM[:]), **R)
                    yield
                    op(dve, lambda: V.tensor_scalar(MK[:], SELM[:], E8[:, 7:8], None, ALU.is_ge), **R)
                    yield
                    op(dve, lambda: V.tensor_copy(MKB[:, i, :], MK[:]), reads=[tr], writes=[tMKB[i]])
                    yield
                    op(dve, lambda: V.tensor_tensor(WGt[:], SC, MK[:], ALU.mult), **R)
                    yield
                    op(dve, lambda: V.tensor_reduce(SM[:], WGt[:], AX.X, ALU.add), **R)
                    yield
                    op(dve, lambda: V.reciprocal(SM[:], SM[:]), **R)
                    yield
                    op(dve, lambda: V.tensor_scalar(GT_[:], WGt[:], SM[:, 0:1], ROUTED_SCALE, ALU.mult, ALU.mult), **R)
                    yield
                    pp, tpp = PSposL[r], tPSposL[r]
                    op(pe, lambda: nc.tensor.matmul(pp[:, 0:NEXP], lhsT=LT[:], rhs=MKB[:, i, :],
                                                    start=True, stop=(i == 0)),
                       reads=[tK, tMKB[i]], writes=[tpp], sig=(i == 0))
                    for i2 in range(i):
                        op(pe, lambda i2=i2: nc.tensor.matmul(pp[:, 0:NEXP], lhsT=ONESM[:], rhs=MKB[:, i2, :],
                                                              start=False, stop=(i2 == i - 1)),
                           reads=[tK, tMKB[i2]], writes=[tpp], sig=(i2 == i - 1))
                    op(dve, lambda: V.tensor_scalar(NMK[:], MK[:], 1.0, -1.0e6, ALU.subtract, ALU.mult), **R)
                    yield
                    op(dve, lambda: V.tensor_tensor(SLOT[:], pp[:, 0:NEXP], EOFF[:], ALU.add),
                       reads=[tpp, tK, tr], writes=[tr])
                    yield
                    op(dve, lambda: V.tensor_tensor(SLOT[:], SLOT[:], NMK[:], ALU.add), **R)
                    yield
                    op(dve, lambda: V.tensor_scalar(SLOT[:], SLOT[:], -1.0, None, ALU.mult), **R)
                    yield
                    op(dve, lambda: V.max(out=N8[:], in_=SLOT[:]), **R)
                    yield
                    op(dve, lambda: V.tensor_scalar(SL8f[:], N8[:], -1.0, None, ALU.mult), **R)
                    yield
                    op(dve, lambda: V.tensor_copy(SL8I[:, i, :], SL8f[:]), reads=[tr], writes=[tSL[i]])
                    yield
                    for k in range(8):
                        op(dve, lambda k=k: V.scalar_tensor_tensor(
                            out=JK[:], in0=SLOT[:], scalar=N8[:, k:k + 1], in1=GT_[:], op0=ALU.is_equal,
                            op1=ALU.mult, accum_out=G8v[:, i, k:k + 1]),
                           reads=[tr], writes=[tr, tSL[i]])
                        yield
                    for k in range(8):
                        dma(pool, None, None, reads=[tXB[i], tSL[i]] + tZ, writes=[Tok()],
                            fn=lambda k=k: nc.gpsimd.indirect_dma_start(
                                out=XG[:, :], out_offset=bass.IndirectOffsetOnAxis(ap=SL8I[:, i, k:k + 1], axis=0),
                                in_=XB[:, i, :], in_offset=None))
                    yield

                layer_norm_x(p3a, ln2_g, ln2_b, PSt, tPSt, per_tile=router, dbg_out=dbg_d.get("d_x2"), post_scale=ALPHA)
                for base_ in range(0, NT, NR):
                    gens = [route_chain(base_ + r, r) for r in range(NR)]
                    while gens:
                        for g_ in list(gens):
                            try:
                                next(g_)
                            except StopIteration:
                                gens.remove(g_)
                K.barrier(skip_pool_dma=True)

            if stop == 5:
                K.barrier()
                pxb.close()
                dump_x()
                return nc
            with contextlib.ExitStack() as p3s:
                PSg = [ps(p3s, "p3g%d" % k, [128, 512]) for k in range(2)]
                PSu = [ps(p3s, "p3u%d" % k, [128, 512]) for k in range(2)]
                PSd = [ps(p3s, "p3d%d" % k, [128, 512]) for k in range(4)]
                tPSg, tPSu, tPSd = toks(2), toks(2), toks(4)
                WG = sb(p3s, "sWG", [128, NKC, 256], BF16)
                WU = sb(p3s, "sWU", [128, NKC, 256], BF16)
                WD = sb(p3s, "sWD", [128, 2, D], BF16)
                tWG, tWU, tWD = Tok(), Tok(), Tok()
                HT = sb(p3s, "sHT", [128, 2, S], BF16)
                tHT = toks(4)
                SG = [sb(p3s, "sSG%d" % k, [128, 512], BF16) for k in range(2)]
                tSG = toks(2)
                sWGs = sb(p3s, "sWGs", [128, NKC, 256], F32)
                sWUs = sb(p3s, "sWUs", [128, NKC, 256], F32)
                sWDs = sb(p3s, "sWDs", [128, 2, D], F32)
                tsW = toks(3)
                dma(sp, sWGs[:], w_sg.rearrange("(c p) n -> p c n", p=128), writes=[tsW[0]])
                dma(sp, sWUs[:], w_su.rearrange("(c p) n -> p c n", p=128), writes=[tsW[1]])
                dma(sp, sWDs[:], w_sd.rearrange("(c p) n -> p c n", p=128), writes=[tsW[2]])
                op(act, lambda: nc.scalar.copy(WG[:], sWGs[:]), reads=[tsW[0]], writes=[tWG])
                op(dve, lambda: nc.vector.tensor_copy(WU[:], sWUs[:]), reads=[tsW[1]], writes=[tWU])
                op(act, lambda: nc.scalar.copy(WD[:], sWDs[:]), reads=[tsW[2]], writes=[tWD])
                cnt = 0
                for tg in range(4):
                    for hc in range(2):
                        k = cnt % 2
                        cnt += 1
                        for kc in range(NKC):
                            op(pe, lambda kc=kc, tg=tg, hc=hc, k=k: nc.tensor.matmul(
                                PSg[k][:], lhsT=WG[:, kc, hc * 128:(hc + 1) * 128],
                                rhs=XT[:, kc, tg * 512:(tg + 1) * 512], start=(kc == 0), stop=(kc == NKC - 1)),
                               reads=[tWG] + tXT[tg * 4:(tg + 1) * 4], writes=[tPSg[k]], sig=(kc == NKC - 1))
                        for kc in range(NKC):
                            op(pe, lambda kc=kc, tg=tg, hc=hc, k=k: nc.tensor.matmul(
                                PSu[k][:], lhsT=WU[:, kc, hc * 128:(hc + 1) * 128],
                                rhs=XT[:, kc, tg * 512:(tg + 1) * 512], start=(kc == 0), stop=(kc == NKC - 1)),
                               reads=[tWU] + tXT[tg * 4:(tg + 1) * 4], writes=[tPSu[k]], sig=(kc == NKC - 1))
                        op(act, lambda k=k: nc.scalar.activation(SG[k][:], PSg[k][:], AF.Silu),
                           reads=[tPSg[k]], writes=[tSG[k]])
                        op(dve, lambda k=k, tg=tg, hc=hc: nc.vector.tensor_tensor(
                            HT[:, hc, tg * 512:(tg + 1) * 512], SG[k][:], PSu[k][:], ALU.mult),
                           reads=[tSG[k], tPSu[k]], writes=[tHT[tg]])
                for i in range(NT):
                    for hf in range(2):
                        k = (i * 2 + hf) % 4
                        for hc in range(2):
                            op(pe, lambda i=i, hf=hf, hc=hc, k=k: nc.tensor.matmul(
                                PSd[k][:], lhsT=HT[:, hc, i * 128:(i + 1) * 128],
                                rhs=WD[:, hc, hf * 512:(hf + 1) * 512], start=(hc == 0), stop=(hc == 1)),
                               reads=[tHT[i // 4], tWD], writes=[tPSd[k]], sig=(hc == 1))
                        op(dve, lambda i=i, hf=hf, k=k: nc.vector.tensor_tensor(
                            X[:, i, hf * 512:(hf + 1) * 512], X[:, i, hf * 512:(hf + 1) * 512], PSd[k][:], ALU.add),
                           reads=[tPSd[k], tX[i]], writes=[tX[i]])
                K.barrier()

            pxb.close()
            stXT.close()
            with contextlib.ExitStack() as p3b:
                PSTr = [ps(p3b, "p3t%d" % k, [128, 1024], BF16) for k in range(2)]
                PSg = [ps(p3b, "p3g%d" % k, [128, 512]) for k in range(2)]
                PSu = [ps(p3b, "p3u%d" % k, [128, 512]) for k in range(2)]
                PSd = [ps(p3b, "p3d%d" % k, [128, 512]) for k in range(2)]
                tPSTr, tPSg, tPSu, tPSd = toks(2), toks(2), toks(2), toks(2)
                NJ = CAP // 128
                XS = [sb(p3b, "XS%d" % k, [128, NJ, D], BF16) for k in range(2)]
                XGT = [sb(p3b, "XGT%d" % k, [128, NKC, CAP], BF16) for k in range(2)]
                WGs = [sb(p3b, "WGs%d" % k, [128, NKC, 256], F32) for k in range(3)]
                WUs = [sb(p3b, "WUs%d" % k, [128, NKC, 256], F32) for k in range(3)]
                WDs = [sb(p3b, "WDs%d" % k, [128, 2, D], F32) for k in range(3)]
                WG = [sb(p3b, "WG%d" % k, [128, NKC, 256], BF16) for k in range(2)]
                WU = [sb(p3b, "WU%d" % k, [128, NKC, 256], BF16) for k in range(2)]
                WD = [sb(p3b, "WD%d" % k, [128, 2, D], BF16) for k in range(2)]
                HT = [sb(p3b, "HT%d" % k, [128, 2, CAP], BF16) for k in range(2)]
                SG1 = sb(p3b, "SG", [128, CAP], BF16)
                SG = [SG1, SG1]
                YS1 = sb(p3b, "YS", [128, NJ, D], BF16)
                YS = [YS1, YS1]
                tXS, tXGT, tWG, tWU, tWD, tHT = (toks(2) for _ in range(6))
                tSG1, tYS1 = Tok(), Tok()
                tSG, tYS = [tSG1, tSG1], [tYS1, tYS1]
                tWGs, tWUs, tWDs = toks(3), toks(3), toks(3)

                def prefetch_xs(e):
                    par = e % 2
                    dma(sp, XS[par][:], XG[e * CAP:(e + 1) * CAP, :].rearrange("(j p) n -> p j n", p=128),
                        writes=[tXS[par]])

                def prefetch_w(e):
                    p3_ = e % 3
                    dma(sp, WGs[p3_][:], w_eg[e].rearrange("(c p) n -> p c n", p=128), writes=[tWGs[p3_]])
                    dma(sp, WUs[p3_][:], w_eu[e].rearrange("(c p) n -> p c n", p=128), writes=[tWUs[p3_]])
                    dma(sp, WDs[p3_][:], w_ed[e].rearrange("(c p) n -> p c n", p=128), writes=[tWDs[p3_]])

                def cast_w(e):
                    p2_, p3_ = e % 2, e % 3
                    op(act, lambda: nc.scalar.copy(WG[p2_][:], WGs[p3_][:]), reads=[tWGs[p3_]], writes=[tWG[p2_]])
                    op(dve, lambda: nc.vector.tensor_copy(WU[p2_][:], WUs[p3_][:]), reads=[tWUs[p3_]],
                       writes=[tWU[p2_]])
                    op(pool, lambda: nc.gpsimd.tensor_copy(WD[p2_][:], WDs[p3_][:]), reads=[tWDs[p3_]],
                       writes=[tWD[p2_]])

                ev = [0]

                def transposes(e):
                    par = e % 2
                    for j in range(NJ):
                        pt, tpt = PSTr[j % 2], tPSTr[j % 2]
                        for c in range(NKC):
                            op(pe, lambda j=j, c=c, pt=pt: nc.tensor.transpose(
                                pt[:, c * 128:(c + 1) * 128], XS[par][:, j, c * 128:(c + 1) * 128], identb[:]),
                               reads=[tXS[par], tC], writes=[tpt], sig=(c == NKC - 1))
                        ev[0] += 1
                        if ev[0] % 2 == 0:
                            op(act, lambda j=j, pt=pt: nc.scalar.copy(
                                XGT[par][:, :, j * 128:(j + 1) * 128], pt[:].rearrange("p (c t) -> p c t", c=NKC)),
                               reads=[tpt], writes=[tXGT[par]])
                        else:
                            op(dve, lambda j=j, pt=pt: nc.vector.tensor_copy(
                                XGT[par][:, :, j * 128:(j + 1) * 128], pt[:].rearrange("p (c t) -> p c t", c=NKC)),
                               reads=[tpt], writes=[tXGT[par]])

                prefetch_xs(0)
                prefetch_w(0)
                prefetch_w(1)
                prefetch_xs(1)
                cast_w(0)
                transposes(0)
                for e in range(NEXP):
                    par = e % 2
                    if e + 2 < NEXP:
                        prefetch_xs(e + 2)
                        prefetch_w(e + 2)
                    if e + 1 < NEXP:
                        cast_w(e + 1)
                    for hc in range(2):
                        k = hc
                        for kc in range(NKC):
                            op(pe, lambda kc=kc, hc=hc, k=k: nc.tensor.matmul(
                                PSg[k][:], lhsT=WG[par][:, kc, hc * 128:(hc + 1) * 128], rhs=XGT[par][:, kc, :],
                                start=(kc == 0), stop=(kc == NKC - 1)),
                               reads=[tWG[par], tXGT[par]], writes=[tPSg[k]], sig=(kc == NKC - 1))
                        for kc in range(NKC):
                            op(pe, lambda kc=kc, hc=hc, k=k: nc.tensor.matmul(
                                PSu[k][:], lhsT=WU[par][:, kc, hc * 128:(hc + 1) * 128], rhs=XGT[par][:, kc, :],
                                start=(kc == 0), stop=(kc == NKC - 1)),
                               reads=[tWU[par], tXGT[par]], writes=[tPSu[k]], sig=(kc == NKC - 1))
                        op(act, lambda k=k: nc.scalar.activation(SG[k][:], PSg[k][:], AF.Silu),
                           reads=[tPSg[k]], writes=[tSG[k]])
                        op(dve, lambda k=k, hc=hc: nc.vector.tensor_tensor(
                            HT[par][:, hc, :], SG[k][:], PSu[k][:], ALU.mult),
                           reads=[tSG[k], tPSu[k]], writes=[tHT[par]])
                    if e + 1 < NEXP:
                        transposes(e + 1)
                    for j in range(NJ):
                        for hf in range(2):
                            k = (j * 2 + hf) % 2
                            for hc in range(2):
                                op(pe, lambda j=j, hf=hf, hc=hc, k=k: nc.tensor.matmul(
                                    PSd[k][:], lhsT=HT[par][:, hc, j * 128:(j + 1) * 128],
                                    rhs=WD[par][:, hc, hf * 512:(hf + 1) * 512], start=(hc == 0), stop=(hc == 1)),
                                   reads=[tHT[par], tWD[par]], writes=[tPSd[k]], sig=(hc == 1))
                            ev[0] += 1
                            if ev[0] % 2 == 0:
                                op(act, lambda j=j, hf=hf, k=k: nc.scalar.copy(
                                    YS[par][:, j, hf * 512:(hf + 1) * 512], PSd[k][:]),
                                   reads=[tPSd[k]], writes=[tYS[par]])
                            else:
                                op(dve, lambda j=j, hf=hf, k=k: nc.vector.tensor_copy(
                                    YS[par][:, j, hf * 512:(hf + 1) * 512], PSd[k][:]),
                                   reads=[tPSd[k]], writes=[tYS[par]])
                    dma(pool, YG[e * CAP:(e + 1) * CAP, :].rearrange("(j p) n -> p j n", p=128), YS[par][:],
                        reads=[tYS[par]])
                K.barrier()

            with contextlib.ExitStack() as p3c:
                NB = 6
                YR = [sb(p3c, "YR%d" % k, [128, D], BF16) for k in range(NB)]
                tYR = toks(NB)
                n = 0
                for i in range(NT):
                    for k in range(8):
                        bfi = n % NB
                        n += 1
                        dma(pool, None, None, reads=[tSL[i]], writes=[tYR[bfi]],
                            fn=lambda i=i, k=k, bfi=bfi: nc.gpsimd.indirect_dma_start(
                                out=YR[bfi][:], out_offset=None, in_=YG[:, :],
                                in_offset=bass.IndirectOffsetOnAxis(ap=SL8I[:, i, k:k + 1], axis=0)))
                        op(dve, lambda i=i, k=k, bfi=bfi: nc.vector.scalar_tensor_tensor(
                            out=X[:, i, :], in0=YR[bfi][:], scalar=G8v[:, i, k:k + 1], in1=X[:, i, :],
                            op0=ALU.mult, op1=ALU.add),
                           reads=[tYR[bfi], tSL[i], tX[i]], writes=[tX[i]])
                K.barrier()

        with contextlib.ExitStack() as pl3:
            layer_norm_x(pl3, ln3_g, ln3_b, None, None, want_xt=False, out_dram=out_d)
            K.barrier()
    return nc


_NC_CACHE = {}


def _prep_inputs(inputs, b):
    f = lambda a: np.ascontiguousarray(np.asarray(a, dtype=np.float32))
    m = {
        "x": f(inputs["x"][b]),
        "mem": f(inputs["mem"][b]),
        "ln_in_g": f(inputs["ln_in_g"]).reshape(1, D),
        "ln_in_b": f(inputs["ln_in_b"]).reshape(1, D),
        "w_in": f(inputs["w_in"][0]),
        "b_in": f(inputs["b_in"][0]).reshape(56, 128),
        "ln_v_g": f(inputs["ln_v_g"][0]).reshape(1, D),
        "ln_v_b": f(inputs["ln_v_b"][0]).reshape(1, D),
        "w_spatial": f(inputs["w_spatial"][0]),
        "b_spatial": f(inputs["b_spatial"][0]),
        "w_out": f(inputs["w_out"][0]),
        "ln1_g": f(inputs["ln1_g"][0]).reshape(1, D),
        "ln1_b": f(inputs["ln1_b"][0]).reshape(1, D),
        "w_mem_q": f(inputs["w_mem_q"][0]),
        "w_mem_kv": f(inputs["w_mem_kv"][0]),
        "w_mem_o": f(inputs["w_mem_o"][0]),
        "ln2_g": f(inputs["ln2_g"][0]).reshape(1, D),
        "ln2_b": f(inputs["ln2_b"][0]).reshape(1, D),
        "w_router": f(inputs["w_router"][0]),
        "router_bias": f(inputs["router_bias"][0]).reshape(1, NEXP),
        "w_exp_gate": f(inputs["w_exp_gate"][0]),
        "w_exp_up": f(inputs["w_exp_up"][0]),
        "w_exp_down": f(inputs["w_exp_down"][0]),
        "w_sh_gate": f(inputs["w_sh_gate"][0]),
        "w_sh_up": f(inputs["w_sh_up"][0]),
        "w_sh_down": f(inputs["w_sh_down"][0]),
        "ln3_g": f(inputs["ln3_g"][0]).reshape(1, D),
        "ln3_b": f(inputs["ln3_b"][0]).reshape(1, D),
    }
    return m


def kernel(**inputs):
    dbg = bool(os.environ.get("MK_DEBUG"))
    if dbg not in _NC_CACHE:
        _NC_CACHE[dbg] = build(dbg, int(os.environ.get("MK_STOP", "99")))
    nc = _NC_CACHE[dbg]
    shared = _prep_inputs(inputs, 0)
    in_maps = []
    for b in range(8):
        m = dict(shared)
        m["x"] = np.ascontiguousarray(np.asarray(inputs["x"][b], dtype=np.float32))
        m["mem"] = np.ascontiguousarray(np.asarray(inputs["mem"][b], dtype=np.float32))
        in_maps.append(m)
    res = run_bass_kernel_spmd(nc, in_maps, core_ids=list(range(8)))
    out = np.stack([np.asarray(r["out"], dtype=np.float32) for r in res.results], axis=0)
    if dbg:
        kernel.debug = [{k: np.asarray(v) for k, v in r.items()} for r in res.results]
    return out
```
